# Optimizing a Trainium2 kernel written in Bass

```python
import math
import jax
import jax.numpy as jnp
from jax import lax
import numpy as np

D_MODEL = 1024
BATCH = 4
SEQ = 4096
DEPTH = 1

NSA_HEADS = 8
NSA_KV_GROUPS = 2
NSA_HPG = NSA_HEADS // NSA_KV_GROUPS
HEAD_DIM = 64
CMP_BLOCK = 32
CMP_STRIDE = 16
CMP_HIDDEN = 128
SLC_BLOCK = 64
SLC_TOPN = 16
WINDOW = 512
Q_BLOCK = 128
GLA_HEADS = 4
GLA_DK = 64
GLA_DV = 128
GLA_GATE_RANK = 16
GLA_TAU = 16.0
GLA_CHUNK = 64
REL_BUCKETS = 32
REL_MAX_EXACT = REL_BUCKETS // 2
REL_MAX_DIST = 128
N_GROUPS = 4
EXPERTS_PER_GROUP = 8
N_EXPERTS = N_GROUPS * EXPERTS_PER_GROUP
TOPK_IN_GROUP = 2
EXPERT_FF = 256
EPS = 1e-6

NSA_WIDTH = NSA_HEADS * HEAD_DIM
KV_WIDTH = NSA_KV_GROUPS * HEAD_DIM
GLA_K_WIDTH = GLA_HEADS * GLA_DK
GLA_V_WIDTH = GLA_HEADS * GLA_DV
IN_SPLITS = (NSA_WIDTH, KV_WIDTH, KV_WIDTH, KV_WIDTH, KV_WIDTH, KV_WIDTH, KV_WIDTH,
             3 * NSA_HEADS, GLA_K_WIDTH, GLA_K_WIDTH, GLA_V_WIDTH, GLA_GATE_RANK,
             GLA_V_WIDTH, D_MODEL, D_MODEL)

kernel_name = "hybrid_nsa_gla_hmoe_block"


def rmsnorm(x, g):
    xf = x.astype(jnp.float32)
    y = xf * lax.rsqrt(jnp.mean(xf * xf, axis=-1, keepdims=True) + EPS)
    return (y * g.astype(jnp.float32)).astype(x.dtype)


def masked_softmax(s, mask):
    s = jnp.where(mask, s.astype(jnp.float32), -jnp.inf)
    m = jnp.max(s, axis=-1, keepdims=True)
    m = jnp.where(jnp.isfinite(m), m, 0.0)
    e = jnp.where(mask, jnp.exp(s - m), 0.0)
    return e / jnp.maximum(jnp.sum(e, axis=-1, keepdims=True), jnp.finfo(jnp.float32).tiny)


def t5_bucket(rel):
    n = jnp.maximum(rel, 0)
    nf = jnp.maximum(n, 1).astype(jnp.float32)
    large = REL_MAX_EXACT + (jnp.log(nf / REL_MAX_EXACT) / math.log(REL_MAX_DIST / REL_MAX_EXACT)
                             * (REL_BUCKETS - REL_MAX_EXACT)).astype(jnp.int32)
    return jnp.where(n < REL_MAX_EXACT, n, jnp.minimum(large, REL_BUCKETS - 1))


def compress(t, pe, w1, w2):
    B, G, S, dh = t.shape
    sub = t.reshape(B, G, S // CMP_STRIDE, CMP_STRIDE, dh)
    blocks = jnp.concatenate([sub[:, :, :-1], sub[:, :, 1:]], axis=3) + pe
    flat = blocks.reshape(B, G, blocks.shape[2], CMP_BLOCK * dh)
    return jax.nn.gelu(flat @ w1) @ w2


def block_overlap(n_cmp, n_slc):
    ci = np.arange(n_cmp)[:, None] * CMP_STRIDE
    sj = np.arange(n_slc)[None, :] * SLC_BLOCK
    return ((ci < sj + SLC_BLOCK) & (ci + CMP_BLOCK > sj)).astype(np.float32)


def selected_attention(qh, ks, vs, idx, tbl):
    B, G, HPG, S, dh = qh.shape
    n_top = idx.shape[-1]
    nqb = S // Q_BLOCK
    kb = ks.reshape(B, G, S // SLC_BLOCK, SLC_BLOCK, dh)
    vb = vs.reshape(B, G, S // SLC_BLOCK, SLC_BLOCK, dh)

    def one_group(q, kb_g, vb_g, idx_g, tpos, tbl_g):
        kg = kb_g[idx_g]
        vg = vb_g[idx_g]
        s = jnp.einsum('hqd,qnld->hqnl', q, kg)
        kpos = idx_g[..., None] * SLC_BLOCK + jnp.arange(SLC_BLOCK)
        rel = tpos[:, None, None] - kpos
        s = s + tbl_g[:, t5_bucket(rel)]
        tq = idx_g.shape[0]
        p = masked_softmax(s.reshape(HPG, tq, n_top * SLC_BLOCK), (rel >= 0).reshape(tq, n_top * SLC_BLOCK))
        return jnp.einsum('hqm,qmd->hqd', p.astype(vg.dtype), vg.reshape(tq, n_top * SLC_BLOCK, dh))

    per_g = jax.vmap(one_group, in_axes=(0, 0, 0, 0, None, 0))
    per_bg = jax.vmap(per_g, in_axes=(0, 0, 0, 0, None, None))
    q_blocks = qh.reshape(B, G, HPG, nqb, Q_BLOCK, dh).transpose(3, 0, 1, 2, 4, 5)
    idx_blocks = idx.reshape(B, G, nqb, Q_BLOCK, n_top).transpose(2, 0, 1, 3, 4)
    t_blocks = jnp.arange(S).reshape(nqb, Q_BLOCK)
    out = lax.map(lambda a: per_bg(a[0], kb, vb, a[1], a[2], tbl), (q_blocks, idx_blocks, t_blocks))
    return out.transpose(1, 2, 3, 0, 4, 5).reshape(B, G, HPG, S, dh)


def window_attention(qh, kw, vw, tbl):
    B, G, HPG, S, dh = qh.shape
    nqb = S // Q_BLOCK
    nprev = WINDOW // Q_BLOCK
    span = (nprev + 1) * Q_BLOCK

    def band(t):
        tp = jnp.pad(t, ((0, 0), (0, 0), (WINDOW, 0), (0, 0))).reshape(B, G, nqb + nprev, Q_BLOCK, dh)
        return jnp.concatenate([tp[:, :, j:j + nqb] for j in range(nprev + 1)], axis=3)

    kband, vband = band(kw), band(vw)
    qb = qh.reshape(B, G, HPG, nqb, Q_BLOCK, dh)
    s = jnp.einsum('bghnqd,bgnkd->bghnqk', qb, kband)
    ql = jnp.arange(Q_BLOCK)[:, None]
    kl = jnp.arange(span)[None, :]
    rel = ql + WINDOW - kl
    kabs = jnp.arange(nqb)[:, None, None] * Q_BLOCK - WINDOW + kl[None]
    mask = (rel >= 0) & (rel < WINDOW) & (kabs >= 0)
    s = s + tbl[:, :, t5_bucket(rel)][:, :, None]
    p = masked_softmax(s, mask)
    o = jnp.einsum('bghnqk,bgnkd->bghnqd', p.astype(vband.dtype), vband)
    return o.reshape(B, G, HPG, S, dh)


def nsa_mixer(q, kc, vc, ks, vs, kw, vw, gates, pe_k, ck1, ck2, pe_v, cv1, cv2, rel_bias):
    B, S, _ = q.shape
    G, HPG, dh = NSA_KV_GROUPS, NSA_HPG, HEAD_DIM
    qh = q.reshape(B, S, G, HPG, dh).transpose(0, 2, 3, 1, 4) * (dh ** -0.5)

    def heads_kv(t):
        return t.reshape(B, S, G, dh).transpose(0, 2, 1, 3)

    tbl = rel_bias.T.reshape(G, HPG, REL_BUCKETS)
    tpos = jnp.arange(S)

    kcb = compress(heads_kv(kc), pe_k, ck1, ck2)
    vcb = compress(heads_kv(vc), pe_v, cv1, cv2)
    n_cmp = kcb.shape[2]
    cend = jnp.arange(n_cmp) * CMP_STRIDE + CMP_BLOCK - 1
    rel_c = tpos[:, None] - cend[None, :]
    s_c = jnp.einsum('bghsd,bgcd->bghsc', qh, kcb) + tbl[:, :, t5_bucket(rel_c)]
    p_c = masked_softmax(s_c, rel_c >= 0)
    o_c = jnp.einsum('bghsc,bgcd->bghsd', p_c.astype(vcb.dtype), vcb)

    n_slc = S // SLC_BLOCK
    n_top = min(SLC_TOPN, n_slc)
    imp = jnp.einsum('bghsc,cj->bgsj', p_c, jnp.asarray(block_overlap(n_cmp, n_slc)))
    j = jnp.arange(n_slc)[None, :]
    tb = (tpos // SLC_BLOCK)[:, None]
    forced = (j == 0) | (j == tb) | (j == tb - 1)
    future = j * SLC_BLOCK > tpos[:, None]
    imp = jnp.where(forced, 1e30, jnp.where(future, -1e30, imp))
    _, idx = lax.top_k(imp, n_top)
    o_s = selected_attention(qh, heads_kv(ks), heads_kv(vs), idx, tbl)

    o_w = window_attention(qh, heads_kv(kw), heads_kv(vw), tbl)

    g = jax.nn.sigmoid(gates.astype(jnp.float32)).reshape(B, S, 3, G, HPG).transpose(2, 0, 3, 4, 1)[..., None]
    o = g[0] * o_c + g[1] * o_s + g[2] * o_w
    return o.transpose(0, 3, 1, 2, 4).reshape(B, S, NSA_WIDTH).astype(q.dtype)


def gla_mixer(q, k, v, a_lr, r, w_alpha, b_alpha, norm_g):
    B, S, _ = q.shape
    H, dk, dv, C = GLA_HEADS, GLA_DK, GLA_DV, GLA_CHUNK
    nc = S // C
    f32 = jnp.float32
    log_a = jax.nn.log_sigmoid((a_lr @ w_alpha + b_alpha).astype(f32)) / GLA_TAU

    def chunks(t, d):
        return t.astype(f32).reshape(B, nc, C, H, d).transpose(0, 3, 1, 2, 4)

    qc = chunks(q, dk) * (dk ** -0.5)
    kc = chunks(k, dk)
    vc = chunks(v, dv)
    bc = jnp.cumsum(chunks(log_a, dk), axis=3)
    b_last = bc[:, :, :, -1:]
    q_in = qc * jnp.exp(bc)
    k_in = kc * jnp.exp(-bc)
    causal = jnp.tril(jnp.ones((C, C), dtype=bool))
    attn = jnp.where(causal, jnp.einsum('bhncd,bhnjd->bhncj', q_in, k_in), 0.0)
    o_intra = jnp.einsum('bhncj,bhnjv->bhncv', attn, vc)
    chunk_state = jnp.einsum('bhncd,bhncv->bhndv', kc * jnp.exp(b_last - bc), vc)
    decay = jnp.exp(b_last[:, :, :, 0])

    def step(state, xs):
        dec, upd = xs
        return dec[..., None] * state + upd, state

    _, prev = lax.scan(step, jnp.zeros((B, H, dk, dv), f32),
                       (decay.transpose(2, 0, 1, 3), chunk_state.transpose(2, 0, 1, 3, 4)))
    prev = prev.transpose(1, 2, 0, 3, 4)
    o = o_intra + jnp.einsum('bhncd,bhndv->bhncv', q_in, prev)
    o = o.transpose(0, 2, 3, 1, 4).reshape(B, S, H, dv)
    o = o * lax.rsqrt(jnp.mean(o * o, axis=-1, keepdims=True) + EPS)
    o = o.reshape(B, S, H * dv) * norm_g.astype(f32) * jax.nn.silu(r.astype(f32))
    return o.astype(q.dtype)


def hier_moe(h, w_rg, b_rg, w_re, b_re, w_gate, w_up, w_down):
    B, S, D = h.shape
    T = B * S
    t = h.reshape(T, D)
    p_group = jax.nn.softmax((t @ w_rg + b_rg).astype(jnp.float32), axis=-1)
    g_sel = jnp.argmax(p_group, axis=-1)
    g_prob = jnp.max(p_group, axis=-1)
    e_logits = (t @ w_re + b_re).astype(jnp.float32).reshape(T, N_GROUPS, EXPERTS_PER_GROUP)
    e_sel = jnp.take_along_axis(e_logits, g_sel[:, None, None], axis=1)[:, 0]
    top_v, top_i = lax.top_k(e_sel, TOPK_IN_GROUP)
    top_w = jax.nn.softmax(top_v, axis=-1) * g_prob[:, None]
    w_local = jnp.einsum('tk,tke->te', top_w, jax.nn.one_hot(top_i, EXPERTS_PER_GROUP, dtype=jnp.float32))
    combine = jax.nn.one_hot(g_sel, N_GROUPS, dtype=jnp.float32)[:, :, None] * w_local[:, None, :]
    wg = w_gate.reshape(N_GROUPS, EXPERTS_PER_GROUP, D, EXPERT_FF)
    wu = w_up.reshape(N_GROUPS, EXPERTS_PER_GROUP, D, EXPERT_FF)
    wd = w_down.reshape(N_GROUPS, EXPERTS_PER_GROUP, EXPERT_FF, D)
    y = jnp.zeros((T, D), dtype=t.dtype)
    for gi in range(N_GROUPS):
        a = jnp.einsum('td,edf->tef', t, wg[gi])
        u = jnp.einsum('td,edf->tef', t, wu[gi])
        hid = jax.nn.silu(a) * u * combine[:, gi, :, None].astype(t.dtype)
        y = y + jnp.einsum('tef,efd->td', hid, wd[gi])
    return y.reshape(B, S, D)


def setup_inputs(seed: int = 0) -> dict:
    key = jax.random.key(seed)
    ks = jax.random.split(key, 26)
    f32 = jnp.float32
    L = DEPTH
    n_in = sum(IN_SPLITS)

    def nrm(k, shape, scale):
        return jax.random.normal(k, shape, f32) * scale

    return {
        "x": nrm(ks[0], (BATCH, SEQ, D_MODEL), 1.0),
        "g_mix": 1.0 + nrm(ks[1], (L, D_MODEL), 0.02),
        "w_in": nrm(ks[2], (L, D_MODEL, n_in), D_MODEL ** -0.5),
        "nsa_pe_k": nrm(ks[3], (L, CMP_BLOCK, HEAD_DIM), 0.1),
        "nsa_cmp_k_w1": nrm(ks[4], (L, CMP_BLOCK * HEAD_DIM, CMP_HIDDEN), (CMP_BLOCK * HEAD_DIM) ** -0.5),
        "nsa_cmp_k_w2": nrm(ks[5], (L, CMP_HIDDEN, HEAD_DIM), CMP_HIDDEN ** -0.5),
        "nsa_pe_v": nrm(ks[6], (L, CMP_BLOCK, HEAD_DIM), 0.1),
        "nsa_cmp_v_w1": nrm(ks[7], (L, CMP_BLOCK * HEAD_DIM, CMP_HIDDEN), (CMP_BLOCK * HEAD_DIM) ** -0.5),
        "nsa_cmp_v_w2": nrm(ks[8], (L, CMP_HIDDEN, HEAD_DIM), CMP_HIDDEN ** -0.5),
        "rel_bias": nrm(ks[9], (REL_BUCKETS, NSA_HEADS), 0.2),
        "gla_w_alpha": nrm(ks[10], (L, GLA_GATE_RANK, GLA_K_WIDTH), GLA_GATE_RANK ** -0.5),
        "gla_b_alpha": nrm(ks[11], (L, GLA_K_WIDTH), 0.01),
        "gla_norm_g": 1.0 + nrm(ks[12], (L, GLA_V_WIDTH), 0.02),
        "w_branch_a": nrm(ks[13], (L, NSA_WIDTH, D_MODEL), NSA_WIDTH ** -0.5),
        "w_branch_b": nrm(ks[14], (L, GLA_V_WIDTH, D_MODEL), GLA_V_WIDTH ** -0.5),
        "w_out": nrm(ks[15], (L, D_MODEL, D_MODEL), D_MODEL ** -0.5),
        "g_ffn": 1.0 + nrm(ks[16], (L, D_MODEL), 0.02),
        "w_router_group": nrm(ks[17], (L, D_MODEL, N_GROUPS), D_MODEL ** -0.5),
        "b_router_group": nrm(ks[18], (L, N_GROUPS), 0.01),
        "w_router_expert": nrm(ks[19], (L, D_MODEL, N_EXPERTS), D_MODEL ** -0.5),
        "b_router_expert": nrm(ks[20], (L, N_EXPERTS), 0.01),
        "w_exp_gate": nrm(ks[21], (L, N_EXPERTS, D_MODEL, EXPERT_FF), D_MODEL ** -0.5),
        "w_exp_up": nrm(ks[22], (L, N_EXPERTS, D_MODEL, EXPERT_FF), D_MODEL ** -0.5),
        "w_exp_down": nrm(ks[23], (L, N_EXPERTS, EXPERT_FF, D_MODEL), EXPERT_FF ** -0.5),
        "g_final": 1.0 + nrm(ks[24], (D_MODEL,), 0.02),
    }


def reference(x, g_mix, w_in, nsa_pe_k, nsa_cmp_k_w1, nsa_cmp_k_w2, nsa_pe_v, nsa_cmp_v_w1,
              nsa_cmp_v_w2, rel_bias, gla_w_alpha, gla_b_alpha, gla_norm_g, w_branch_a, w_branch_b,
              w_out, g_ffn, w_router_group, b_router_group, w_router_expert, b_router_expert,
              w_exp_gate, w_exp_up, w_exp_down, g_final):
    split_at = np.cumsum(IN_SPLITS)[:-1].tolist()
    for l in range(DEPTH):
        h = rmsnorm(x, g_mix[l])
        proj = h @ w_in[l]
        (q_a, k_cmp, v_cmp, k_slc, v_slc, k_win, v_win, gate_a,
         q_b, k_b, v_b, a_b, r_b, mg_a, mg_b) = jnp.split(proj, split_at, axis=-1)
        y_a = nsa_mixer(q_a, k_cmp, v_cmp, k_slc, v_slc, k_win, v_win, gate_a,
                        nsa_pe_k[l], nsa_cmp_k_w1[l], nsa_cmp_k_w2[l],
                        nsa_pe_v[l], nsa_cmp_v_w1[l], nsa_cmp_v_w2[l], rel_bias)
        y_b = gla_mixer(q_b, k_b, v_b, a_b, r_b, gla_w_alpha[l], gla_b_alpha[l], gla_norm_g[l])
        merged = jax.nn.sigmoid(mg_a) * (y_a @ w_branch_a[l]) + jax.nn.sigmoid(mg_b) * (y_b @ w_branch_b[l])
        x = x + merged @ w_out[l]
        x = x + hier_moe(rmsnorm(x, g_ffn[l]), w_router_group[l], b_router_group[l],
                         w_router_expert[l], b_router_expert[l],
                         w_exp_gate[l], w_exp_up[l], w_exp_down[l])
    return rmsnorm(x, g_final)
```

```python
import math
from contextlib import ExitStack
import numpy as np
import ml_dtypes
import concourse.bass as bass
import concourse.mybir as mybir
from concourse.bass_utils import run_bass_kernel_spmd

F32 = mybir.dt.float32
BF16 = mybir.dt.bfloat16
AF = mybir.ActivationFunctionType
ALU = mybir.AluOpType
AX = mybir.AxisListType
BF = ml_dtypes.bfloat16

S = 4096
D = 1024
NB = 32
HALF = 2048
NEG = -30000.0
EPS = 1e-6
N_EXP = 32
W1C = 1168
W2C = 396
GC1 = 0.7978845608028654
GC2 = 0.044715


class Prog:
    def __init__(self):
        self.ops = []
        self.lw = {}
        self.rd = {}

    stopped = False

    def add(self, eng, fn, reads=(), writes=(), dma=None, inc=16):
        if self.stopped:
            return -1
        i = len(self.ops)
        reads = tuple(reads)
        writes = tuple(writes) + tuple(k for k in reads if k.startswith("ps") and k not in writes)
        deps = {}
        for k in reads:
            w = self.lw.get(k)
            if w is not None:
                deps[w] = True
        for k in writes:
            w = self.lw.get(k)
            if w is not None:
                deps.setdefault(w, False)
            for r in self.rd.get(k, ()):
                deps.setdefault(r, False)
        for k in writes:
            self.lw[k] = i
            self.rd[k] = []
        for k in reads:
            if k not in writes:
                self.rd.setdefault(k, []).append(i)
        self.ops.append(dict(eng=eng, fn=fn, deps=deps, dma=dma, need=False, waits=[], inc=inc))
        return i

    def barrier(self, engines):
        if self.stopped:
            return
        last = {}
        lastd = {}
        for i, o in enumerate(self.ops):
            last[o["eng"]] = i
            if o["dma"] is not None:
                lastd[o["dma"]] = i
        deps = {i: True for i in list(last.values()) + list(lastd.values())}
        for e in engines:
            self.ops.append(dict(eng=e, fn=(lambda en: en.nop()), deps=dict(deps), dma=None,
                                 need=False, waits=[], bar=True, inc=16))

    def finalize(self):
        ops = self.ops
        for i, o in enumerate(ops):
            for j, raw in o["deps"].items():
                pj = ops[j]
                if pj["dma"] is None and o["dma"] is None and pj["eng"] == o["eng"] and not o.get("bar"):
                    if o["eng"] == "pe":
                        continue
                pj["need"] = True
                o["waits"].append(j)
        cnt = {}
        dcnt = {}
        for o in ops:
            ws = []
            for j in sorted(o["waits"]):
                pj = ops[j]
                if pj["dma"] is not None:
                    ws.append(("dma:" + pj["dma"], dcnt[pj["dma"]]))
                else:
                    ws.append(pj["sig"])
            o["ws"] = ws
            if o["dma"] is not None:
                dcnt[o["dma"]] = dcnt.get(o["dma"], 0) + o["inc"]
                o["sig"] = ("dma:" + o["dma"], dcnt[o["dma"]])
            elif o["need"]:
                cnt[o["eng"]] = cnt.get(o["eng"], 0) + 1
                o["sig"] = (o["eng"], cnt[o["eng"]])
        self.dcnt = dcnt
        print('SEM counts', cnt, 'n_dma_keys', len(dcnt))
        return sorted(dcnt.keys())

    def check(self):
        ops = self.ops
        q = {}
        for i, o in enumerate(ops):
            q.setdefault(o["eng"], []).append(i)
        pos = {e: 0 for e in q}
        sem = {}
        prog = True
        n_done = 0
        while prog:
            prog = False
            for e, lst in q.items():
                while pos[e] < len(lst):
                    o = ops[lst[pos[e]]]
                    if all(sem.get(s_, 0) >= v_ for s_, v_ in o["ws"]):
                        if o["dma"] is not None:
                            k_ = "dma:" + o["dma"]
                            sem[k_] = sem.get(k_, 0) + o["inc"]
                        elif o["need"]:
                            sem[e] = sem.get(e, 0) + 1
                        pos[e] += 1
                        n_done += 1
                        prog = True
                    else:
                        break
        stuck = {e: (pos[e], len(lst)) for e, lst in q.items() if pos[e] < len(lst)}
        if stuck:
            for e, (p_, n_) in stuck.items():
                o = ops[q[e][p_]]
                print("STUCK", e, p_, n_, [(s_, v_, sem.get(s_, 0)) for s_, v_ in o["ws"] if sem.get(s_, 0) < v_])
        else:
            print("protocol check OK", n_done)
        return not stuck

    def emit_engine(self, ename, e, semh):
        ops = self.ops
        seen = {}
        for o in ops:
            if o["eng"] != ename:
                continue
            for s, v in o["ws"]:
                if seen.get(s, 0) >= v:
                    continue
                seen[s] = v
                e.wait_ge(semh[s], v)
            ins = o["fn"](e)
            if o["dma"] is not None:
                ins.then_inc(semh["dma:" + o["dma"]], o["inc"])
            elif o["need"]:
                ins.then_inc(semh[ename], 1)
        if ename == "sp":
            for k, v in self.dcnt.items():
                if seen.get("dma:" + k, 0) < v:
                    e.wait_ge(semh["dma:" + k], v)


def _bucket(rel):
    n = np.maximum(rel, 0)
    nf = np.maximum(n, 1).astype(np.float32)
    large = 16 + (np.log(nf / np.float32(16)) / np.float32(math.log(8.0)) * np.float32(16)).astype(np.int32)
    return np.where(n < 16, n, np.minimum(large, 31))


def _consts():
    c = {}
    k = np.arange(128)[:, None]
    q = np.arange(128)[None, :]
    c["idx0"] = _bucket(q - k)
    c["vis0"] = (q >= k)
    c["idx1"] = _bucket(128 + q - k)
    c["M0"] = np.where(q >= k, 0.0, NEG).astype(np.float32)
    c["M4"] = np.where(k > q, 0.0, NEG).astype(np.float32)
    qq = np.arange(128)[:, None]
    dd = np.arange(504)[None, :] - 248
    relw = qq - 16 * dd - 31
    c["idxw"] = _bucket(relw)
    c["visw"] = relw >= 0
    c["MW"] = np.where(relw >= 0, 0.0, NEG).astype(np.float32)
    jj = np.arange(128)[None, :] - 64
    tbr = (np.arange(128)[:, None] >= 64).astype(np.int64)
    forced = (jj == tbr) | (jj == tbr - 1)
    future = jj > tbr
    c["KEEP"] = np.where(forced | future, 0.0, 1.0).astype(np.float32)
    c["ADD"] = np.where(forced, 1e30, np.where(future, -1e30, 0.0)).astype(np.float32)
    c["EXPM"] = (np.arange(S)[None, :] // 64 == np.arange(64)[:, None]).astype(BF)
    c["identb"] = np.eye(128).astype(BF)
    c["identf"] = np.eye(128).astype(np.float32)
    c["MT"] = ((k // 64 == q // 64) & (k <= q)).astype(np.float32)
    sm = np.ones((64, 512), np.float32)
    sm[:, ::64] = 0.0
    c["scanmask"] = sm
    ci = np.arange(256)[:, None] * 16
    sj = np.arange(64)[None, :] * 64
    ov = ((ci < sj + 64) & (ci + 32 > sj)).astype(np.float32)
    ov[255] = 0.0
    c["ovl"] = np.ascontiguousarray(ov.reshape(2, 128, 64).transpose(1, 0, 2)).astype(BF)
    selm = np.zeros((32, 32, 128), np.float32)
    for e in range(32):
        selm[e, e, :] = 1.0
    c["SELM"] = selm.reshape(32, 32 * 128)
    return c


OPTS = dict(nch=8, sub=99, nexp=32)


def build_nc(debug=False, stop=99):
    nc = bass.Bass("TRN2", target_bir_lowering=False)
    P = Prog()
    din = {}

    def dram_in(name, shape, dt=F32):
        din[name] = nc.dram_tensor(name, list(shape), dt, kind="ExternalInput").ap()
        return din[name]

    xT = dram_in("xT", [D, S])
    xTh = dram_in("xTh", [D, HALF])
    sel01 = dram_in("sel01", [128, 2])
    w1 = dram_in("w1", [D, W1C])
    w2 = dram_in("w2", [D, W2C])
    w3 = dram_in("w3", [D, 2048])
    gvec = dram_in("gvec", [128, 16])
    gfin = dram_in("gfin", [128, D])
    cw1 = dram_in("cw1", [2, 128, 16 * 128])
    cw2 = dram_in("cw2", [2, 128, 64])
    cpe = dram_in("cpe", [2, 128, 16])
    ovl_d = dram_in("ovl", [128, 2, 64], BF16)
    t0raw = dram_in("t0raw", [128, 512])
    t1raw = dram_in("t1raw", [128, 512])
    wraw = dram_in("wraw", [128, 4 * 504])
    m0_d = dram_in("M0", [128, 128])
    m4_d = dram_in("M4", [128, 128])
    mw_d = dram_in("MW", [128, 504])
    b31_d = dram_in("b31", [128, 4])
    keep_d = dram_in("KEEP", [128, 128])
    add_d = dram_in("ADD", [128, 128])
    expm_d = dram_in("EXPM", [64, S], BF16)
    identb_d = dram_in("identb", [128, 128], BF16)
    identf_d = dram_in("identf", [128, 128])
    mt_d = dram_in("MT", [128, 128])
    scanm_d = dram_in("scanmask", [64, 512])
    wal_d = dram_in("wal", [16, 128])
    nbal_d = dram_in("nbal", [64, 2])
    ng_d = dram_in("ng", [128, 2])
    wa_d = dram_in("wa", [512, D])
    wb_d = dram_in("wb", [512, D])
    wo_d = dram_in("wo", [D, D])
    wr_d = dram_in("wr", [D, 36])
    br_d = dram_in("br", [128, 36])
    selm_d = dram_in("SELM", [32, 32 * 128])
    wg_d = dram_in("wg", [OPTS["nexp"], D, 256])
    wu_d = dram_in("wu", [OPTS["nexp"], D, 256])
    wd_d = dram_in("wd", [OPTS["nexp"], 256, D])
    out_d = nc.dram_tensor("out", [HALF, D], F32, kind="ExternalOutput").ap()
    ysrc = [nc.dram_tensor("ysrc%d" % i, [512, 512], BF16) for i in range(8)]
    ydst = [nc.dram_tensor("ydst%d" % i, [1024, 512], BF16) for i in range(8)]
    w3b = [nc.dram_tensor("w3b%d" % i, [128, 8 * 128], BF16) for i in range(16)]
    wab_s = nc.dram_tensor("wab_s", [128, 4 * D], BF16)
    wbb_s = nc.dram_tensor("wbb_s", [128, 4 * D], BF16)
    wob_s = nc.dram_tensor("wob_s", [128, 8 * D], BF16)
    if debug:
        dbg_y = nc.dram_tensor("dbg_y", [1024, S], BF16, kind="ExternalOutput").ap()
        dbg_x1 = nc.dram_tensor("dbg_x1", [D, HALF], F32, kind="ExternalOutput").ap()
        dbg_br = nc.dram_tensor("dbg_br", [3, S, 256], F32, kind="ExternalOutput").ap()

    SB0 = 16512
    sboff = {}
    off = [SB0]

    def sb(name, shape, dt, at=None):
        nbytes = int(np.prod(shape[1:])) * (4 if dt == F32 else 2)
        nbytes = (nbytes + 63) // 64 * 64
        if at is None:
            o = off[0]
            off[0] += nbytes
        else:
            o = at
        assert o + nbytes <= 229344, (name, o, nbytes)
        sboff[name] = o
        return nc.alloc_sbuf_tensor_at(name, list(shape), dt, offset=o)

    BIG = sb("BIG", [128, 16384], F32)
    hT = nc.alloc_sbuf_tensor_at("hT", [128, 8 * S], BF16, offset=SB0)
    identb = sb("identb", [128, 128], BF16)
    identf = sb("identf", [128, 128], F32)
    onesb = sb("onesb", [128, 128], BF16)
    gv = sb("gv", [128, 16], F32)
    s01 = sb("s01", [128, 2], F32)
    common_end = off[0]

    W1b = sb("W1b", [128, 8 * W1C], BF16)
    W2b = sb("W2b", [128, 8 * W2C], BF16)
    KE = sb("KE", [128, S], BF16)
    KwT = sb("KwT", [64, S], BF16)
    VS = sb("VS", [128, NB * 65], BF16)
    VW = sb("VW", [128, NB * 65], BF16)
    cw2b = [sb("cw2b%d" % i, [128, 64], BF16) for i in range(2)]
    cconst = sb("cconst", [128, 2], F32)
    kcbT = sb("kcbT", [64, 256], BF16)
    VAUG = sb("VAUG", [128, 2 * 128], BF16)
    T0 = sb("T0", [128, 512], BF16)
    T1 = sb("T1", [128, 512], BF16)
    T4 = sb("T4", [128, 512], BF16)
    Wt = sb("Wt", [128, 4 * 504], F32)
    b31 = sb("b31", [128, 4], F32)
    KEEP = sb("KEEP", [128, 128], F32)
    ADDT = sb("ADDT", [128, 128], F32)
    MTs = sb("MTs", [128, 128], F32)
    scanm = sb("scanm", [64, 512], F32)
    walb = sb("walb", [16, 128], BF16)
    nbal = sb("nbal", [64, 2], F32)
    ngs = sb("ngs", [128, 2], F32)
    GS = sb("GS", [128, NB * 12], F32)
    r_start = off[0]
    wst = [sb("wst%d" % i, [128, 8 * 256], F32) for i in range(2)]
    xs = wst
    sqb = sb("sqb", [128, 8 * 256], BF16)
    rstd = sb("rstd", [128, 256], F32)
    kcT2 = sb("kcT2", [128, S + 16], BF16)
    vcT2 = sb("vcT2", [128, S + 16], BF16)
    cw1b = [sb("cw1b%d" % i, [128, 16 * 128], BF16) for i in range(2)]
    cpeb = [sb("cpeb%d" % i, [128, 16], BF16) for i in range(2)]
    cx = sb("cx", [128, 256], F32)
    ct1 = sb("ct1", [128, 256], F32)
    ct2 = sb("ct2", [128, 256], F32)
    chid = [sb("chid%d" % i, [128, 256], BF16) for i in range(2)]
    tmpT = sb("tmpT", [128, 4 * 504], F32)
    MWs = sb("MWs", [128, 504], F32)
    M0s = sb("M0s", [128, 128], F32)
    M4s = sb("M4s", [128, 128], F32)
    walf = sb("walf", [16, 128], F32)
    r1_end = off[0]
    off[0] = r_start
    QN = [sb("QN%d" % i, [128, 4 * 512], BF16) for i in range(2)]
    VB = [sb("VB%d" % i, [128, 4 * 256], BF16) for i in range(2)]
    scc = sb("scc", [128, 256], F32)
    ecb = sb("ecb", [128, 256], BF16)
    eT = sb("eT", [128, 256], BF16)
    ssum = sb("ssum", [128, 4], F32)
    rr = sb("rr", [128, 4], F32)
    coef = sb("coef", [128, 4], F32)
    ya = sb("ya", [128, 256], F32)
    yab = sb("yab", [128, 256], BF16)
    imp = sb("imp", [128, 64], F32)
    impm = sb("impm", [128, 64], F32)
    imp2 = sb("imp2", [128, 64], F32)
    m8 = sb("m8", [128, 16], F32)
    negp = sb("negp", [128, 128], BF16)
    PT = [sb("PT%d" % i, [128, 512], BF16) for i in range(4)]
    yw = sb("yw", [128, 256], F32)
    yc = [sb("yc%d" % i, [128, 256], F32) for i in range(2)]
    rrC = sb("rrC", [128, 4], F32)
    coefC = sb("coefC", [128, 4], F32)
    rrB = sb("rrB", [128, 4], F32)
    coefB = sb("coefB", [128, 4], F32)
    gl1 = sb("gl1", [128, 128], F32)
    YT = [sb("YT%d" % i, [128, 4 * 512], BF16) for i in range(2)]
    gq = sb("gq", [64, 2 * 512], BF16)
    gk = sb("gk", [64, 2 * 512], BF16)
    gab = sb("gab", [16, 512], BF16)
    ge1 = sb("ge1", [64, 2 * 512], F32)
    gbc = sb("gbc", [64, 2 * 512], F32)
    qin = [sb("qin%d" % i, [64, 2 * 512], BF16) for i in range(2)]
    kin = [sb("kin%d" % i, [64, 2 * 512], BF16) for i in range(2)]
    kdT = [sb("kdT%d" % i, [64, 2 * 512], BF16) for i in range(2)]
    gdec = [sb("gdec%d" % i, [64, 2 * 8], F32) for i in range(2)]
    rTs = [sb("rTs%d" % i, [128, 2 * 512], BF16) for i in range(2)]
    ATb = [sb("ATb%d" % i, [128, 128], BF16) for i in range(2)]
    kdtok = [sb("kdtok%d" % i, [128, 64], BF16) for i in range(2)]
    St = [sb("St%d" % i, [64, 128], F32) for i in range(2)]
    Sbf = [[sb("Sbf%d_%d" % (i, j), [64, 128], BF16) for j in range(2)] for i in range(2)]
    gsq = sb("gsq", [128, 128], BF16)
    grs = sb("grs", [128, 128], F32)
    gyb = sb("gyb", [128, 128], F32)
    ybr = [sb("ybr%d" % i, [128, 256], F32) for i in range(2)] if debug else None
    off[0] = max(off[0], r1_end)
    cvf = [sb("cvf%d" % i, [128, 512], F32) for i in range(2)]
    cvb = [sb("cvb%d" % i, [128, 512], BF16) for i in range(2)]
    p1_end = off[0]

    off[0] = common_end
    HX = sb("HX", [128, 8 * HALF], BF16)
    COMBT = sb("COMBT", [32, HALF], F32)
    WRf = sb("WRf", [128, 8 * 36], F32)
    brs = sb("brs", [128, 36], F32)
    onesf = sb("onesf", [32, 128], F32)
    rt = sb("rt", [128, 64], F32)
    Lg = sb("Lg", [128, 36], F32)
    comb = sb("comb", [128, 32], F32)
    esel = sb("esel", [128, 8], F32)
    wst2 = [sb("wst2_%d" % i, [128, 8 * 256], F32) for i in range(2)]
    pm_start = off[0]
    Wab = sb("Wab", [128, 4 * D], BF16)
    Wbb = sb("Wbb", [128, 4 * D], BF16)
    Wob = sb("Wob", [128, 8 * D], BF16)
    W3p = [sb("W3p%d" % i, [128, 8 * 128], BF16) for i in range(4)]
    w3cv = [sb("w3cv%d" % i, [128, 8 * 128], BF16) for i in range(2)]
    YM = sb("YM", [128, 8 * 512], BF16)
    YH = sb("YH", [128, 8 * 512], BF16)
    SGp = [sb("SGp%d" % i, [128, 512], BF16) for i in range(2)]
    MG = sb("MG", [128, 8 * 512], BF16)
    m1 = sb("m1", [128, 512], F32)
    m2 = sb("m2", [128, 512], F32)
    xh = [sb("xh%d" % i, [128, 512], F32) for i in range(2)]
    sq2 = YH
    rstd2 = [sb("rstd2_%d" % i, [128, 512], F32) for i in range(2)]
    assert sboff["m2"] == sboff["m1"] + 2048 and sboff["xh1"] == sboff["xh0"] + 2048
    w3st = [nc.alloc_sbuf_tensor_at("w3st0", [128, 8 * 128], F32, offset=sboff["m1"]),
            nc.alloc_sbuf_tensor_at("w3st1", [128, 8 * 128], F32, offset=sboff["xh0"])]
    w3k = [("m1", "m2"), ("xh0", "xh1")]
    pm_end = off[0]
    off[0] = pm_start
    WGb = [sb("WGb%d" % i, [128, 8 * 256], BF16) for i in range(4)]
    WUb = [sb("WUb%d" % i, [128, 8 * 256], BF16) for i in range(4)]
    WDb = [sb("WDb%d" % i, [128, 2 * D], BF16) for i in range(4)]
    CB = [sb("CB%d" % i, [128, 512], BF16) for i in range(4)]
    cmask = [sb("cmask%d" % i, [32, 512], BF16) for i in range(4)]
    sil = [sb("sil%d" % i, [128, 512], BF16) for i in range(2)]
    t1b = [sb("t1b%d" % i, [128, 512], BF16) for i in range(2)]
    hid = [sb("hid%d" % i, [128, 2 * 512], BF16) for i in range(4)]
    xtm = sb("xtm", [128, D], F32)
    otm = sb("otm", [128, D], F32)
    gfs = sb("gfs", [128, D], F32)
    fss = sb("fss", [128, 4], F32)
    junk = hid[0]
    off[0] = max(off[0], pm_end)

    print('SBUF usage', r1_end, pm_end, off[0])
    es = ExitStack()
    PS = [es.enter_context(nc.psum_tensor("ps%d" % i, [128, 512], F32)) for i in range(8)]

    streams = {}
    cur = [None]

    def A(eng, fn, r=(), w=(), dma=None):
        if cur[0] is None:
            return P.add(eng, fn, r, w, dma)
        streams[cur[0]].append((eng, fn, r, w, dma))

    def begin(name):
        cur[0] = name
        streams.setdefault(name, [])

    def end():
        cur[0] = None

    def merge(names):
        lists = [streams.pop(n_) for n_ in names]
        idx = [0] * len(lists)
        tot = [max(1, len(l_)) for l_ in lists]
        while True:
            best = None
            for i_, l_ in enumerate(lists):
                if idx[i_] < len(l_):
                    f_ = (idx[i_] + 1) / tot[i_]
                    if best is None or f_ < best[0]:
                        best = (f_, i_)
            if best is None:
                break
            i_ = best[1]
            P.add(*lists[i_][idx[i_]])
            idx[i_] += 1

    ckn = [0]

    def dma(q, out, in_, r, w, key):
        if len(key) == 2 and key[0] == "c" and key[1].isdigit():
            ckn[0] += 1
            key = "k%d" % ckn[0]
        A(q, lambda e, out=out, in_=in_: e.dma_start(out=out, in_=in_), r, w, dma=key)

    def mm(out, lhsT, rhs, start, stop, r, w, sgc=False):
        A("pe", lambda e, out=out, lhsT=lhsT, rhs=rhs, start=start, stop=stop, sgc=sgc:
          e.matmul(out, lhsT=lhsT, rhs=rhs, start=start, stop=stop, skip_group_check=sgc), r, w)

    def tr(out, in_, ident, r, w):
        A("pe", lambda e, out=out, in_=in_, ident=ident: e.transpose(out=out, in_=in_, identity=ident), r, w)

    def act(out, in_, func, r, w, bias=None, scale=None, accum=None):
        def f(e, out=out, in_=in_, func=func, bias=bias, scale=scale, accum=accum):
            kw = {}
            if bias is not None:
                kw["bias"] = bias
            if scale is not None:
                kw["scale"] = scale
            if accum is not None:
                kw["accum_out"] = accum
            return e.activation(out=out, in_=in_, func=func, **kw)
        A("act", f, r, w)

    def tt(eng, out, in0, in1, op, r, w):
        A(eng, lambda e, out=out, in0=in0, in1=in1, op=op: e.tensor_tensor(out=out, in0=in0, in1=in1, op=op), r, w)

    def ts(eng, out, in0, s1, s2, op0, op1, r, w):
        def f(e, out=out, in0=in0, s1=s1, s2=s2, op0=op0, op1=op1):
            if op1 is None:
                return e.tensor_scalar(out=out, in0=in0, scalar1=s1, scalar2=None, op0=op0)
            return e.tensor_scalar(out=out, in0=in0, scalar1=s1, scalar2=s2, op0=op0, op1=op1)
        A(eng, f, r, w)

    def stt(eng, out, in0, scalar, in1, op0, op1, r, w):
        A(eng, lambda e, out=out, in0=in0, scalar=scalar, in1=in1, op0=op0, op1=op1:
          e.scalar_tensor_tensor(out=out, in0=in0, scalar=scalar, in1=in1, op0=op0, op1=op1), r, w)

    def cp(eng, out, in_, r, w):
        if eng == "act":
            A("act", lambda e, out=out, in_=in_: e.copy(out=out, in_=in_), r, w)
        else:
            A(eng, lambda e, out=out, in_=in_: e.tensor_copy(out=out, in_=in_), r, w)

    def ms(eng, ap, val, w):
        A(eng, lambda e, ap=ap, val=val: e.memset(ap, val), (), w)

    def recip(out, in_, r, w):
        A("dve", lambda e, out=out, in_=in_: e.reciprocal(out=out, in_=in_), r, w)

    def v3(ap, a):
        return ap.rearrange("p (a b) -> p a b", a=a)

    dma("sp", identb[:], identb_d, (), ("identb",), "c0")
    dma("sp", identf[:], identf_d, (), ("identf",), "c0")
    dma("sp", gv[:], gvec, (), ("gv",), "c0")
    dma("sp", s01[:], sel01, (), ("s01",), "c0")
    ms("dve", onesb[:], 1.0, ("onesb",))
    dma("sp", tmpT[:, 0:512], t0raw, (), ("tmpT",), "c1")
    dma("sp", M0s[:], m0_d, (), ("M0s",), "c1")
    dma("sp", M4s[:], m4_d, (), ("M4s",), "c1")
    dma("sp", b31[:], b31_d, (), ("b31",), "c1")
    for h in range(4):
        ts("dve", tmpT[:, h * 128:(h + 1) * 128], tmpT[:, h * 128:(h + 1) * 128], b31[:, h:h + 1], None,
           ALU.subtract, None, ("tmpT", "b31"), ("tmpT",))
        tt("dve", T0[:, h * 128:(h + 1) * 128], tmpT[:, h * 128:(h + 1) * 128], M0s[:], ALU.add,
           ("tmpT", "M0s"), ("T0",))
        cp("dve", T4[:, h * 128:(h + 1) * 128], M4s[:], ("M4s",), ("T4",))
    dma("sp", tmpT[:, 0:512], t1raw, ("T0",), ("tmpT",), "c2")
    for h in range(4):
        ts("dve", T1[:, h * 128:(h + 1) * 128], tmpT[:, h * 128:(h + 1) * 128], b31[:, h:h + 1], None,
           ALU.subtract, None, ("tmpT", "b31"), ("T1",))
    dma("sp", tmpT[:], wraw, ("T1",), ("tmpT",), "c3")
    dma("sp", MWs[:], mw_d, (), ("MWs",), "c3")
    for h in range(4):
        ts("dve", tmpT[:, h * 504:(h + 1) * 504], tmpT[:, h * 504:(h + 1) * 504], b31[:, h:h + 1], None,
           ALU.subtract, None, ("tmpT", "b31"), ("tmpT",))
        tt("dve", Wt[:, h * 504:(h + 1) * 504], tmpT[:, h * 504:(h + 1) * 504], MWs[:], ALU.add,
           ("tmpT", "MWs"), ("Wt",))
    dma("sp", KEEP[:], keep_d, (), ("KEEP",), "c4")
    dma("sp", ADDT[:], add_d, (), ("ADDT",), "c4")
    dma("sp", MTs[:], mt_d, (), ("MTs",), "c4")
    dma("sp", scanm[:], scanm_d, (), ("scanm",), "c4")
    dma("sp", walf[:], wal_d, (), ("walf",), "c4")
    cp("dve", walb[:], walf[:], ("walf",), ("walb",))
    dma("sp", nbal[:], nbal_d, (), ("nbal",), "c4")
    dma("sp", ngs[:], ng_d, (), ("ngs",), "c4")
    dma("sp", KE[64:128, :], expm_d, (), ("KEexp",), "c4")
    dma("sp", v3(VAUG[:], 2)[:, :, 64:128], ovl_d, (), ("VAUGo",), "c4")
    ms("dve", v3(VS[:], NB)[:, :, 64:65], 1.0, ("VSone",))
    ms("dve", v3(VW[:], NB)[:, :, 64:65], 1.0, ("VWone",))
    ms("dve", v3(VAUG[:], 2)[:, :, 0:64], 0.0, ("VAUGv",))
    ms("dve", kcbT[:], 0.0, ("kcbT",))

    wcnt = [0]

    def load_w(dst, src, ncols, gofs, key, dstkey):
        dv = v3(dst[:], 8)
        sv = src.rearrange("(kc p) n -> p kc n", p=128)
        c0 = 0
        while c0 < ncols:
            cw = min(256, ncols - c0)
            i = wcnt[0] % 2
            wcnt[0] += 1
            st = v3(wst[i][:], 8)
            dma("sp", st[:, :, 0:cw], sv[:, :, c0:c0 + cw], (), ("wst%d" % i,), "w%d" % i)
            cp("act" if (wcnt[0] % 2) else "pool", dv[:, :, c0:c0 + cw], st[:, :, 0:cw], ("wst%d" % i,), (dstkey,))
            c0 += cw

    load_w(W1b, w1, W1C, 0, "w", "W1b")
    load_w(W2b, w2, W2C, 0, "w", "W2b")
    for i in range(2):
        dma("sp", wst[i][:, 0:2048], cw1[i], ("W1b", "W2b"), ("wst%d" % i,), "w%d" % i)
        cp("dve", cw1b[i][:], wst[i][:, 0:2048], ("wst%d" % i,), ("cw1b%d" % i,))
        dma("sp", ct1[:, 0:64], cw2[i], ("cw2b%d" % (i - 1),) if i else (), ("ct1",), "c5")
        cp("dve", cw2b[i][:], ct1[:, 0:64], ("ct1",), ("cw2b%d" % i,))
        dma("sp", ct2[:, 0:16], cpe[i], ("cpeb%d" % (i - 1),) if i else (), ("ct2",), "c5")
        cp("dve", cpeb[i][:], ct2[:, 0:16], ("ct2",), ("cpeb%d" % i,))

    W1v = v3(W1b[:], 8)
    W2v = v3(W2b[:], 8)
    hTv = v3(hT[:], 8)
    if stop <= 0:
        P.stopped = True
    for c in range(16):
        t0 = c * 256
        i = c % 2
        xv = v3(xs[i][:], 8)
        dma("sp", xv, xT.rearrange("(kc p) t -> p kc t", p=128)[:, :, t0:t0 + 256],
            (), ("wst%d" % i,), "w%d" % i)
        act(sqb[:], xs[i][:], AF.Square, ("wst%d" % i,), ("sqb",))
        sqv = v3(sqb[:], 8)
        for kc in range(8):
            mm(PS[0][:, 0:256], onesb[:], sqv[:, kc, :], kc == 0, kc == 7, ("onesb", "sqb"), ("ps0",))
        act(rstd[:], PS[0][:, 0:256], AF.Sqrt, ("ps0",), ("rstd",), bias=EPS, scale=1.0 / D)
        recip(rstd[:], rstd[:], ("rstd",), ("rstd",))
        for kc in range(8):
            stt("dve", hTv[:, kc, t0:t0 + 256], xv[:, kc, :], gv[:, kc:kc + 1], rstd[:],
                ALU.mult, ALU.mult, ("wst%d" % i, "rstd", "gv"), ("hT%d" % (c // 2),))
        if c % 2 == 1:
            ch = c // 2
            tc0 = ch * 512
            for which, (dstT, col0) in enumerate(((kcT2, 256), (vcT2, 384))):
                pb = PS[1 + which]
                for kc in range(8):
                    mm(pb[:, :], W1v[:, kc, col0:col0 + 128], hTv[:, kc, tc0:tc0 + 512], kc == 0, kc == 7,
                       ("W1b", "hT%d" % ch), ("ps%d" % (1 + which),))
                nm = "kcT2" if which == 0 else "vcT2"
                cp("act", dstT[0:64, 16 + tc0:16 + tc0 + 512], pb[0:64, :], ("ps%d" % (1 + which),), (nm,))
                cp("dve", dstT[64:128, tc0:tc0 + 512], pb[64:128, :], ("ps%d" % (1 + which),), (nm,))

    if stop <= 1:
        P.stopped = True
    for i in range(2):
        srcT = kcT2 if i == 0 else vcT2
        nm = "kcT2" if i == 0 else "vcT2"
        w1v_ = v3(cw1b[i][:], 16)
        for l in range(16):
            mm(PS[0][:, 0:1], w1v_[:, l, :], cpeb[i][:, l:l + 1], l == 0, l == 15,
               ("cw1b%d" % i, "cpeb%d" % i), ("ps0",))
        cp("dve", cconst[:, i:i + 1], PS[0][:, 0:1], ("ps0",), ("cconst",))
        for l in range(16):
            mm(PS[1][:, 0:255], w1v_[:, l, :], srcT[:, 16 + l:16 + l + 16 * 255:16], l == 0, l == 15,
               ("cw1b%d" % i, nm), ("ps1",))
        act(cx[:, 0:255], PS[1][:, 0:255], AF.Identity, ("ps1", "cconst"), ("cx",), bias=cconst[:, i:i + 1])
        tt("dve", ct1[:, 0:255], cx[:, 0:255], cx[:, 0:255], ALU.mult, ("cx",), ("ct1",))
        ts("dve", ct1[:, 0:255], ct1[:, 0:255], GC2, 1.0, ALU.mult, ALU.add, ("ct1",), ("ct1",))
        tt("dve", ct1[:, 0:255], ct1[:, 0:255], cx[:, 0:255], ALU.mult, ("ct1", "cx"), ("ct1",))
        act(ct2[:, 0:255], ct1[:, 0:255], AF.Sigmoid, ("ct1",), ("ct2",), scale=2.0 * GC1)
        tt("dve", chid[i][:, 0:255], cx[:, 0:255], ct2[:, 0:255], ALU.mult, ("cx", "ct2"), ("chid%d" % i,))
    mm(PS[2][0:64, 0:255], cw2b[0][:], chid[0][:, 0:255], True, True, ("cw2b0", "chid0"), ("ps2",))
    cp("act", kcbT[:, 0:255], PS[2][0:64, 0:255], ("ps2", "kcbT"), ("kcbT",))
    VAv = v3(VAUG[:], 2)
    mm(PS[3][:, 0:64], chid[1][:, 0:128], cw2b[1][:], True, True, ("cw2b1", "chid1"), ("ps3",))
    cp("act", VAv[:, 0, 0:64], PS[3][:, 0:64], ("ps3", "VAUGv"), ("VAUGv",))
    mm(PS[3][0:127, 64:128], chid[1][:, 128:255], cw2b[1][:], True, True, ("cw2b1", "chid1"), ("ps3",))
    cp("act", VAv[0:127, 1, 0:64], PS[3][0:127, 64:128], ("ps3", "VAUGv"), ("VAUGv",))

    if stop <= 2:
        P.stopped = True
    P.barrier(["pe", "act", "dve", "pool", "sp"])
    ms("dve", negp[:, 0:64], 0.0, ("negp0",))
    for i in range(2):
        ms("dve", St[i][:], 0.0, ("St%d" % i,))
        ms("dve", Sbf[i][0][:], 0.0, ("Sbf%d_0" % i,))
    VSv = v3(VS[:], NB)
    VWv = v3(VW[:], NB)
    GSv = v3(GS[:], NB)
    Wtv = v3(Wt[:], 4)
    psb5 = PS[4][:, 320:512].bitcast(BF16)
    psb6 = PS[6][:, 384:512].bitcast(BF16)
    psb7 = PS[7][:, 128:160].bitcast(BF16)

    def fm_proj(col0, m, pbank, tc0, rkeys):
        for kc in range(8):
            mm(PS[pbank][0:m, :], W1v[:, kc, col0:col0 + m], hTv[:, kc, tc0:tc0 + 512], kc == 0, kc == 7,
               ("W1b",) + rkeys, ("ps%d" % pbank,))

    sidx = [0, 0]

    def emit_prep(ch):
        tc0 = ch * 512
        par = ch % 2
        hk = ("hT%d" % ch,)
        qnk = "QN%d" % par
        QNv = QN[par][:].rearrange("p (b h q) -> p b h q", b=4, h=4)
        VBv = v3(VB[par][:], 4)
        PB = PS[0]

        def fmp(col0, m):
            for kc in range(8):
                mm(PB[0:m, :], W1v[:, kc, col0:col0 + m], hTv[:, kc, tc0:tc0 + 512], kc == 0, kc == 7,
                   ("W1b",) + hk, ("ps0",))
        for h in range(4):
            fmp(h * 64, 64)
            act(QNv[0:64, :, h, :], PB[0:64, :].rearrange("p (b q) -> p b q", b=4), AF.Identity,
                ("ps0",), (qnk + "q",), scale=0.125)
        fmp(512, 64)
        cp("act", KE[0:64, tc0:tc0 + 512], PB[0:64, :], ("ps0",), ("KEk%d" % ch,))
        fmp(576, 64)
        cp("dve", KwT[:, tc0:tc0 + 512], PB[0:64, :], ("ps0",), ("KwT%d" % ch,))
        gqv = v3(gq[:], 2)
        gkv = v3(gk[:], 2)
        for h in range(2):
            fmp(640 + h * 64, 64)
            act(gqv[:, h, :], PB[0:64, :], AF.Identity, ("ps0",), ("gq",), scale=0.125)
            fmp(768 + h * 64, 64)
            cp("dve", gkv[:, h, :], PB[0:64, :], ("ps0",), ("gk",))
        fmp(896, 16)
        cp("act", gab[:], PB[0:16, :], ("ps0",), ("gab",))
        rTv = v3(rTs[par][:], 2)
        for h in range(2):
            fmp(912 + h * 128, 128)
            act(rTv[:, h, :], PB[:, :], AF.Silu, ("ps0",), ("rTs%d" % par,))
        for tl in range(4):
            n = ch * 4 + tl
            for kc in range(8):
                mm(PB[:, 0:W2C], hTv[:, kc, n * 128:(n + 1) * 128], W2v[:, kc, :], kc == 0, kc == 7,
                   ("W2b",) + hk, ("ps0",))
            cp("dve", VSv[:, n, 0:64], PB[:, 0:64], ("ps0",), ("VS%d" % ch,))
            cp("dve", VWv[:, n, 0:64], PB[:, 64:128], ("ps0",), ("VW%d" % ch,))
            act(GSv[:, n, :], PB[:, 128:140], AF.Sigmoid, ("ps0",), ("GS%d" % ch,))
            cp("act", VBv[:, tl, :], PB[:, 140:396], ("ps0",), ("VB%d" % par,))
        ge1v = v3(ge1[:], 2)
        gbcv = v3(gbc[:], 2)
        gAqv = v3(gbc[:], 2)
        kinv = v3(kin[par][:], 2)
        kdTv = v3(kdT[par][:], 2)
        gdecv = v3(gdec[par][:], 2)
        for h in range(2):
            mm(PB[0:64, :], walb[:, h * 64:(h + 1) * 64], gab[:], True, True, ("walb", "gab"), ("ps0",))
            act(ge1v[:, h, :], PB[0:64, :], AF.Exp, ("ps0", "nbal"), ("ge1",), bias=nbal[:, h:h + 1], scale=-1.0)
            act(ge1v[:, h, :], ge1v[:, h, :], AF.Ln, ("ge1",), ("ge1",), bias=1.0)
            A("dve", lambda e, o=gbcv[:, h, :], d1=ge1v[:, h, :]: e.tensor_tensor_scan(
                out=o, data0=scanm[:], data1=d1, initial=0.0, op0=ALU.mult, op1=ALU.add),
              ("ge1", "scanm"), ("gbc",))
        act(ge1[:], gbc[:], AF.Exp, ("gbc",), ("ge1",), scale=1.0 / 16.0)
        act(gbc[:], gbc[:], AF.Exp, ("gbc",), ("gbc",), scale=-1.0 / 16.0)
        tt("dve", qin[par][:], gq[:], gbc[:], ALU.mult, ("gq", "gbc"), ("qin%d" % par,))
        tt("dve", kin[par][:], gk[:], ge1[:], ALU.mult, ("gk", "ge1"), ("kin%d" % par,))
        for h in range(2):
            cp("dve", gdecv[:, h, :], gAqv[:, h, 63:512:64], ("gbc",), ("gdec%d" % par,))
            tt("dve", kdTv[:, h, :].rearrange("p (c t) -> p c t", c=8),
               kinv[:, h, :].rearrange("p (c t) -> p c t", c=8),
               gdecv[:, h, :].unsqueeze(2).to_broadcast([64, 8, 64]), ALU.mult, ("kin%d" % par, "gdec%d" % par),
               ("kdT%d" % par,))

    w3v = w3.rearrange("(kc p) n -> p kc n", p=128)
    cvjobs = []
    for c16 in range(16):
        for hf in range(2):
            cvjobs.append((w3v[:, hf * 4:hf * 4 + 4, c16 * 128:(c16 + 1) * 128], 4,
                           w3b[c16].ap()[:, hf * 512:(hf + 1) * 512], "w3b%d" % c16))
    for wsrc, wdst, wkey, nkc in ((wa_d, wab_s, "wab_s", 4), (wb_d, wbb_s, "wbb_s", 4), (wo_d, wob_s, "wob_s", 8)):
        for kc in range(nkc):
            for hf in range(2):
                cvjobs.append((wsrc[kc * 128:(kc + 1) * 128, hf * 512:(hf + 1) * 512], 1,
                               wdst.ap()[:, kc * D + hf * 512:kc * D + (hf + 1) * 512], wkey))
    cvi = [0]

    def emit_cv(njobs):
        for _ in range(njobs):
            if cvi[0] >= len(cvjobs):
                return
            src, nk, dst, dkey = cvjobs[cvi[0]]
            i = cvi[0] % 2
            cvi[0] += 1
            stv = v3(cvf[i][:], nk) if nk > 1 else cvf[i][:]
            dma("sp", stv, src, (), ("cvf%d" % i,), "cvi%d" % i)
            cp("pool", cvb[i][:], cvf[i][:], ("cvf%d" % i,), ("cvb%d" % i,))
            dma("sp", dst, cvb[i][:], ("cvb%d" % i,), (dkey,), "cvo%d" % i)

    emit_prep(0)
    for ch in range(OPTS["nch"]):
        tc0 = ch * 512
        emit_cv(8)
        par = ch % 2
        QNc = QN[par]
        qnk = "QN%d" % par
        QNv = QNc[:].rearrange("p (b h q) -> p b h q", b=4, h=4)
        YTc = YT[par]
        ytk = "YT%d" % par
        YTv = v3(YTc[:], 4)
        VBv = v3(VB[par][:], 4)
        rTv = v3(rTs[par][:], 2)
        qinv = v3(qin[par][:], 2)
        kinv = v3(kin[par][:], 2)
        kdTv = v3(kdT[par][:], 2)
        gdecv = v3(gdec[par][:], 2)
        vbk = "VB%d" % par
        pq = [[], [], [], []]
        if ch + 1 < OPTS["nch"]:
            begin("P")
            emit_prep(ch + 1)
            end()
            pl = streams.pop("P")
            q4 = (len(pl) + 3) // 4
            pq = [pl[i * q4:(i + 1) * q4] for i in range(4)]
        def emit_A1(n, tl):
            for h in range(4):
                mm(PS[6][:, 0:256], QNv[0:64, tl, h, :], kcbT[:], True, True, (qnk + "q", "kcbT"), ("ps6",))
                o0 = 248 - 8 * n
                tt("dve", scc[:], PS[6][:, 0:256], Wtv[:, h, o0:o0 + 256], ALU.add, ("ps6", "Wt"), ("scc",))
                ms("dve", ssum[:, h:h + 1], 0.0, ("ssum",))
                act(ecb[:], scc[:], AF.Exp, ("scc", "ssum"), ("ecb", "ssum"), accum=ssum[:, h:h + 1])
                for cc in range(2):
                    tr(psb6[:, cc * 128:(cc + 1) * 128], ecb[:, cc * 128:(cc + 1) * 128], identb[:],
                       ("ecb", "identb"), ("ps6",))
                cp("act", eT[:], psb6[:, :], ("ps6",), ("eT",))
                for cc in range(2):
                    mm(PS[6][:, 256:384], eT[:, cc * 128:(cc + 1) * 128], VAv[:, cc, :], cc == 0, cc == 1,
                       ("eT", "VAUGv", "VAUGo"), ("ps6",))
                ts("dve", rrC[:, h:h + 1], ssum[:, h:h + 1], 1e-30, None, ALU.max, None, ("ssum",), ("rrC",))
                recip(rrC[:, h:h + 1], rrC[:, h:h + 1], ("rrC",), ("rrC",))
                tt("dve", coefC[:, h:h + 1], rrC[:, h:h + 1], GSv[:, n, h:h + 1], ALU.mult, ("rrC", "GS%d" % ch), ("coefC",))
                ts("dve", yc[n % 2][:, h * 64:(h + 1) * 64], PS[6][:, 256:320], coefC[:, h:h + 1], None, ALU.mult, None,
                   ("ps6", "coefC"), ("yc%d" % (n % 2),))
                if h == 0:
                    ts("dve", imp[:], PS[6][:, 320:384], rrC[:, h:h + 1], None, ALU.mult, None, ("ps6", "rrC"), ("imp",))
                else:
                    stt("dve", imp[:], PS[6][:, 320:384], rrC[:, h:h + 1], imp[:], ALU.mult, ALU.add,
                        ("ps6", "rrC", "imp"), ("imp",))
            k0 = 64 - 2 * n
            tt("dve", impm[:], imp[:], KEEP[:, k0:k0 + 64], ALU.mult, ("imp", "KEEP"), ("impm",))
            tt("dve", impm[:], impm[:], ADDT[:, k0:k0 + 64], ALU.add, ("impm", "ADDT"), ("impm",))
            ms("dve", impm[:, 0:1], 1e30, ("impm",))
            A("dve", lambda e: e.max(out=m8[:, 0:8], in_=impm[:]), ("impm",), ("m8",))
            A("dve", lambda e: e.match_replace(out=imp2[:], in_to_replace=m8[:, 0:8], in_values=impm[:],
                                              imm_value=-3e38), ("impm", "m8"), ("imp2",))
            A("dve", lambda e: e.max(out=m8[:, 8:16], in_=imp2[:]), ("imp2",), ("m8",))
            ts("dve", negp[:, 64:128], impm[:], m8[:, 15:16], NEG, ALU.is_lt, ALU.mult, ("impm", "m8"), ("negp",))
            tr(psb6[:, 0:128], negp[:], identb[:], ("negp", "negp0", "identb"), ("ps6",))
            for h in range(4):
                cp("act" if h % 2 else "dve", QNv[64:128, tl, h, :], psb6[64:128, 0:128], ("ps6",), (qnk + "n%d" % tl,))
        for tl in range(4):
            n = ch * 4 + tl
            if tl == 0:
                begin("A1")
                emit_A1(n, tl)
                end()
                merge(["A1"])
            begin("A")
            for br in range(2):
                if br == 1:
                    end()
                    begin("B")
                kbs = list(range(0, n + 1)) if br == 0 else list(range(max(0, n - 4), n + 1))
                accb = PS[4] if br == 0 else PS[5]
                acck = "ps4" if br == 0 else "ps5"
                accv = accb[:, 0:260].rearrange("p (h c) -> p h c", h=4)
                Vv = VSv if br == 0 else VWv
                vk0 = "VS" if br == 0 else "VW"
                rrX = rr if br == 0 else rrB
                coefX = coef if br == 0 else coefB
                rrk = "rr" if br == 0 else "rrB"
                cfk = "coef" if br == 0 else "coefB"
                for ki, kb in enumerate(kbs):
                    sb_ = (2 + (sidx[br] % 2)) if br == 0 else 1
                    pi_ = (0 if br == 0 else 2) + (sidx[br] % 2)
                    pt = PT[pi_]
                    ptk = "PT%d" % pi_
                    sidx[br] += 1
                    extra = []
                    if kb == n:
                        extra.append((T0, "T0"))
                    if kb == n - 1:
                        extra.append((T1, "T1"))
                    if br == 1 and kb == n - 4:
                        extra.append((T4, "T4"))
                    if br == 0:
                        mm(PS[sb_][:, :], KE[:, kb * 128:(kb + 1) * 128], QNc[:, tl * 512:(tl + 1) * 512],
                           True, not extra, ("KEk%d" % (kb // 4), "KEexp", qnk + "q", qnk + "n%d" % tl), ("ps%d" % sb_,))
                    else:
                        mm(PS[sb_][:, :], KwT[:, kb * 128:(kb + 1) * 128], QNc[0:64, tl * 512:(tl + 1) * 512],
                           True, not extra, ("KwT%d" % (kb // 4), qnk + "q"), ("ps%d" % sb_,))
                    for xi, (tb, tk) in enumerate(extra):
                        mm(PS[sb_][:, :], identb[:], tb[:], False, xi == len(extra) - 1, ("identb", tk), ("ps%d" % sb_,))
                    act(pt[:], PS[sb_][:, :], AF.Exp, ("ps%d" % sb_,), (ptk,))
                    for h in range(4):
                        mm(accv[:, h, :], pt[:, h * 128:(h + 1) * 128], Vv[:, kb, :], ki == 0 and h == 0, ki == len(kbs) - 1,
                           (ptk, vk0 + "%d" % (kb // 4), vk0 + "one"), (acck,), sgc=True)
                gofs = 4 if br == 0 else 8
                cp("dve", rrX[:, 0:4].unsqueeze(2), accv[:, :, 64:65], (acck,), (rrk,))
                recip(rrX[:, 0:4], rrX[:, 0:4], (rrk,), (rrk,))
                tt("dve", coefX[:, 0:4], rrX[:, 0:4], GSv[:, n, gofs:gofs + 4], ALU.mult, (rrk, "GS%d" % ch), (cfk,))
                if debug and br == 0:
                    dma("sp", dbg_br[0, n * 128:(n + 1) * 128, :], yc[n % 2][:], ("yc%d" % (n % 2),), ("dbgbr",), "dbgb")
                for h in range(4):
                    if debug:
                        ts("dve", ybr[br][:, h * 64:(h + 1) * 64], accv[:, h, 0:64], coefX[:, h:h + 1], None, ALU.mult, None,
                           (acck, cfk), ("ybr%d" % br,))
                    if br == 0:
                        ts("dve", ya[:, h * 64:(h + 1) * 64], accv[:, h, 0:64], coefX[:, h:h + 1], None, ALU.mult, None,
                           (acck, cfk), ("ya",))
                    else:
                        ts("dve", yw[:, h * 64:(h + 1) * 64], accv[:, h, 0:64], coefX[:, h:h + 1], None, ALU.mult, None,
                           (acck, cfk), ("yw",))
                if debug:
                    dma("sp", dbg_br[1 + br, n * 128:(n + 1) * 128, :], ybr[br][:], ("ybr%d" % br,), ("dbgbr",), "dbgb")
            end()
            begin("C")
            for h in range(2):
                ts0 = tl * 128
                mm(PS[7][:, 0:128], kinv[:, h, ts0:ts0 + 128], qinv[:, h, ts0:ts0 + 128], True, True,
                   ("kin%d" % par, "qin%d" % par), ("ps7",))
                atb = ATb[h]
                tt("dve", atb[:], PS[7][:, 0:128], MTs[:], ALU.mult, ("ps7", "MTs"), ("ATb%d" % h,))
                tr(psb7[:, 0:64], kdTv[:, h, ts0:ts0 + 128], identb[0:64, 0:64], ("kdT%d" % par, "identb"), ("ps7",))
                cp("act", kdtok[h][:], psb7[:, 0:64], ("ps7",), ("kdtok%d" % h,))
                mm(PS[7][0:64, 256:384], kdtok[h][0:64, :], VBv[0:64, tl, h * 128:(h + 1) * 128], True, True,
                   ("kdtok%d" % h, vbk), ("ps7",))
                stt("dve", St[h][:], St[h][:], gdecv[:, h, tl * 2:tl * 2 + 1], PS[7][0:64, 256:384], ALU.mult, ALU.add,
                    ("St%d" % h, "gdec%d" % par, "ps7"), ("St%d" % h,))
                cp("act", Sbf[h][1][:], St[h][:], ("St%d" % h,), ("Sbf%d_1" % h,))
                mm(PS[7][:, 128:192], Sbf[h][0][:], qinv[:, h, ts0:ts0 + 64], True, False, ("Sbf%d_0" % h, "qin%d" % par), ("ps7",), sgc=True)
                mm(PS[7][:, 192:256], Sbf[h][1][:], qinv[:, h, ts0 + 64:ts0 + 128], False, False, ("Sbf%d_1" % h, "qin%d" % par), ("ps7",), sgc=True)
                mm(PS[7][:, 128:256], VBv[:, tl, h * 128:(h + 1) * 128], atb[:], False, True, (vbk, "ATb%d" % h), ("ps7",), sgc=True)
                mm(PS[7][0:64, 384:512], kdtok[h][64:128, :], VBv[64:128, tl, h * 128:(h + 1) * 128], True, True,
                   ("kdtok%d" % h, vbk), ("ps7",))
                stt("dve", St[h][:], St[h][:], gdecv[:, h, tl * 2 + 1:tl * 2 + 2], PS[7][0:64, 384:512], ALU.mult, ALU.add,
                    ("St%d" % h, "gdec%d" % par, "ps7"), ("St%d" % h,))
                cp("act", Sbf[h][0][:], St[h][:], ("St%d" % h,), ("Sbf%d_0" % h,))
                act(gsq[:], PS[7][:, 128:256], AF.Square, ("ps7",), ("gsq",))
                mm(PS[7][:, 0:128], onesb[:], gsq[:], True, True, ("onesb", "gsq"), ("ps7",))
                act(gl1[:], PS[7][:, 0:128], AF.Ln, ("ps7",), ("gl1",), bias=EPS, scale=1.0 / 128.0)
                act(grs[:], gl1[:], AF.Exp, ("gl1",), ("grs",), scale=-0.5)
                tt("dve", gyb[:], PS[7][:, 128:256], grs[:], ALU.mult, ("ps7", "grs"), ("gyb",))
                stt("dve", YTv[:, 2 + h, ts0:ts0 + 128], gyb[:], ngs[:, h:h + 1], rTv[:, h, ts0:ts0 + 128],
                    ALU.mult, ALU.mult, ("gyb", "ngs", "rTs%d" % par), (ytk,))
            end()
            if tl < 3:
                begin("A1")
                emit_A1(n + 1, tl + 1)
                end()
                streams["PQ"] = pq[tl]
                merge(["A", "B", "C", "A1", "PQ"])
            else:
                streams["PQ"] = pq[tl]
                merge(["A", "B", "C", "PQ"])
            tt("dve", ya[:], ya[:], yw[:], ALU.add, ("ya", "yw"), ("ya",))
            tt("dve", ya[:], ya[:], yc[n % 2][:], ALU.add, ("ya", "yc%d" % (n % 2)), ("ya",))
            cp("act", yab[:], ya[:], ("ya",), ("yab",))
            for cc in range(2):
                tr(psb5[:, cc * 128:(cc + 1) * 128], yab[:, cc * 128:(cc + 1) * 128], identb[:], ("yab", "identb"), ("ps4",))
            cp("act", YTv[:, 0:2, tl * 128:(tl + 1) * 128], psb5[:, 0:256].rearrange("p (c q) -> p c q", c=2),
               ("ps4",), (ytk,))
        dma("act", ysrc[ch].ap().rearrange("(c p) t -> p c t", p=128), YTv, (ytk,), ("ysrc%d" % ch,), "ys%d" % ch)
        P.add("pool", lambda e, ch=ch: e.collective_compute("AllGather", ALU.bypass,
                                                          replica_groups=[[0, 1], [2, 3], [4, 5], [6, 7]],
                                                          ins=[ysrc[ch].ap()], outs=[ydst[ch].ap()]),
              ("ysrc%d" % ch,), ("ydst%d" % ch,), dma="cc%d" % ch, inc=1)

    if stop <= 3:
        P.stopped = True
    if debug:
        for i in range(8):
            dma("sp", dbg_y[:, i * 512:(i + 1) * 512], ydst[i].ap(), ("ydst%d" % i,), ("dbgy",), "dbg")

    if stop <= 4:
        P.stopped = True
    HXv = v3(HX[:], 8)
    P.barrier(["pe", "act", "dve", "pool", "sp"])
    for kc in range(8):
        eng = "dve"
        ts(eng, HXv[:, kc, :], hTv[:, kc, 0:HALF], s01[:, 0:1], None, ALU.mult, None,
           tuple("hT%d" % i for i in range(8)) + ("s01",), ("HX",))
        stt(eng, HXv[:, kc, :], hTv[:, kc, HALF:S], s01[:, 1:2], HXv[:, kc, :], ALU.mult, ALU.add,
            tuple("hT%d" % i for i in range(8)) + ("s01", "HX"), ("HX",))
    P.barrier(["pe", "act", "dve", "pool", "sp"])

    ACC = BIG
    ACCv = v3(ACC[:], 8)
    wcnt2 = [0]

    def load_w2(dstv, src_view, nk, ncols, dstkey, q="sp"):
        c0 = 0
        while c0 < ncols:
            cw = min(2048 // nk, ncols - c0)
            i = wcnt2[0] % 2
            wcnt2[0] += 1
            st = v3(wst2[i][:, 0:nk * cw], nk)
            dma(q, st, src_view[:, :, c0:c0 + cw], (), ("wst2_%d" % i,), "v%d" % i)
            cp("act" if (wcnt2[0] % 2) else "pool", dstv[:, :, c0:c0 + cw], st, ("wst2_%d" % i,), (dstkey,))
            c0 += cw

    Wav = v3(Wab[:], 4)
    Wbv = v3(Wbb[:], 4)
    Wov = v3(Wob[:], 8)
    emit_cv(len(cvjobs))
    dma("sp", Wab[:], wab_s.ap(), ("wab_s",), ("Wab",), "wl")
    dma("sp", Wbb[:], wbb_s.ap(), ("wbb_s",), ("Wbb",), "wl")
    dma("sp", Wob[:], wob_s.ap(), ("wob_s",), ("Wob",), "wl")
    dma("sp", v3(WRf[:], 8), wr_d.rearrange("(kc p) n -> p kc n", p=128), (), ("WRf0",), "c6")
    WRv = v3(WRf[:], 8)
    for kc in range(8):
        ts("dve", WRv[:, kc, :], WRv[:, kc, :], gv[:, 8 + kc:9 + kc], None, ALU.mult, None, ("WRf0", "gv"), ("WRf",))
    dma("sp", brs[:], br_d, (), ("brs",), "c6")
    ms("dve", onesf[:], 1.0, ("onesf",))

    ydv = [y_.ap().rearrange("(c p) t -> p c t", p=128) for y_ in ydst]
    w3v = w3.rearrange("(kc p) n -> p kc n", p=128)
    xhd = xTh.rearrange("(kc p) t -> p kc t", p=128)
    YMv = v3(YM[:], 8)
    MGv = v3(MG[:], 8)
    sq2v = v3(sq2[:], 8)
    FA = (0, 1, 4, 5)
    FB = (2, 3, 6, 7)
    w3c = [0]
    def emit_routing(tcx, t0):
        for tl in range(4):
            tt0 = t0 + tl * 128
            for kc in range(8):
                mm(PS[7][:, 0:36], ACCv[:, kc, tt0:tt0 + 128], WRv[:, kc, :], kc == 0, kc == 7,
                   ("ACC%d" % tcx, "WRf"), ("ps7",))
            mm(PS[7][:, 64:65], rstd2[tcx % 2][0:1, tl * 128:(tl + 1) * 128], identf[0:1, 0:1], True, True,
               ("rstd2_%d" % (tcx % 2), "identf"), ("ps7",))
            cp("dve", rt[:, 0:1], PS[7][:, 64:65], ("ps7",), ("rt0",))
            stt("dve", Lg[:], PS[7][:, 0:36], rt[:, 0:1], brs[:], ALU.mult, ALU.add, ("ps7", "rt0", "brs"), ("Lg",))
            A("dve", lambda e: e.reduce_max(out=rt[:, 1:2], in_=Lg[:, 0:4], axis=AX.X), ("Lg",), ("rt1",))
            ts("dve", rt[:, 2:3], rt[:, 1:2], -1.0, None, ALU.mult, None, ("rt1",), ("rt2",))
            ms("dve", rt[:, 3:4], 0.0, ("rt3",))
            act(rt[:, 8:12], Lg[:, 0:4], AF.Exp, ("Lg", "rt2", "rt3"), ("rt8", "rt3"), bias=rt[:, 2:3], accum=rt[:, 3:4])
            recip(rt[:, 4:5], rt[:, 3:4], ("rt3",), ("rt4",))
            ts("dve", rt[:, 12:16], Lg[:, 0:4], rt[:, 1:2], None, ALU.is_ge, None, ("Lg", "rt1"), ("rt12",))
            for gi in range(4):
                if gi == 0:
                    ts("dve", esel[:], Lg[:, 4:12], rt[:, 12:13], None, ALU.mult, None, ("Lg", "rt12"), ("esel",))
                else:
                    stt("dve", esel[:], Lg[:, 4 + 8 * gi:12 + 8 * gi], rt[:, 12 + gi:13 + gi], esel[:], ALU.mult, ALU.add,
                        ("Lg", "rt12", "esel"), ("esel",))
            A("dve", lambda e: e.max(out=rt[:, 16:24], in_=esel[:]), ("esel",), ("rt16",))
            tt("dve", rt[:, 24:25], rt[:, 17:18], rt[:, 16:17], ALU.subtract, ("rt16",), ("rt24",))
            act(rt[:, 25:26], rt[:, 24:25], AF.Exp, ("rt24",), ("rt25",))
            ts("dve", rt[:, 25:26], rt[:, 25:26], 1.0, None, ALU.add, None, ("rt25",), ("rt25",))
            recip(rt[:, 26:27], rt[:, 25:26], ("rt25",), ("rt26",))
            ts("dve", rt[:, 27:28], rt[:, 26:27], -1.0, 1.0, ALU.mult, ALU.add, ("rt26",), ("rt27",))
            tt("dve", rt[:, 26:27], rt[:, 26:27], rt[:, 4:5], ALU.mult, ("rt26", "rt4"), ("rt26",))
            tt("dve", rt[:, 27:28], rt[:, 27:28], rt[:, 4:5], ALU.mult, ("rt27", "rt4"), ("rt27",))
            ts("dve", rt[:, 32:40], esel[:], rt[:, 16:17], rt[:, 26:27], ALU.is_equal, ALU.mult, ("esel", "rt16", "rt26"), ("rt32",))
            ts("dve", rt[:, 40:48], esel[:], rt[:, 17:18], rt[:, 27:28], ALU.is_equal, ALU.mult, ("esel", "rt16", "rt27"), ("rt40",))
            tt("dve", rt[:, 32:40], rt[:, 32:40], rt[:, 40:48], ALU.add, ("rt32", "rt40"), ("rt32",))
            for gi in range(4):
                ts("dve", comb[:, gi * 8:(gi + 1) * 8], rt[:, 32:40], rt[:, 12 + gi:13 + gi], None, ALU.mult, None,
                   ("rt32", "rt12"), ("comb",))
            tr(PS[7][0:32, 128:256], comb[:], identf[:], ("comb", "identf"), ("ps7",))
            cp("act", COMBT[:, tt0:tt0 + 128], PS[7][0:32, 128:256], ("ps7",), ("COMBT%d" % tcx,))

    for tcx in range(4):
        t0 = tcx * 512
        begin("M")
        dma("sp", v3(YH[:], 8), ydv[tcx], ("ydst%d" % tcx,), ("YH",), "yh")
        ts("dve", YM[:], YH[:], s01[:, 0:1], None, ALU.mult, None, ("YH", "s01"), ("YM",))
        dma("sp", v3(YH[:], 8), ydv[4 + tcx], ("ydst%d" % (4 + tcx),), ("YH",), "yh")
        stt("dve", YM[:], YH[:], s01[:, 1:2], YM[:], ALU.mult, ALU.add, ("YH", "s01", "YM"), ("YM",))
        for cc in range(8):
            sgs = []
            for ab in range(2):
                i = w3c[0] % 4
                w3c[0] += 1
                dma("sp", W3p[i][:], w3b[ab * 8 + cc].ap(), ("w3b%d" % (ab * 8 + cc),), ("W3p%d" % i,), "w3p%d" % i)
                W3pv = v3(W3p[i][:], 8)
                for kc in range(8):
                    mm(PS[ab][:, :], W3pv[:, kc, :], HXv[:, kc, t0:t0 + 512], kc == 0, kc == 7,
                       ("W3p%d" % i, "HX"), ("ps%d" % ab,))
                act(SGp[ab][:], PS[ab][:, :], AF.Sigmoid, ("ps%d" % ab,), ("SGp%d" % ab,))
            for j, fc in enumerate(FA):
                mm(PS[2][:, :], Wav[:, j, cc * 128:(cc + 1) * 128], YMv[:, fc, :], j == 0, j == 3, ("Wab", "YM"), ("ps2",))
            for j, fc in enumerate(FB):
                mm(PS[3][:, :], Wbv[:, j, cc * 128:(cc + 1) * 128], YMv[:, fc, :], j == 0, j == 3, ("Wbb", "YM"), ("ps3",))
            tt("dve", m1[:], PS[2][:, :], SGp[0][:], ALU.mult, ("ps2", "SGp0"), ("m1",))
            tt("dve", m2[:], PS[3][:, :], SGp[1][:], ALU.mult, ("ps3", "SGp1"), ("m2",))
            tt("dve", MGv[:, cc, :], m1[:], m2[:], ALU.add, ("m1", "m2"), ("MG",))
        for co in range(8):
            pbk = 4 + co % 2
            xb = xh[co % 2]
            dma("sp", xb[:], xhd[:, co, t0:t0 + 512], (), ("xh%d" % (co % 2),), "xh%d" % (co % 2))
            for cc in range(8):
                mm(PS[pbk][:, :], Wov[:, cc, co * 128:(co + 1) * 128], MGv[:, cc, :], cc == 0, cc == 7,
                   ("Wob", "MG"), ("ps%d" % pbk,))
            tt("dve", ACCv[:, co, t0:t0 + 512], PS[pbk][:, :], xb[:], ALU.add, ("ps%d" % pbk, "xh%d" % (co % 2)), ("ACC%d" % tcx,))
        for kc in range(8):
            act(sq2v[:, kc, :], ACCv[:, kc, t0:t0 + 512], AF.Square, ("ACC%d" % tcx,), ("YH",))
        for kc in range(8):
            mm(PS[6][:, :], onesb[:], sq2v[:, kc, :], kc == 0, kc == 7, ("onesb", "YH"), ("ps6",))
        act(rstd2[tcx % 2][:], PS[6][:, :], AF.Sqrt, ("ps6",), ("rstd2_%d" % (tcx % 2),), bias=EPS, scale=1.0 / D)
        recip(rstd2[tcx % 2][:], rstd2[tcx % 2][:], ("rstd2_%d" % (tcx % 2),), ("rstd2_%d" % (tcx % 2),))
        for kc in range(8):
            stt("dve", HXv[:, kc, t0:t0 + 512], ACCv[:, kc, t0:t0 + 512], gv[:, 8 + kc:9 + kc], rstd2[tcx % 2][:],
                ALU.mult, ALU.mult, ("ACC%d" % tcx, "rstd2_%d" % (tcx % 2), "gv"), ("HX",))
        end()
        if tcx > 0:
            begin("R")
            emit_routing(tcx - 1, (tcx - 1) * 512)
            end()
            merge(["M", "R"])
        else:
            merge(["M"])
    emit_routing(3, 3 * 512)
    if debug:
        dma("sp", dbg_x1.rearrange("(kc p) t -> p kc t", p=128), ACCv, tuple("ACC%d" % i for i in range(4)), ("dbgx",), "dbg")

    if stop <= 5:
        P.stopped = True
    P.barrier(["pe", "act", "dve", "pool", "sp"])
    dma("sp", gfs[:], gfin, (), ("gfs",), "c7")
    NE = OPTS["nexp"]

    def moe_load(e_):
        i = e_ % 4
        load_w2(v3(WGb[i][:], 8), wg_d[e_].rearrange("(kc p) n -> p kc n", p=128), 8, 256, "WGb%d" % i, q="sp")
        load_w2(v3(WUb[i][:], 8), wu_d[e_].rearrange("(kc p) n -> p kc n", p=128), 8, 256, "WUb%d" % i, q="sp")
        load_w2(v3(WDb[i][:], 2), wd_d[e_].rearrange("(kc p) n -> p kc n", p=128), 2, D, "WDb%d" % i, q="sp")

    def moe_prep(st):
        p, tcx = divmod(st, 4)
        t0 = tcx * 512
        for x in range(2):
            e_ = 2 * p + x
            j = (st % 2) * 2 + x
            ts("dve", cmask[j][:], COMBT[:, t0:t0 + 512], identf[0:32, e_:e_ + 1], None, ALU.mult, None,
               ("COMBT%d" % tcx, "identf"), ("cmask%d" % j,))

    def moe_gu(st):
        p, tcx = divmod(st, 4)
        t0 = tcx * 512
        for x in range(2):
            j = (st % 2) * 2 + x
            mm(PS[6 + x][:, :], onesb[0:32, :], cmask[j][:], True, True, ("onesb", "cmask%d" % j), ("ps%d" % (6 + x),))
            cp("act", CB[j][:], PS[6 + x][:, :], ("ps%d" % (6 + x),), ("CB%d" % j,))
        for x in range(2):
            e_ = 2 * p + x
            i = e_ % 4
            j = (st % 2) * 2 + x
            WGv = v3(WGb[i][:], 8)
            WUv = v3(WUb[i][:], 8)
            hdv = v3(hid[j][:], 2)
            for fc in range(2):
                for kc in range(8):
                    mm(PS[fc][:, :], WGv[:, kc, fc * 128:(fc + 1) * 128], HXv[:, kc, t0:t0 + 512], kc == 0, kc == 7,
                       ("WGb%d" % i, "HX"), ("ps%d" % fc,))
                for kc in range(8):
                    mm(PS[2 + fc][:, :], WUv[:, kc, fc * 128:(fc + 1) * 128], HXv[:, kc, t0:t0 + 512], kc == 0, kc == 7,
                       ("WUb%d" % i, "HX"), ("ps%d" % (2 + fc),))
                act(sil[fc][:], PS[fc][:, :], AF.Silu, ("ps%d" % fc,), ("sil%d" % fc,))
                tt("dve", t1b[fc][:], sil[fc][:], PS[2 + fc][:, :], ALU.mult, ("sil%d" % fc, "ps%d" % (2 + fc)), ("t1b%d" % fc,))
                tt("dve", hdv[:, fc, :], t1b[fc][:], CB[j][:], ALU.mult, ("t1b%d" % fc, "CB%d" % j), ("hid%d" % j,))

    def moe_down(st):
        p, tcx = divmod(st, 4)
        t0 = tcx * 512
        for co in range(8):
            pbk = 4 + co % 2
            k = 0
            for x in range(2):
                i = (2 * p + x) % 4
                j = (st % 2) * 2 + x
                WDv = v3(WDb[i][:], 2)
                hdv = v3(hid[j][:], 2)
                for fc in range(2):
                    mm(PS[pbk][:, :], WDv[:, fc, co * 128:(co + 1) * 128], hdv[:, fc, :], k == 0, k == 3,
                       ("WDb%d" % i, "hid%d" % j), ("ps%d" % pbk,))
                    k += 1
            tt("dve", ACCv[:, co, t0:t0 + 512], PS[pbk][:, :], ACCv[:, co, t0:t0 + 512], ALU.add,
               ("ps%d" % pbk, "ACC%d" % tcx), ("ACC%d" % tcx,))

    NP = NE // 2
    for e_ in range(min(4, NE)):
        moe_load(e_)
    moe_prep(0)
    for st in range(NP * 4):
        moe_gu(st)
        if st + 1 < NP * 4:
            moe_prep(st + 1)
        if st > 0:
            moe_down(st - 1)
        p, tcx = divmod(st, 4)
        if tcx == 0 and p >= 1 and p + 1 < NP:
            moe_load(2 * (p + 1))
            moe_load(2 * (p + 1) + 1)
    moe_down(NP * 4 - 1)

    if stop <= 6:
        P.stopped = True
    for tl in range(16):
        tcx = tl // 4
        tt0 = tl * 128
        for co in range(8):
            tr(PS[co // 4][:, (co % 4) * 128:(co % 4 + 1) * 128], ACCv[:, co, tt0:tt0 + 128], identf[:],
               ("ACC%d" % tcx, "identf"), ("ps%d" % (co // 4),))
        cp("act", xtm[:, 0:512], PS[0][:, :], ("ps0",), ("xtm",))
        cp("dve", xtm[:, 512:1024], PS[1][:, :], ("ps1",), ("xtm",))
        ms("dve", fss[:, 0:1], 0.0, ("fss",))
        act(junk[:], xtm[:], AF.Square, ("xtm", "fss"), ("hid0", "fss"), accum=fss[:, 0:1])
        act(fss[:, 1:2], fss[:, 0:1], AF.Sqrt, ("fss",), ("fss1",), bias=EPS, scale=1.0 / D)
        recip(fss[:, 2:3], fss[:, 1:2], ("fss1",), ("fss2",))
        stt("dve", otm[:], xtm[:], fss[:, 2:3], gfs[:], ALU.mult, ALU.mult, ("xtm", "fss2", "gfs"), ("otm",))
        dma("sp", out_d[tt0:tt0 + 128, :], otm[:], ("otm",), ("outd",), "out")

    keys = P.finalize()
    P.check()
    semh = {}
    for k in ("pe", "act", "dve", "pool", "sp"):
        semh[k] = es.enter_context(nc.semaphore("s_" + k))
    for k in keys:
        semh["dma:" + k] = es.enter_context(nc.semaphore("d_" + k))
    with nc.Block() as block:
        block.tensor(lambda e: P.emit_engine("pe", e, semh))
        block.scalar(lambda e: P.emit_engine("act", e, semh))
        block.vector(lambda e: P.emit_engine("dve", e, semh))
        block.gpsimd(lambda e: P.emit_engine("pool", e, semh))
        block.sync(lambda e: P.emit_engine("sp", e, semh))
    es.close()
    return nc


_C = None


def make_in_maps(inp):
    global _C
    if _C is None:
        _C = _consts()
    C = _C
    f = lambda a: np.ascontiguousarray(a, dtype=np.float32)
    x = inp["x"]
    w_in = inp["w_in"][0]
    rb = inp["rel_bias"]
    gvec = np.concatenate([inp["g_mix"][0].reshape(8, 128).T, inp["g_ffn"][0].reshape(8, 128).T], axis=1)
    gfin = np.broadcast_to(inp["g_final"][None, :], (128, D))
    wr = np.concatenate([inp["w_router_group"][0], inp["w_router_expert"][0]], axis=1)
    br = np.broadcast_to(np.concatenate([inp["b_router_group"][0], inp["b_router_expert"][0]])[None, :], (128, 36))

    def cw1_layout(w):
        return w.reshape(2, 16, 64, 128).transpose(0, 2, 1, 3).reshape(128, 16 * 128)

    def pe_layout(pe):
        return pe.reshape(2, 16, 64).transpose(0, 2, 1).reshape(128, 16)

    cw1 = np.stack([cw1_layout(inp["nsa_cmp_k_w1"][0]), cw1_layout(inp["nsa_cmp_v_w1"][0])])
    cw2 = np.stack([inp["nsa_cmp_k_w2"][0], inp["nsa_cmp_v_w2"][0]])
    cpe = np.stack([pe_layout(inp["nsa_pe_k"][0]), pe_layout(inp["nsa_pe_v"][0])])
    shared = dict(
        w3=f(w_in[:, 2856:4904]), gvec=f(gvec), gfin=f(gfin), cw1=f(cw1), cw2=f(cw2), cpe=f(cpe),
        ovl=C["ovl"], M0=C["M0"], M4=C["M4"], MW=C["MW"], KEEP=C["KEEP"], ADD=C["ADD"], EXPM=C["EXPM"],
        identb=C["identb"], identf=C["identf"], MT=C["MT"], scanmask=C["scanmask"],
        wa=f(inp["w_branch_a"][0]), wb=f(inp["w_branch_b"][0]), wo=f(inp["w_out"][0]), wr=f(wr), br=f(br),
        SELM=C["SELM"], wg=f(inp["w_exp_gate"][0]), wu=f(inp["w_exp_up"][0]), wd=f(inp["w_exp_down"][0]),
    )
    maps = []
    for core in range(8):
        b, g = core // 2, core % 2
        hs = np.arange(4 * g, 4 * g + 4)
        cols1 = np.concatenate([
            np.arange(256 * g, 256 * g + 256),
            np.tile(np.arange(512 + 64 * g, 512 + 64 * g + 64), 2),
            np.tile(np.arange(640 + 64 * g, 640 + 64 * g + 64), 2),
            np.arange(768 + 64 * g, 768 + 64 * g + 64),
            np.arange(1024 + 64 * g, 1024 + 64 * g + 64),
            np.arange(1304 + 128 * g, 1304 + 128 * g + 128),
            np.arange(1560 + 128 * g, 1560 + 128 * g + 128),
            np.arange(2328, 2344),
            np.arange(2344 + 256 * g, 2344 + 256 * g + 256),
        ])
        gate_cols = np.concatenate([1280 + brn * 8 + hs for brn in range(3)])
        cols2 = np.concatenate([
            np.arange(896 + 64 * g, 896 + 64 * g + 64),
            np.arange(1152 + 64 * g, 1152 + 64 * g + 64),
            gate_cols,
            np.arange(1816 + 256 * g, 1816 + 256 * g + 256),
        ])
        rbh = rb[:, hs]
        t0raw = np.where(C["vis0"][:, None, :], rbh[C["idx0"]].transpose(0, 2, 1), 0.0)
        t1raw = rbh[C["idx1"]].transpose(0, 2, 1)
        wraw = np.where(C["visw"][:, None, :], rbh[C["idxw"]].transpose(0, 2, 1), 0.0)
        sel = np.zeros((128, 2), np.float32)
        sel[:, g] = 1.0
        m = dict(shared)
        m.update(
            xT=f(x[b].T), xTh=f(x[b, g * HALF:(g + 1) * HALF].T), sel01=sel,
            w1=f(w_in[:, cols1]), w2=f(w_in[:, cols2]),
            t0raw=f(t0raw.reshape(128, 512)), t1raw=f(t1raw.reshape(128, 512)), wraw=f(wraw.reshape(128, 4 * 504)),
            b31=f(np.broadcast_to(rbh[31][None, :], (128, 4))),
            wal=f(inp["gla_w_alpha"][0][:, 128 * g:128 * g + 128]),
            nbal=f(-inp["gla_b_alpha"][0][128 * g:128 * g + 128].reshape(2, 64).T),
            ng=f(inp["gla_norm_g"][0][256 * g:256 * g + 256].reshape(2, 128).T),
        )
        maps.append(m)
    return maps


_NC = {}


def kernel(**inputs):
    inp = {k: np.asarray(v) for k, v in inputs.items()}
    if "nc" not in _NC:
        _NC["nc"] = build_nc(False)
    nc = _NC["nc"]
    maps = make_in_maps(inp)
    res = run_bass_kernel_spmd(nc, maps, core_ids=list(range(8)))
    out = np.zeros((4, S, D), np.float32)
    for core in range(8):
        b, g = core // 2, core % 2
        out[b, g * HALF:(g + 1) * HALF] = res.results[core]["out"]
    return out
```

```python
import math
from contextlib import ExitStack
import numpy as np
import ml_dtypes
import concourse.bass as bass
import concourse.mybir as mybir
from concourse.bass_utils import run_bass_kernel_spmd

F32 = mybir.dt.float32
BF16 = mybir.dt.bfloat16
AF = mybir.ActivationFunctionType
ALU = mybir.AluOpType
AX = mybir.AxisListType
BF = ml_dtypes.bfloat16

S = 4096
D = 1024
NB = 32
HALF = 2048
NEG = -30000.0
EPS = 1e-6
N_EXP = 32
W1C = 1168
W2C = 396
GC1 = 0.7978845608028654
GC2 = 0.044715


class Prog:
    def __init__(self):
        self.ops = []
        self.lw = {}
        self.rd = {}

    stopped = False

    def add(self, eng, fn, reads=(), writes=(), dma=None, inc=16):
        if self.stopped:
            return -1
        i = len(self.ops)
        reads = tuple(reads)
        writes = tuple(writes) + tuple(k for k in reads if k.startswith("ps") and k not in writes)
        deps = {}
        for k in reads:
            w = self.lw.get(k)
            if w is not None:
                deps[w] = True
        for k in writes:
            w = self.lw.get(k)
            if w is not None:
                deps.setdefault(w, False)
            for r in self.rd.get(k, ()):
                deps.setdefault(r, False)
        for k in writes:
            self.lw[k] = i
            self.rd[k] = []
        for k in reads:
            if k not in writes:
                self.rd.setdefault(k, []).append(i)
        self.ops.append(dict(eng=eng, fn=fn, deps=deps, dma=dma, need=False, waits=[], inc=inc))
        return i

    def barrier(self, engines):
        if self.stopped:
            return
        last = {}
        lastd = {}
        for i, o in enumerate(self.ops):
            last[o["eng"]] = i
            if o["dma"] is not None:
                lastd[o["dma"]] = i
        deps = {i: True for i in list(last.values()) + list(lastd.values())}
        for e in engines:
            self.ops.append(dict(eng=e, fn=(lambda en: en.nop()), deps=dict(deps), dma=None,
                                 need=False, waits=[], bar=True, inc=16))

    def finalize(self):
        ops = self.ops
        for i, o in enumerate(ops):
            for j, raw in o["deps"].items():
                pj = ops[j]
                if pj["dma"] is None and o["dma"] is None and pj["eng"] == o["eng"] and not o.get("bar"):
                    if o["eng"] == "pe":
                        continue
                pj["need"] = True
                o["waits"].append(j)
        cnt = {}
        dcnt = {}
        for o in ops:
            ws = []
            for j in sorted(o["waits"]):
                pj = ops[j]
                if pj["dma"] is not None:
                    ws.append(("dma:" + pj["dma"], dcnt[pj["dma"]]))
                else:
                    ws.append(pj["sig"])
            o["ws"] = ws
            if o["dma"] is not None:
                dcnt[o["dma"]] = dcnt.get(o["dma"], 0) + o["inc"]
                o["sig"] = ("dma:" + o["dma"], dcnt[o["dma"]])
            elif o["need"]:
                cnt[o["eng"]] = cnt.get(o["eng"], 0) + 1
                o["sig"] = (o["eng"], cnt[o["eng"]])
        self.dcnt = dcnt
        print('SEM counts', cnt, 'n_dma_keys', len(dcnt))
        return sorted(dcnt.keys())

    def check(self):
        ops = self.ops
        q = {}
        for i, o in enumerate(ops):
            q.setdefault(o["eng"], []).append(i)
        pos = {e: 0 for e in q}
        sem = {}
        prog = True
        n_done = 0
        while prog:
            prog = False
            for e, lst in q.items():
                while pos[e] < len(lst):
                    o = ops[lst[pos[e]]]
                    if all(sem.get(s_, 0) >= v_ for s_, v_ in o["ws"]):
                        if o["dma"] is not None:
                            k_ = "dma:" + o["dma"]
                            sem[k_] = sem.get(k_, 0) + o["inc"]
                        elif o["need"]:
                            sem[e] = sem.get(e, 0) + 1
                        pos[e] += 1
                        n_done += 1
                        prog = True
                    else:
                        break
        stuck = {e: (pos[e], len(lst)) for e, lst in q.items() if pos[e] < len(lst)}
        if stuck:
            for e, (p_, n_) in stuck.items():
                o = ops[q[e][p_]]
                print("STUCK", e, p_, n_, [(s_, v_, sem.get(s_, 0)) for s_, v_ in o["ws"] if sem.get(s_, 0) < v_])
        else:
            print("protocol check OK", n_done)
        return not stuck

    def emit_engine(self, ename, e, semh):
        ops = self.ops
        seen = {}
        for o in ops:
            if o["eng"] != ename:
                continue
            for s, v in o["ws"]:
                if seen.get(s, 0) >= v:
                    continue
                seen[s] = v
                e.wait_ge(semh[s], v)
            ins = o["fn"](e)
            if o["dma"] is not None:
                ins.then_inc(semh["dma:" + o["dma"]], o["inc"])
            elif o["need"]:
                ins.then_inc(semh[ename], 1)
        if ename == "sp":
            for k, v in self.dcnt.items():
                if seen.get("dma:" + k, 0) < v:
                    e.wait_ge(semh["dma:" + k], v)


def _bucket(rel):
    n = np.maximum(rel, 0)
    nf = np.maximum(n, 1).astype(np.float32)
    large = 16 + (np.log(nf / np.float32(16)) / np.float32(math.log(8.0)) * np.float32(16)).astype(np.int32)
    return np.where(n < 16, n, np.minimum(large, 31))


def _consts():
    c = {}
    k = np.arange(128)[:, None]
    q = np.arange(128)[None, :]
    c["idx0"] = _bucket(q - k)
    c["vis0"] = (q >= k)
    c["idx1"] = _bucket(128 + q - k)
    c["M0"] = np.where(q >= k, 0.0, NEG).astype(np.float32)
    c["M4"] = np.where(k > q, 0.0, NEG).astype(np.float32)
    qq = np.arange(128)[:, None]
    dd = np.arange(504)[None, :] - 248
    relw = qq - 16 * dd - 31
    c["idxw"] = _bucket(relw)
    c["visw"] = relw >= 0
    c["MW"] = np.where(relw >= 0, 0.0, NEG).astype(np.float32)
    jj = np.arange(128)[None, :] - 64
    tbr = (np.arange(128)[:, None] >= 64).astype(np.int64)
    forced = (jj == tbr) | (jj == tbr - 1)
    future = jj > tbr
    c["KEEP"] = np.where(forced | future, 0.0, 1.0).astype(np.float32)
    c["ADD"] = np.where(forced, 1e30, np.where(future, -1e30, 0.0)).astype(np.float32)
    c["EXPM"] = (np.arange(S)[None, :] // 64 == np.arange(64)[:, None]).astype(BF)
    c["identb"] = np.eye(128).astype(BF)
    c["identf"] = np.eye(128).astype(np.float32)
    c["MT"] = ((k // 64 == q // 64) & (k <= q)).astype(np.float32)
    sm = np.ones((64, 512), np.float32)
    sm[:, ::64] = 0.0
    c["scanmask"] = sm
    ci = np.arange(256)[:, None] * 16
    sj = np.arange(64)[None, :] * 64
    ov = ((ci < sj + 64) & (ci + 32 > sj)).astype(np.float32)
    ov[255] = 0.0
    c["ovl"] = np.ascontiguousarray(ov.reshape(2, 128, 64).transpose(1, 0, 2)).astype(BF)
    selm = np.zeros((32, 32, 128), np.float32)
    for e in range(32):
        selm[e, e, :] = 1.0
    c["SELM"] = selm.reshape(32, 32 * 128)
    return c


OPTS = dict(nch=8, sub=99, nexp=32)


def build_nc(debug=False, stop=99):
    nc = bass.Bass("TRN2", target_bir_lowering=False)
    P = Prog()
    din = {}

    def dram_in(name, shape, dt=F32):
        din[name] = nc.dram_tensor(name, list(shape), dt, kind="ExternalInput").ap()
        return din[name]

    xT = dram_in("xT", [D, S])
    xTh = dram_in("xTh", [D, HALF])
    sel01 = dram_in("sel01", [128, 2])
    w1 = dram_in("w1", [D, W1C])
    w2 = dram_in("w2", [D, W2C])
    w3 = dram_in("w3", [D, 2048])
    gvec = dram_in("gvec", [128, 16])
    gfin = dram_in("gfin", [128, D])
    cw1 = dram_in("cw1", [2, 128, 16 * 128])
    cw2 = dram_in("cw2", [2, 128, 64])
    cpe = dram_in("cpe", [2, 128, 16])
    ovl_d = dram_in("ovl", [128, 2, 64], BF16)
    t0raw = dram_in("t0raw", [128, 512])
    t1raw = dram_in("t1raw", [128, 512])
    wraw = dram_in("wraw", [128, 4 * 504])
    m0_d = dram_in("M0", [128, 128])
    m4_d = dram_in("M4", [128, 128])
    mw_d = dram_in("MW", [128, 504])
    b31_d = dram_in("b31", [128, 4])
    keep_d = dram_in("KEEP", [128, 128])
    add_d = dram_in("ADD", [128, 128])
    expm_d = dram_in("EXPM", [64, S], BF16)
    identb_d = dram_in("identb", [128, 128], BF16)
    identf_d = dram_in("identf", [128, 128])
    mt_d = dram_in("MT", [128, 128])
    scanm_d = dram_in("scanmask", [64, 512])
    wal_d = dram_in("wal", [16, 128])
    nbal_d = dram_in("nbal", [64, 2])
    ng_d = dram_in("ng", [128, 2])
    wa_d = dram_in("wa", [512, D])
    wb_d = dram_in("wb", [512, D])
    wo_d = dram_in("wo", [D, D])
    wr_d = dram_in("wr", [D, 36])
    br_d = dram_in("br", [128, 36])
    selm_d = dram_in("SELM", [32, 32 * 128])
    wg_d = dram_in("wg", [OPTS["nexp"], D, 256])
    wu_d = dram_in("wu", [OPTS["nexp"], D, 256])
    wd_d = dram_in("wd", [OPTS["nexp"], 256, D])
    out_d = nc.dram_tensor("out", [HALF, D], F32, kind="ExternalOutput").ap()
    ysrc = [nc.dram_tensor("ysrc%d" % i, [512, 512], BF16) for i in range(8)]
    ydst = [nc.dram_tensor("ydst%d" % i, [1024, 512], BF16) for i in range(8)]
    w3b = [nc.dram_tensor("w3b%d" % i, [128, 8 * 128], BF16, **(dict(kind="ExternalOutput") if debug else {})) for i in range(16)]
    _k = dict(kind="ExternalOutput") if debug else {}
    wab_s = nc.dram_tensor("wab_s", [128, 4 * D], BF16, **_k)
    wbb_s = nc.dram_tensor("wbb_s", [128, 4 * D], BF16, **_k)
    wob_s = nc.dram_tensor("wob_s", [128, 8 * D], BF16, **_k)
    if debug:
        dbg_y = nc.dram_tensor("dbg_y", [1024, S], BF16, kind="ExternalOutput").ap()
        dbg_x1 = nc.dram_tensor("dbg_x1", [D, HALF], F32, kind="ExternalOutput").ap()
        dbg_br = nc.dram_tensor("dbg_br", [3, S, 256], F32, kind="ExternalOutput").ap()

    SB0 = 16512
    sboff = {}
    off = [SB0]

    def sb(name, shape, dt, at=None):
        nbytes = int(np.prod(shape[1:])) * (4 if dt == F32 else 2)
        nbytes = (nbytes + 63) // 64 * 64
        if at is None:
            o = off[0]
            off[0] += nbytes
        else:
            o = at
        assert o + nbytes <= 229344, (name, o, nbytes)
        sboff[name] = o
        return nc.alloc_sbuf_tensor_at(name, list(shape), dt, offset=o)

    BIG = sb("BIG", [128, 16384], F32)
    hT = nc.alloc_sbuf_tensor_at("hT", [128, 8 * S], BF16, offset=SB0)
    identb = sb("identb", [128, 128], BF16)
    identf = sb("identf", [128, 128], F32)
    onesb = sb("onesb", [128, 128], BF16)
    gv = sb("gv", [128, 16], F32)
    s01 = sb("s01", [128, 2], F32)
    common_end = off[0]

    W1b = sb("W1b", [128, 8 * W1C], BF16)
    W2b = sb("W2b", [128, 8 * W2C], BF16)
    KE = sb("KE", [128, S], BF16)
    KwT = sb("KwT", [64, S], BF16)
    VS = sb("VS", [128, NB * 65], BF16)
    VW = sb("VW", [128, NB * 65], BF16)
    cw2b = [sb("cw2b%d" % i, [128, 64], BF16) for i in range(2)]
    cconst = sb("cconst", [128, 2], F32)
    kcbT = sb("kcbT", [64, 256], BF16)
    VAUG = sb("VAUG", [128, 2 * 128], BF16)
    T0 = sb("T0", [128, 512], BF16)
    T1 = sb("T1", [128, 512], BF16)
    T4 = sb("T4", [128, 512], BF16)
    Wt = sb("Wt", [128, 4 * 504], F32)
    b31 = sb("b31", [128, 4], F32)
    KEEP = sb("KEEP", [128, 128], F32)
    ADDT = sb("ADDT", [128, 128], F32)
    MTs = sb("MTs", [128, 128], F32)
    scanm = sb("scanm", [64, 512], F32)
    walb = sb("walb", [16, 128], BF16)
    nbal = sb("nbal", [64, 2], F32)
    ngs = sb("ngs", [128, 2], F32)
    GS = sb("GS", [128, NB * 12], F32)
    r_start = off[0]
    wst = [sb("wst%d" % i, [128, 8 * 256], F32) for i in range(2)]
    xs = wst
    sqb = sb("sqb", [128, 8 * 256], BF16)
    rstd = sb("rstd", [128, 256], F32)
    kcT2 = sb("kcT2", [128, S + 16], BF16)
    vcT2 = sb("vcT2", [128, S + 16], BF16)
    cw1b = [sb("cw1b%d" % i, [128, 16 * 128], BF16) for i in range(2)]
    cpeb = [sb("cpeb%d" % i, [128, 16], BF16) for i in range(2)]
    cx = sb("cx", [128, 256], F32)
    ct1 = sb("ct1", [128, 256], F32)
    ct2 = sb("ct2", [128, 256], F32)
    chid = [sb("chid%d" % i, [128, 256], BF16) for i in range(2)]
    tmpT = sb("tmpT", [128, 4 * 504], F32)
    MWs = sb("MWs", [128, 504], F32)
    M0s = sb("M0s", [128, 128], F32)
    M4s = sb("M4s", [128, 128], F32)
    walf = sb("walf", [16, 128], F32)
    r1_end = off[0]
    off[0] = r_start
    QN = [sb("QN%d" % i, [128, 4 * 512], BF16) for i in range(2)]
    VB = sb("VB", [128, 4 * 256], BF16)
    scc = sb("scc", [128, 256], F32)
    ecb = sb("ecb", [128, 256], BF16)
    eT = sb("eT", [128, 256], BF16)
    ssum = sb("ssum", [128, 4], F32)
    rr = sb("rr", [128, 4], F32)
    coef = sb("coef", [128, 4], F32)
    ya = sb("ya", [128, 256], F32)
    yab = sb("yab", [128, 256], BF16)
    imp = sb("imp", [128, 64], F32)
    impm = sb("impm", [128, 64], F32)
    imp2 = sb("imp2", [128, 64], F32)
    m8 = sb("m8", [128, 16], F32)
    negp = sb("negp", [128, 128], BF16)
    PT = [sb("PT%d" % i, [128, 512], BF16) for i in range(4)]
    yw = sb("yw", [128, 256], F32)
    yc = [sb("yc%d" % i, [128, 256], F32) for i in range(2)]
    rrC = sb("rrC", [128, 4], F32)
    coefC = sb("coefC", [128, 4], F32)
    rrB = sb("rrB", [128, 4], F32)
    coefB = sb("coefB", [128, 4], F32)
    ngs2 = sb("ngs2", [128, 2], F32)
    gl1 = sb("gl1", [128, 128], F32)
    YT = [sb("YT%d" % i, [128, 4 * 512], BF16) for i in range(2)]
    gq = sb("gq", [64, 2 * 512], F32)
    gk = sb("gk", [64, 2 * 512], F32)
    gab = sb("gab", [16, 512], BF16)
    ge1 = sb("ge1", [64, 2 * 512], F32)
    gbc = sb("gbc", [64, 2 * 512], F32)
    gAq = sb("gAq", [64, 2 * 512], F32)
    gAk = sb("gAk", [64, 2 * 512], F32)
    qin = sb("qin", [64, 2 * 512], BF16)
    kin = sb("kin", [64, 2 * 512], BF16)
    kdT = sb("kdT", [64, 2 * 512], BF16)
    gdec = sb("gdec", [64, 2 * 8], F32)
    rTs = sb("rTs", [128, 2 * 512], BF16)
    ATb = [sb("ATb%d" % i, [128, 128], BF16) for i in range(2)]
    kdtok = [sb("kdtok%d" % i, [128, 64], BF16) for i in range(2)]
    St = [sb("St%d" % i, [64, 128], F32) for i in range(2)]
    Sbf = [[sb("Sbf%d_%d" % (i, j), [64, 128], BF16) for j in range(2)] for i in range(2)]
    gsq = sb("gsq", [128, 128], BF16)
    grs = sb("grs", [128, 128], F32)
    gyb = sb("gyb", [128, 128], F32)
    ybr = [sb("ybr%d" % i, [128, 256], F32) for i in range(2)] if debug else None
    off[0] = max(off[0], r1_end)
    cvf = [sb("cvf%d" % i, [128, 512], F32) for i in range(2)]
    cvb = [sb("cvb%d" % i, [128, 512], BF16) for i in range(2)]
    p1_end = off[0]

    off[0] = common_end
    HX = sb("HX", [128, 8 * HALF], BF16)
    COMBT = sb("COMBT", [32, HALF], F32)
    WRf = sb("WRf", [128, 8 * 36], F32)
    brs = sb("brs", [128, 36], F32)
    onesf = sb("onesf", [32, 128], F32)
    rt = sb("rt", [128, 64], F32)
    Lg = sb("Lg", [128, 36], F32)
    comb = sb("comb", [128, 32], F32)
    esel = sb("esel", [128, 8], F32)
    wst2 = [sb("wst2_%d" % i, [128, 8 * 256], F32) for i in range(2)]
    pm_start = off[0]
    Wab = sb("Wab", [128, 4 * D], BF16)
    Wbb = sb("Wbb", [128, 4 * D], BF16)
    Wob = sb("Wob", [128, 8 * D], BF16)
    W3p = [sb("W3p%d" % i, [128, 8 * 128], BF16) for i in range(4)]
    w3cv = [sb("w3cv%d" % i, [128, 8 * 128], BF16) for i in range(2)]
    YM = sb("YM", [128, 8 * 512], BF16)
    YH = sb("YH", [128, 8 * 512], BF16)
    SGp = [sb("SGp%d" % i, [128, 512], BF16) for i in range(2)]
    MG = sb("MG", [128, 8 * 512], BF16)
    m1 = sb("m1", [128, 512], F32)
    m2 = sb("m2", [128, 512], F32)
    xh = [sb("xh%d" % i, [128, 512], F32) for i in range(2)]
    sq2 = YH
    rstd2 = [sb("rstd2_%d" % i, [128, 512], F32) for i in range(2)]
    assert sboff["m2"] == sboff["m1"] + 2048 and sboff["xh1"] == sboff["xh0"] + 2048
    w3st = [nc.alloc_sbuf_tensor_at("w3st0", [128, 8 * 128], F32, offset=sboff["m1"]),
            nc.alloc_sbuf_tensor_at("w3st1", [128, 8 * 128], F32, offset=sboff["xh0"])]
    w3k = [("m1", "m2"), ("xh0", "xh1")]
    pm_end = off[0]
    off[0] = pm_start
    WGb = [sb("WGb%d" % i, [128, 8 * 256], BF16) for i in range(4)]
    WUb = [sb("WUb%d" % i, [128, 8 * 256], BF16) for i in range(4)]
    WDb = [sb("WDb%d" % i, [128, 2 * D], BF16) for i in range(4)]
    CB = [sb("CB%d" % i, [128, 512], BF16) for i in range(4)]
    cmask = [sb("cmask%d" % i, [32, 512], BF16) for i in range(4)]
    sil = [sb("sil%d" % i, [128, 512], BF16) for i in range(2)]
    t1b = [sb("t1b%d" % i, [128, 512], BF16) for i in range(2)]
    hid = [sb("hid%d" % i, [128, 2 * 512], BF16) for i in range(4)]
    xtm = sb("xtm", [128, D], F32)
    otm = sb("otm", [128, D], F32)
    gfs = sb("gfs", [128, D], F32)
    fss = sb("fss", [128, 4], F32)
    junk = hid[0]
    off[0] = max(off[0], pm_end)

    print('SBUF usage', r1_end, pm_end, off[0])
    es = ExitStack()
    PS = [es.enter_context(nc.psum_tensor("ps%d" % i, [128, 512], F32)) for i in range(8)]

    streams = {}
    cur = [None]

    def A(eng, fn, r=(), w=(), dma=None):
        if cur[0] is None:
            return P.add(eng, fn, r, w, dma)
        streams[cur[0]].append((eng, fn, r, w, dma))

    def begin(name):
        cur[0] = name
        streams.setdefault(name, [])

    def end():
        cur[0] = None

    def merge(names):
        lists = [streams.pop(n_) for n_ in names]
        idx = [0] * len(lists)
        tot = [max(1, len(l_)) for l_ in lists]
        while True:
            best = None
            for i_, l_ in enumerate(lists):
                if idx[i_] < len(l_):
                    f_ = (idx[i_] + 1) / tot[i_]
                    if best is None or f_ < best[0]:
                        best = (f_, i_)
            if best is None:
                break
            i_ = best[1]
            P.add(*lists[i_][idx[i_]])
            idx[i_] += 1

    ckn = [0]

    def dma(q, out, in_, r, w, key):
        if len(key) == 2 and key[0] == "c" and key[1].isdigit():
            ckn[0] += 1
            key = "k%d" % ckn[0]
        A(q, lambda e, out=out, in_=in_: e.dma_start(out=out, in_=in_), r, w, dma=key)

    def mm(out, lhsT, rhs, start, stop, r, w, sgc=False):
        A("pe", lambda e, out=out, lhsT=lhsT, rhs=rhs, start=start, stop=stop, sgc=sgc:
          e.matmul(out, lhsT=lhsT, rhs=rhs, start=start, stop=stop, skip_group_check=sgc), r, w)

    def tr(out, in_, ident, r, w):
        A("pe", lambda e, out=out, in_=in_, ident=ident: e.transpose(out=out, in_=in_, identity=ident), r, w)

    def act(out, in_, func, r, w, bias=None, scale=None, accum=None):
        def f(e, out=out, in_=in_, func=func, bias=bias, scale=scale, accum=accum):
            kw = {}
            if bias is not None:
                kw["bias"] = bias
            if scale is not None:
                kw["scale"] = scale
            if accum is not None:
                kw["accum_out"] = accum
            return e.activation(out=out, in_=in_, func=func, **kw)
        A("act", f, r, w)

    def tt(eng, out, in0, in1, op, r, w):
        A(eng, lambda e, out=out, in0=in0, in1=in1, op=op: e.tensor_tensor(out=out, in0=in0, in1=in1, op=op), r, w)

    def ts(eng, out, in0, s1, s2, op0, op1, r, w):
        def f(e, out=out, in0=in0, s1=s1, s2=s2, op0=op0, op1=op1):
            if op1 is None:
                return e.tensor_scalar(out=out, in0=in0, scalar1=s1, scalar2=None, op0=op0)
            return e.tensor_scalar(out=out, in0=in0, scalar1=s1, scalar2=s2, op0=op0, op1=op1)
        A(eng, f, r, w)

    def stt(eng, out, in0, scalar, in1, op0, op1, r, w):
        A(eng, lambda e, out=out, in0=in0, scalar=scalar, in1=in1, op0=op0, op1=op1:
          e.scalar_tensor_tensor(out=out, in0=in0, scalar=scalar, in1=in1, op0=op0, op1=op1), r, w)

    def cp(eng, out, in_, r, w):
        if eng == "act":
            A("act", lambda e, out=out, in_=in_: e.copy(out=out, in_=in_), r, w)
        else:
            A(eng, lambda e, out=out, in_=in_: e.tensor_copy(out=out, in_=in_), r, w)

    def ms(eng, ap, val, w):
        A(eng, lambda e, ap=ap, val=val: e.memset(ap, val), (), w)

    def recip(out, in_, r, w):
        A("dve", lambda e, out=out, in_=in_: e.reciprocal(out=out, in_=in_), r, w)

    def v3(ap, a):
        return ap.rearrange("p (a b) -> p a b", a=a)

    dma("sp", identb[:], identb_d, (), ("identb",), "c0")
    dma("sp", identf[:], identf_d, (), ("identf",), "c0")
    dma("sp", gv[:], gvec, (), ("gv",), "c0")
    dma("sp", s01[:], sel01, (), ("s01",), "c0")
    ms("dve", onesb[:], 1.0, ("onesb",))
    dma("sp", tmpT[:, 0:512], t0raw, (), ("tmpT",), "c1")
    dma("sp", M0s[:], m0_d, (), ("M0s",), "c1")
    dma("sp", M4s[:], m4_d, (), ("M4s",), "c1")
    dma("sp", b31[:], b31_d, (), ("b31",), "c1")
    for h in range(4):
        ts("dve", tmpT[:, h * 128:(h + 1) * 128], tmpT[:, h * 128:(h + 1) * 128], b31[:, h:h + 1], None,
           ALU.subtract, None, ("tmpT", "b31"), ("tmpT",))
        tt("dve", T0[:, h * 128:(h + 1) * 128], tmpT[:, h * 128:(h + 1) * 128], M0s[:], ALU.add,
           ("tmpT", "M0s"), ("T0",))
        cp("dve", T4[:, h * 128:(h + 1) * 128], M4s[:], ("M4s",), ("T4",))
    dma("sp", tmpT[:, 0:512], t1raw, ("T0",), ("tmpT",), "c2")
    for h in range(4):
        ts("dve", T1[:, h * 128:(h + 1) * 128], tmpT[:, h * 128:(h + 1) * 128], b31[:, h:h + 1], None,
           ALU.subtract, None, ("tmpT", "b31"), ("T1",))
    dma("sp", tmpT[:], wraw, ("T1",), ("tmpT",), "c3")
    dma("sp", MWs[:], mw_d, (), ("MWs",), "c3")
    for h in range(4):
        ts("dve", tmpT[:, h * 504:(h + 1) * 504], tmpT[:, h * 504:(h + 1) * 504], b31[:, h:h + 1], None,
           ALU.subtract, None, ("tmpT", "b31"), ("tmpT",))
        tt("dve", Wt[:, h * 504:(h + 1) * 504], tmpT[:, h * 504:(h + 1) * 504], MWs[:], ALU.add,
           ("tmpT", "MWs"), ("Wt",))
    dma("sp", KEEP[:], keep_d, (), ("KEEP",), "c4")
    dma("sp", ADDT[:], add_d, (), ("ADDT",), "c4")
    dma("sp", MTs[:], mt_d, (), ("MTs",), "c4")
    dma("sp", scanm[:], scanm_d, (), ("scanm",), "c4")
    dma("sp", walf[:], wal_d, (), ("walf",), "c4")
    cp("dve", walb[:], walf[:], ("walf",), ("walb",))
    dma("sp", nbal[:], nbal_d, (), ("nbal",), "c4")
    dma("sp", ngs[:], ng_d, (), ("ngs",), "c4")
    dma("sp", KE[64:128, :], expm_d, (), ("KEexp",), "c4")
    dma("sp", v3(VAUG[:], 2)[:, :, 64:128], ovl_d, (), ("VAUGo",), "c4")
    ms("dve", v3(VS[:], NB)[:, :, 64:65], 1.0, ("VSone",))
    ms("dve", v3(VW[:], NB)[:, :, 64:65], 1.0, ("VWone",))
    ms("dve", v3(VAUG[:], 2)[:, :, 0:64], 0.0, ("VAUGv",))
    ms("dve", kcbT[:], 0.0, ("kcbT",))

    wcnt = [0]

    def load_w(dst, src, ncols, gofs, key, dstkey):
        dv = v3(dst[:], 8)
        sv = src.rearrange("(kc p) n -> p kc n", p=128)
        c0 = 0
        while c0 < ncols:
            cw = min(256, ncols - c0)
            i = wcnt[0] % 2
            wcnt[0] += 1
            st = v3(wst[i][:], 8)
            dma("sp", st[:, :, 0:cw], sv[:, :, c0:c0 + cw], (), ("wst%d" % i,), "w%d" % i)
            cp("act" if (wcnt[0] % 2) else "pool", dv[:, :, c0:c0 + cw], st[:, :, 0:cw], ("wst%d" % i,), (dstkey,))
            c0 += cw

    load_w(W1b, w1, W1C, 0, "w", "W1b")
    load_w(W2b, w2, W2C, 0, "w", "W2b")
    for i in range(2):
        dma("sp", wst[i][:, 0:2048], cw1[i], ("W1b", "W2b"), ("wst%d" % i,), "w%d" % i)
        cp("dve", cw1b[i][:], wst[i][:, 0:2048], ("wst%d" % i,), ("cw1b%d" % i,))
        dma("sp", ct1[:, 0:64], cw2[i], ("cw2b%d" % (i - 1),) if i else (), ("ct1",), "c5")
        cp("dve", cw2b[i][:], ct1[:, 0:64], ("ct1",), ("cw2b%d" % i,))
        dma("sp", ct2[:, 0:16], cpe[i], ("cpeb%d" % (i - 1),) if i else (), ("ct2",), "c5")
        cp("dve", cpeb[i][:], ct2[:, 0:16], ("ct2",), ("cpeb%d" % i,))

    W1v = v3(W1b[:], 8)
    W2v = v3(W2b[:], 8)
    hTv = v3(hT[:], 8)
    if stop <= 0:
        P.stopped = True
    for c in range(16):
        t0 = c * 256
        i = c % 2
        xv = v3(xs[i][:], 8)
        dma("sp", xv, xT.rearrange("(kc p) t -> p kc t", p=128)[:, :, t0:t0 + 256],
            (), ("wst%d" % i,), "w%d" % i)
        act(sqb[:], xs[i][:], AF.Square, ("wst%d" % i,), ("sqb",))
        sqv = v3(sqb[:], 8)
        for kc in range(8):
            mm(PS[0][:, 0:256], onesb[:], sqv[:, kc, :], kc == 0, kc == 7, ("onesb", "sqb"), ("ps0",))
        act(rstd[:], PS[0][:, 0:256], AF.Sqrt, ("ps0",), ("rstd",), bias=EPS, scale=1.0 / D)
        recip(rstd[:], rstd[:], ("rstd",), ("rstd",))
        for kc in range(8):
            stt("dve", hTv[:, kc, t0:t0 + 256], xv[:, kc, :], gv[:, kc:kc + 1], rstd[:],
                ALU.mult, ALU.mult, ("wst%d" % i, "rstd", "gv"), ("hT%d" % (c // 2),))
        if c % 2 == 1:
            ch = c // 2
            tc0 = ch * 512
            for which, (dstT, col0) in enumerate(((kcT2, 256), (vcT2, 384))):
                pb = PS[1 + which]
                for kc in range(8):
                    mm(pb[:, :], W1v[:, kc, col0:col0 + 128], hTv[:, kc, tc0:tc0 + 512], kc == 0, kc == 7,
                       ("W1b", "hT%d" % ch), ("ps%d" % (1 + which),))
                nm = "kcT2" if which == 0 else "vcT2"
                cp("act", dstT[0:64, 16 + tc0:16 + tc0 + 512], pb[0:64, :], ("ps%d" % (1 + which),), (nm,))
                cp("dve", dstT[64:128, tc0:tc0 + 512], pb[64:128, :], ("ps%d" % (1 + which),), (nm,))

    if stop <= 1:
        P.stopped = True
    for i in range(2):
        srcT = kcT2 if i == 0 else vcT2
        nm = "kcT2" if i == 0 else "vcT2"
        w1v_ = v3(cw1b[i][:], 16)
        for l in range(16):
            mm(PS[0][:, 0:1], w1v_[:, l, :], cpeb[i][:, l:l + 1], l == 0, l == 15,
               ("cw1b%d" % i, "cpeb%d" % i), ("ps0",))
        cp("dve", cconst[:, i:i + 1], PS[0][:, 0:1], ("ps0",), ("cconst",))
        for l in range(16):
            mm(PS[1][:, 0:255], w1v_[:, l, :], srcT[:, 16 + l:16 + l + 16 * 255:16], l == 0, l == 15,
               ("cw1b%d" % i, nm), ("ps1",))
        act(cx[:, 0:255], PS[1][:, 0:255], AF.Identity, ("ps1", "cconst"), ("cx",), bias=cconst[:, i:i + 1])
        tt("dve", ct1[:, 0:255], cx[:, 0:255], cx[:, 0:255], ALU.mult, ("cx",), ("ct1",))
        ts("dve", ct1[:, 0:255], ct1[:, 0:255], GC2, 1.0, ALU.mult, ALU.add, ("ct1",), ("ct1",))
        tt("dve", ct1[:, 0:255], ct1[:, 0:255], cx[:, 0:255], ALU.mult, ("ct1", "cx"), ("ct1",))
        act(ct2[:, 0:255], ct1[:, 0:255], AF.Sigmoid, ("ct1",), ("ct2",), scale=2.0 * GC1)
        tt("dve", chid[i][:, 0:255], cx[:, 0:255], ct2[:, 0:255], ALU.mult, ("cx", "ct2"), ("chid%d" % i,))
    mm(PS[2][0:64, 0:255], cw2b[0][:], chid[0][:, 0:255], True, True, ("cw2b0", "chid0"), ("ps2",))
    cp("act", kcbT[:, 0:255], PS[2][0:64, 0:255], ("ps2", "kcbT"), ("kcbT",))
    VAv = v3(VAUG[:], 2)
    mm(PS[3][:, 0:64], chid[1][:, 0:128], cw2b[1][:], True, True, ("cw2b1", "chid1"), ("ps3",))
    cp("act", VAv[:, 0, 0:64], PS[3][:, 0:64], ("ps3", "VAUGv"), ("VAUGv",))
    mm(PS[3][0:127, 64:128], chid[1][:, 128:255], cw2b[1][:], True, True, ("cw2b1", "chid1"), ("ps3",))
    cp("act", VAv[0:127, 1, 0:64], PS[3][0:127, 64:128], ("ps3", "VAUGv"), ("VAUGv",))

    if stop <= 2:
        P.stopped = True
    P.barrier(["pe", "act", "dve", "pool", "sp"])
    ms("dve", negp[:, 0:64], 0.0, ("negp0",))
    for i in range(2):
        ms("dve", St[i][:], 0.0, ("St%d" % i,))
        ms("dve", Sbf[i][0][:], 0.0, ("Sbf%d_0" % i,))
    VSv = v3(VS[:], NB)
    VWv = v3(VW[:], NB)
    GSv = v3(GS[:], NB)
    VBv = v3(VB[:], 4)
    Wtv = v3(Wt[:], 4)
    psb5 = PS[4][:, 320:512].bitcast(BF16)
    psb6 = PS[6][:, 384:512].bitcast(BF16)
    psb7 = PS[7][:, 128:160].bitcast(BF16)

    def fm_proj(col0, m, pbank, tc0, rkeys):
        for kc in range(8):
            mm(PS[pbank][0:m, :], W1v[:, kc, col0:col0 + m], hTv[:, kc, tc0:tc0 + 512], kc == 0, kc == 7,
               ("W1b",) + rkeys, ("ps%d" % pbank,))

    sidx = [0, 0]
    w3v = w3.rearrange("(kc p) n -> p kc n", p=128)
    cvjobs = []
    for c16 in range(16):
        for hf in range(2):
            cvjobs.append((w3v[:, hf * 4:hf * 4 + 4, c16 * 128:(c16 + 1) * 128], 4,
                           w3b[c16].ap()[:, hf * 512:(hf + 1) * 512], "w3b%d" % c16))
    for wsrc, wdst, wkey, nkc in ((wa_d, wab_s, "wab_s", 4), (wb_d, wbb_s, "wbb_s", 4), (wo_d, wob_s, "wob_s", 8)):
        for kc in range(nkc):
            for hf in range(2):
                cvjobs.append((wsrc[kc * 128:(kc + 1) * 128, hf * 512:(hf + 1) * 512], 1,
                               wdst.ap()[:, kc * D + hf * 512:kc * D + (hf + 1) * 512], wkey))
    cvi = [0]

    def emit_cv(njobs):
        for _ in range(njobs):
            if cvi[0] >= len(cvjobs):
                return
            src, nk, dst, dkey = cvjobs[cvi[0]]
            i = cvi[0] % 2
            cvi[0] += 1
            stv = v3(cvf[i][:], nk) if nk > 1 else cvf[i][:]
            dma("sp", stv, src, (), ("cvf%d" % i,), "cvi%d" % i)
            cp("pool", cvb[i][:], cvf[i][:], ("cvf%d" % i,), ("cvb%d" % i,))
            dma("sp", dst, cvb[i][:], ("cvb%d" % i,), (dkey,), "cvo%d" % i)

    for ch in range(OPTS["nch"]):
        tc0 = ch * 512
        emit_cv(8)
        hk = ("hT%d" % ch,)
        QNc = QN[ch % 2]
        qnk = "QN%d" % (ch % 2)
        QNv = QNc[:].rearrange("p (b h q) -> p b h q", b=4, h=4)
        YTc = YT[ch % 2]
        ytk = "YT%d" % (ch % 2)
        YTv = v3(YTc[:], 4)
        for h in range(4):
            pbk = h % 2
            fm_proj(h * 64, 64, pbk, tc0, hk)
            act(QNv[0:64, :, h, :], PS[pbk][0:64, :].rearrange("p (b q) -> p b q", b=4), AF.Identity,
                ("ps%d" % pbk,), (qnk + "q",), scale=0.125)
        for tl in range(4):
            n = ch * 4 + tl
            pbk = tl % 2
            for kc in range(8):
                mm(PS[pbk][:, 0:W2C], hTv[:, kc, n * 128:(n + 1) * 128], W2v[:, kc, :], kc == 0, kc == 7,
                   ("W2b",) + hk, ("ps%d" % pbk,))
            cp("dve", VSv[:, n, 0:64], PS[pbk][:, 0:64], ("ps%d" % pbk,), ("VS",))
            cp("dve", VWv[:, n, 0:64], PS[pbk][:, 64:128], ("ps%d" % pbk,), ("VW",))
            act(GSv[:, n, :], PS[pbk][:, 128:140], AF.Sigmoid, ("ps%d" % pbk,), ("GS",))
            cp("act", VBv[:, tl, :], PS[pbk][:, 140:396], ("ps%d" % pbk,), ("VB",))

        begin("P")
        fm_proj(512, 64, 0, tc0, hk)
        cp("act", KE[0:64, tc0:tc0 + 512], PS[0][0:64, :], ("ps0",), ("KEk",))
        fm_proj(576, 64, 1, tc0, hk)
        cp("dve", KwT[:, tc0:tc0 + 512], PS[1][0:64, :], ("ps1",), ("KwT",))
        gqv = v3(gq[:], 2)
        gkv = v3(gk[:], 2)
        for h in range(2):
            fm_proj(640 + h * 64, 64, 0, tc0, hk)
            act(gqv[:, h, :], PS[0][0:64, :], AF.Identity, ("ps0",), ("gq",), scale=0.125)
            fm_proj(768 + h * 64, 64, 1, tc0, hk)
            cp("dve", gkv[:, h, :], PS[1][0:64, :], ("ps1",), ("gk",))
        fm_proj(896, 16, 0, tc0, hk)
        cp("act", gab[:], PS[0][0:16, :], ("ps0",), ("gab",))
        rTv = v3(rTs[:], 2)
        for h in range(2):
            fm_proj(912 + h * 128, 128, 1, tc0, hk)
            act(rTv[:, h, :], PS[1][:, :], AF.Silu, ("ps1",), ("rTs",))
        ge1v = v3(ge1[:], 2)
        gbcv = v3(gbc[:], 2)
        gAqv = v3(gAq[:], 2)
        gAkv = v3(gAk[:], 2)
        qinv = v3(qin[:], 2)
        kinv = v3(kin[:], 2)
        kdTv = v3(kdT[:], 2)
        gdecv = v3(gdec[:], 2)
        for h in range(2):
            mm(PS[0][0:64, :], walb[:, h * 64:(h + 1) * 64], gab[:], True, True, ("walb", "gab"), ("ps0",))
            act(ge1v[:, h, :], PS[0][0:64, :], AF.Exp, ("ps0", "nbal"), ("ge1",), bias=nbal[:, h:h + 1], scale=-1.0)
            act(ge1v[:, h, :], ge1v[:, h, :], AF.Ln, ("ge1",), ("ge1",), bias=1.0)
            A("dve", lambda e, o=gbcv[:, h, :], d1=ge1v[:, h, :]: e.tensor_tensor_scan(
                out=o, data0=scanm[:], data1=d1, initial=0.0, op0=ALU.mult, op1=ALU.add),
              ("ge1", "scanm"), ("gbc",))
        act(gAq[:], gbc[:], AF.Exp, ("gbc",), ("gAq",), scale=-1.0 / 16.0)
        act(gAk[:], gbc[:], AF.Exp, ("gbc",), ("gAk",), scale=1.0 / 16.0)
        tt("dve", qin[:], gq[:], gAq[:], ALU.mult, ("gq", "gAq"), ("qin",))
        tt("dve", kin[:], gk[:], gAk[:], ALU.mult, ("gk", "gAk"), ("kin",))
        for h in range(2):
            cp("dve", gdecv[:, h, :], gAqv[:, h, 63:512:64], ("gAq",), ("gdec",))
            tt("dve", kdTv[:, h, :].rearrange("p (c t) -> p c t", c=8),
               kinv[:, h, :].rearrange("p (c t) -> p c t", c=8),
               gdecv[:, h, :].unsqueeze(2).to_broadcast([64, 8, 64]), ALU.mult, ("kin", "gdec"), ("kdT",))

        end()
        def emit_A1(n, tl):
            for h in range(4):
                mm(PS[6][:, 0:256], QNv[0:64, tl, h, :], kcbT[:], True, True, (qnk + "q", "kcbT"), ("ps6",))
                o0 = 248 - 8 * n
                tt("dve", scc[:], PS[6][:, 0:256], Wtv[:, h, o0:o0 + 256], ALU.add, ("ps6", "Wt"), ("scc",))
                ms("dve", ssum[:, h:h + 1], 0.0, ("ssum",))
                act(ecb[:], scc[:], AF.Exp, ("scc", "ssum"), ("ecb", "ssum"), accum=ssum[:, h:h + 1])
                for cc in range(2):
                    tr(psb6[:, cc * 128:(cc + 1) * 128], ecb[:, cc * 128:(cc + 1) * 128], identb[:],
                       ("ecb", "identb"), ("ps6",))
                cp("act", eT[:], psb6[:, :], ("ps6",), ("eT",))
                for cc in range(2):
                    mm(PS[6][:, 256:384], eT[:, cc * 128:(cc + 1) * 128], VAv[:, cc, :], cc == 0, cc == 1,
                       ("eT", "VAUGv", "VAUGo"), ("ps6",))
                ts("dve", rrC[:, h:h + 1], ssum[:, h:h + 1], 1e-30, None, ALU.max, None, ("ssum",), ("rrC",))
                recip(rrC[:, h:h + 1], rrC[:, h:h + 1], ("rrC",), ("rrC",))
                tt("dve", coefC[:, h:h + 1], rrC[:, h:h + 1], GSv[:, n, h:h + 1], ALU.mult, ("rrC", "GS"), ("coefC",))
                ts("dve", yc[n % 2][:, h * 64:(h + 1) * 64], PS[6][:, 256:320], coefC[:, h:h + 1], None, ALU.mult, None,
                   ("ps6", "coefC"), ("yc%d" % (n % 2),))
                if h == 0:
                    ts("dve", imp[:], PS[6][:, 320:384], rrC[:, h:h + 1], None, ALU.mult, None, ("ps6", "rrC"), ("imp",))
                else:
                    stt("dve", imp[:], PS[6][:, 320:384], rrC[:, h:h + 1], imp[:], ALU.mult, ALU.add,
                        ("ps6", "rrC", "imp"), ("imp",))
            k0 = 64 - 2 * n
            tt("dve", impm[:], imp[:], KEEP[:, k0:k0 + 64], ALU.mult, ("imp", "KEEP"), ("impm",))
            tt("dve", impm[:], impm[:], ADDT[:, k0:k0 + 64], ALU.add, ("impm", "ADDT"), ("impm",))
            ms("dve", impm[:, 0:1], 1e30, ("impm",))
            A("dve", lambda e: e.max(out=m8[:, 0:8], in_=impm[:]), ("impm",), ("m8",))
            A("dve", lambda e: e.match_replace(out=imp2[:], in_to_replace=m8[:, 0:8], in_values=impm[:],
                                              imm_value=-3e38), ("impm", "m8"), ("imp2",))
            A("dve", lambda e: e.max(out=m8[:, 8:16], in_=imp2[:]), ("imp2",), ("m8",))
            ts("dve", negp[:, 64:128], impm[:], m8[:, 15:16], NEG, ALU.is_lt, ALU.mult, ("impm", "m8"), ("negp",))
            tr(psb6[:, 0:128], negp[:], identb[:], ("negp", "negp0", "identb"), ("ps6",))
            for h in range(4):
                cp("act" if h % 2 else "dve", QNv[64:128, tl, h, :], psb6[64:128, 0:128], ("ps6",), (qnk + "n%d" % tl,))
        for tl in range(4):
            n = ch * 4 + tl
            if tl == 0:
                begin("A1")
                emit_A1(n, tl)
                end()
                merge(["P", "A1"])
            begin("A")
            for br in range(2):
                if br == 1:
                    end()
                    begin("B")
                kbs = list(range(0, n + 1)) if br == 0 else list(range(max(0, n - 4), n + 1))
                accb = PS[4] if br == 0 else PS[5]
                acck = "ps4" if br == 0 else "ps5"
                accv = accb[:, 0:260].rearrange("p (h c) -> p h c", h=4)
                Vv = VSv if br == 0 else VWv
                vk = "VS" if br == 0 else "VW"
                rrX = rr if br == 0 else rrB
                coefX = coef if br == 0 else coefB
                rrk = "rr" if br == 0 else "rrB"
                cfk = "coef" if br == 0 else "coefB"
                for ki, kb in enumerate(kbs):
                    sb_ = (2 + (sidx[br] % 2)) if br == 0 else 1
                    pi_ = (0 if br == 0 else 2) + (sidx[br] % 2)
                    pt = PT[pi_]
                    ptk = "PT%d" % pi_
                    sidx[br] += 1
                    extra = []
                    if kb == n:
                        extra.append((T0, "T0"))
                    if kb == n - 1:
                        extra.append((T1, "T1"))
                    if br == 1 and kb == n - 4:
                        extra.append((T4, "T4"))
                    if br == 0:
                        mm(PS[sb_][:, :], KE[:, kb * 128:(kb + 1) * 128], QNc[:, tl * 512:(tl + 1) * 512],
                           True, not extra, ("KEk", "KEexp", qnk + "q", qnk + "n%d" % tl), ("ps%d" % sb_,))
                    else:
                        mm(PS[sb_][:, :], KwT[:, kb * 128:(kb + 1) * 128], QNc[0:64, tl * 512:(tl + 1) * 512],
                           True, not extra, ("KwT", qnk + "q"), ("ps%d" % sb_,))
                    for xi, (tb, tk) in enumerate(extra):
                        mm(PS[sb_][:, :], identb[:], tb[:], False, xi == len(extra) - 1, ("identb", tk), ("ps%d" % sb_,))
                    act(pt[:], PS[sb_][:, :], AF.Exp, ("ps%d" % sb_,), (ptk,))
                    for h in range(4):
                        mm(accv[:, h, :], pt[:, h * 128:(h + 1) * 128], Vv[:, kb, :], ki == 0 and h == 0, ki == len(kbs) - 1,
                           (ptk, vk, vk + "one"), (acck,), sgc=True)
                gofs = 4 if br == 0 else 8
                cp("dve", rrX[:, 0:4].unsqueeze(2), accv[:, :, 64:65], (acck,), (rrk,))
                recip(rrX[:, 0:4], rrX[:, 0:4], (rrk,), (rrk,))
                tt("dve", coefX[:, 0:4], rrX[:, 0:4], GSv[:, n, gofs:gofs + 4], ALU.mult, (rrk, "GS"), (cfk,))
                if debug and br == 0:
                    dma("sp", dbg_br[0, n * 128:(n + 1) * 128, :], yc[n % 2][:], ("yc%d" % (n % 2),), ("dbgbr",), "dbgb")
                for h in range(4):
                    if debug:
                        ts("dve", ybr[br][:, h * 64:(h + 1) * 64], accv[:, h, 0:64], coefX[:, h:h + 1], None, ALU.mult, None,
                           (acck, cfk), ("ybr%d" % br,))
                    if br == 0:
                        ts("dve", ya[:, h * 64:(h + 1) * 64], accv[:, h, 0:64], coefX[:, h:h + 1], None, ALU.mult, None,
                           (acck, cfk), ("ya",))
                    else:
                        ts("dve", yw[:, h * 64:(h + 1) * 64], accv[:, h, 0:64], coefX[:, h:h + 1], None, ALU.mult, None,
                           (acck, cfk), ("yw",))
                if debug:
                    dma("sp", dbg_br[1 + br, n * 128:(n + 1) * 128, :], ybr[br][:], ("ybr%d" % br,), ("dbgbr",), "dbgb")
            end()
            begin("C")
            for h in range(2):
                ts0 = tl * 128
                mm(PS[7][:, 0:128], kinv[:, h, ts0:ts0 + 128], qinv[:, h, ts0:ts0 + 128], True, True,
                   ("kin", "qin"), ("ps7",))
                atb = ATb[h]
                tt("dve", atb[:], PS[7][:, 0:128], MTs[:], ALU.mult, ("ps7", "MTs"), ("ATb%d" % h,))
                tr(psb7[:, 0:64], kdTv[:, h, ts0:ts0 + 128], identb[0:64, 0:64], ("kdT", "identb"), ("ps7",))
                cp("act", kdtok[h][:], psb7[:, 0:64], ("ps7",), ("kdtok%d" % h,))
                mm(PS[7][0:64, 256:384], kdtok[h][0:64, :], VBv[0:64, tl, h * 128:(h + 1) * 128], True, True,
                   ("kdtok%d" % h, "VB"), ("ps7",))
                mm(PS[0][0:64, 0:128], kdtok[h][64:128, :], VBv[64:128, tl, h * 128:(h + 1) * 128], True, True,
                   ("kdtok%d" % h, "VB"), ("ps0",))
                stt("dve", St[h][:], St[h][:], gdecv[:, h, tl * 2:tl * 2 + 1], PS[7][0:64, 256:384], ALU.mult, ALU.add,
                    ("St%d" % h, "gdec", "ps7"), ("St%d" % h,))
                cp("act", Sbf[h][1][:], St[h][:], ("St%d" % h,), ("Sbf%d_1" % h,))
                mm(PS[7][:, 128:192], Sbf[h][0][:], qinv[:, h, ts0:ts0 + 64], True, False, ("Sbf%d_0" % h, "qin"), ("ps7",), sgc=True)
                mm(PS[7][:, 192:256], Sbf[h][1][:], qinv[:, h, ts0 + 64:ts0 + 128], False, False, ("Sbf%d_1" % h, "qin"), ("ps7",), sgc=True)
                mm(PS[7][:, 128:256], VBv[:, tl, h * 128:(h + 1) * 128], atb[:], False, True, ("VB", "ATb%d" % h), ("ps7",), sgc=True)
                stt("dve", St[h][:], St[h][:], gdecv[:, h, tl * 2 + 1:tl * 2 + 2], PS[0][0:64, 0:128], ALU.mult, ALU.add,
                    ("St%d" % h, "gdec", "ps0"), ("St%d" % h,))
                cp("act", Sbf[h][0][:], St[h][:], ("St%d" % h,), ("Sbf%d_0" % h,))
                act(gsq[:], PS[7][:, 128:256], AF.Square, ("ps7",), ("gsq",))
                mm(PS[7][:, 0:128], onesb[:], gsq[:], True, True, ("onesb", "gsq"), ("ps7",))
                act(gl1[:], PS[7][:, 0:128], AF.Ln, ("ps7",), ("gl1",), bias=EPS, scale=1.0 / 128.0)
                act(grs[:], gl1[:], AF.Exp, ("gl1",), ("grs",), scale=-0.5)
                tt("dve", gyb[:], PS[7][:, 128:256], grs[:], ALU.mult, ("ps7", "grs"), ("gyb",))
                stt("dve", YTv[:, 2 + h, ts0:ts0 + 128], gyb[:], ngs[:, h:h + 1], rTv[:, h, ts0:ts0 + 128],
                    ALU.mult, ALU.mult, ("gyb", "ngs", "rTs"), (ytk,))
            end()
            if tl < 3:
                begin("A1")
                emit_A1(n + 1, tl + 1)
                end()
                merge(["A", "B", "C", "A1"])
            else:
                merge(["A", "B", "C"])
            tt("dve", ya[:], ya[:], yw[:], ALU.add, ("ya", "yw"), ("ya",))
            tt("dve", ya[:], ya[:], yc[n % 2][:], ALU.add, ("ya", "yc%d" % (n % 2)), ("ya",))
            cp("act", yab[:], ya[:], ("ya",), ("yab",))
            for cc in range(2):
                tr(psb5[:, cc * 128:(cc + 1) * 128], yab[:, cc * 128:(cc + 1) * 128], identb[:], ("yab", "identb"), ("ps4",))
            cp("act", YTv[:, 0:2, tl * 128:(tl + 1) * 128], psb5[:, 0:256].rearrange("p (c q) -> p c q", c=2),
               ("ps4",), (ytk,))
        dma("act", ysrc[ch].ap().rearrange("(c p) t -> p c t", p=128), YTv, (ytk,), ("ysrc%d" % ch,), "ys%d" % ch)
        P.add("pool", lambda e, ch=ch: e.collective_compute("AllGather", ALU.bypass,
                                                          replica_groups=[[0, 1], [2, 3], [4, 5], [6, 7]],
                                                          ins=[ysrc[ch].ap()], outs=[ydst[ch].ap()]),
              ("ysrc%d" % ch,), ("ydst%d" % ch,), dma="cc%d" % ch, inc=1)

    if stop <= 3:
        P.stopped = True
    if debug:
        for i in range(8):
            dma("sp", dbg_y[:, i * 512:(i + 1) * 512], ydst[i].ap(), ("ydst%d" % i,), ("dbgy",), "dbg")

    if stop <= 4:
        P.stopped = True
    HXv = v3(HX[:], 8)
    P.barrier(["pe", "act", "dve", "pool", "sp"])
    for kc in range(8):
        eng = "dve"
        ts(eng, HXv[:, kc, :], hTv[:, kc, 0:HALF], s01[:, 0:1], None, ALU.mult, None,
           tuple("hT%d" % i for i in range(8)) + ("s01",), ("HX",))
        stt(eng, HXv[:, kc, :], hTv[:, kc, HALF:S], s01[:, 1:2], HXv[:, kc, :], ALU.mult, ALU.add,
            tuple("hT%d" % i for i in range(8)) + ("s01", "HX"), ("HX",))
    P.barrier(["pe", "act", "dve", "pool", "sp"])

    ACC = BIG
    ACCv = v3(ACC[:], 8)
    wcnt2 = [0]

    def load_w2(dstv, src_view, nk, ncols, dstkey, q="sp"):
        c0 = 0
        while c0 < ncols:
            cw = min(2048 // nk, ncols - c0)
            i = wcnt2[0] % 2
            wcnt2[0] += 1
            st = v3(wst2[i][:, 0:nk * cw], nk)
            dma(q, st, src_view[:, :, c0:c0 + cw], (), ("wst2_%d" % i,), "v%d" % i)
            cp("act" if (wcnt2[0] % 2) else "pool", dstv[:, :, c0:c0 + cw], st, ("wst2_%d" % i,), (dstkey,))
            c0 += cw

    Wav = v3(Wab[:], 4)
    Wbv = v3(Wbb[:], 4)
    Wov = v3(Wob[:], 8)
    emit_cv(len(cvjobs))
    dma("sp", Wab[:], wab_s.ap(), ("wab_s",), ("Wab",), "wl")
    dma("sp", Wbb[:], wbb_s.ap(), ("wbb_s",), ("Wbb",), "wl")
    dma("sp", Wob[:], wob_s.ap(), ("wob_s",), ("Wob",), "wl")
    dma("sp", v3(WRf[:], 8), wr_d.rearrange("(kc p) n -> p kc n", p=128), (), ("WRf0",), "c6")
    WRv = v3(WRf[:], 8)
    for kc in range(8):
        ts("dve", WRv[:, kc, :], WRv[:, kc, :], gv[:, 8 + kc:9 + kc], None, ALU.mult, None, ("WRf0", "gv"), ("WRf",))
    dma("sp", brs[:], br_d, (), ("brs",), "c6")
    ms("dve", onesf[:], 1.0, ("onesf",))

    ydv = [y_.ap().rearrange("(c p) t -> p c t", p=128) for y_ in ydst]
    w3v = w3.rearrange("(kc p) n -> p kc n", p=128)
    xhd = xTh.rearrange("(kc p) t -> p kc t", p=128)
    YMv = v3(YM[:], 8)
    MGv = v3(MG[:], 8)
    sq2v = v3(sq2[:], 8)
    FA = (0, 1, 4, 5)
    FB = (2, 3, 6, 7)
    w3c = [0]
    def emit_routing(tcx, t0):
        for tl in range(4):
            tt0 = t0 + tl * 128
            for kc in range(8):
                mm(PS[7][:, 0:36], ACCv[:, kc, tt0:tt0 + 128], WRv[:, kc, :], kc == 0, kc == 7,
                   ("ACC%d" % tcx, "WRf"), ("ps7",))
            mm(PS[7][:, 64:65], rstd2[tcx % 2][0:1, tl * 128:(tl + 1) * 128], identf[0:1, 0:1], True, True,
               ("rstd2_%d" % (tcx % 2), "identf"), ("ps7",))
            cp("dve", rt[:, 0:1], PS[7][:, 64:65], ("ps7",), ("rt0",))
            stt("dve", Lg[:], PS[7][:, 0:36], rt[:, 0:1], brs[:], ALU.mult, ALU.add, ("ps7", "rt0", "brs"), ("Lg",))
            A("dve", lambda e: e.reduce_max(out=rt[:, 1:2], in_=Lg[:, 0:4], axis=AX.X), ("Lg",), ("rt1",))
            ts("dve", rt[:, 2:3], rt[:, 1:2], -1.0, None, ALU.mult, None, ("rt1",), ("rt2",))
            ms("dve", rt[:, 3:4], 0.0, ("rt3",))
            act(rt[:, 8:12], Lg[:, 0:4], AF.Exp, ("Lg", "rt2", "rt3"), ("rt8", "rt3"), bias=rt[:, 2:3], accum=rt[:, 3:4])
            recip(rt[:, 4:5], rt[:, 3:4], ("rt3",), ("rt4",))
            ts("dve", rt[:, 12:16], Lg[:, 0:4], rt[:, 1:2], None, ALU.is_ge, None, ("Lg", "rt1"), ("rt12",))
            for gi in range(4):
                if gi == 0:
                    ts("dve", esel[:], Lg[:, 4:12], rt[:, 12:13], None, ALU.mult, None, ("Lg", "rt12"), ("esel",))
                else:
                    stt("dve", esel[:], Lg[:, 4 + 8 * gi:12 + 8 * gi], rt[:, 12 + gi:13 + gi], esel[:], ALU.mult, ALU.add,
                        ("Lg", "rt12", "esel"), ("esel",))
            A("dve", lambda e: e.max(out=rt[:, 16:24], in_=esel[:]), ("esel",), ("rt16",))
            tt("dve", rt[:, 24:25], rt[:, 17:18], rt[:, 16:17], ALU.subtract, ("rt16",), ("rt24",))
            act(rt[:, 25:26], rt[:, 24:25], AF.Exp, ("rt24",), ("rt25",))
            ts("dve", rt[:, 25:26], rt[:, 25:26], 1.0, None, ALU.add, None, ("rt25",), ("rt25",))
            recip(rt[:, 26:27], rt[:, 25:26], ("rt25",), ("rt26",))
            ts("dve", rt[:, 27:28], rt[:, 26:27], -1.0, 1.0, ALU.mult, ALU.add, ("rt26",), ("rt27",))
            tt("dve", rt[:, 26:27], rt[:, 26:27], rt[:, 4:5], ALU.mult, ("rt26", "rt4"), ("rt26",))
            tt("dve", rt[:, 27:28], rt[:, 27:28], rt[:, 4:5], ALU.mult, ("rt27", "rt4"), ("rt27",))
            ts("dve", rt[:, 32:40], esel[:], rt[:, 16:17], rt[:, 26:27], ALU.is_equal, ALU.mult, ("esel", "rt16", "rt26"), ("rt32",))
            ts("dve", rt[:, 40:48], esel[:], rt[:, 17:18], rt[:, 27:28], ALU.is_equal, ALU.mult, ("esel", "rt16", "rt27"), ("rt40",))
            tt("dve", rt[:, 32:40], rt[:, 32:40], rt[:, 40:48], ALU.add, ("rt32", "rt40"), ("rt32",))
            for gi in range(4):
                ts("dve", comb[:, gi * 8:(gi + 1) * 8], rt[:, 32:40], rt[:, 12 + gi:13 + gi], None, ALU.mult, None,
                   ("rt32", "rt12"), ("comb",))
            tr(PS[7][0:32, 128:256], comb[:], identf[:], ("comb", "identf"), ("ps7",))
            cp("act", COMBT[:, tt0:tt0 + 128], PS[7][0:32, 128:256], ("ps7",), ("COMBT%d" % tcx,))

    for tcx in range(4):
        t0 = tcx * 512
        begin("M")
        dma("sp", v3(YH[:], 8), ydv[tcx], ("ydst%d" % tcx,), ("YH",), "yh")
        ts("dve", YM[:], YH[:], s01[:, 0:1], None, ALU.mult, None, ("YH", "s01"), ("YM",))
        dma("sp", v3(YH[:], 8), ydv[4 + tcx], ("ydst%d" % (4 + tcx),), ("YH",), "yh")
        stt("dve", YM[:], YH[:], s01[:, 1:2], YM[:], ALU.mult, ALU.add, ("YH", "s01", "YM"), ("YM",))
        for cc in range(8):
            sgs = []
            for ab in range(2):
                i = w3c[0] % 4
                w3c[0] += 1
                dma("sp", W3p[i][:], w3b[ab * 8 + cc].ap(), ("w3b%d" % (ab * 8 + cc),), ("W3p%d" % i,), "w3p%d" % i)
                W3pv = v3(W3p[i][:], 8)
                for kc in range(8):
                    mm(PS[ab][:, :], W3pv[:, kc, :], HXv[:, kc, t0:t0 + 512], kc == 0, kc == 7,
                       ("W3p%d" % i, "HX"), ("ps%d" % ab,))
                act(SGp[ab][:], PS[ab][:, :], AF.Sigmoid, ("ps%d" % ab,), ("SGp%d" % ab,))
            for j, fc in enumerate(FA):
                mm(PS[2][:, :], Wav[:, j, cc * 128:(cc + 1) * 128], YMv[:, fc, :], j == 0, j == 3, ("Wab", "YM"), ("ps2",))
            for j, fc in enumerate(FB):
                mm(PS[3][:, :], Wbv[:, j, cc * 128:(cc + 1) * 128], YMv[:, fc, :], j == 0, j == 3, ("Wbb", "YM"), ("ps3",))
            tt("dve", m1[:], PS[2][:, :], SGp[0][:], ALU.mult, ("ps2", "SGp0"), ("m1",))
            tt("dve", m2[:], PS[3][:, :], SGp[1][:], ALU.mult, ("ps3", "SGp1"), ("m2",))
            tt("dve", MGv[:, cc, :], m1[:], m2[:], ALU.add, ("m1", "m2"), ("MG",))
        for co in range(8):
            pbk = 4 + co % 2
            xb = xh[co % 2]
            dma("sp", xb[:], xhd[:, co, t0:t0 + 512], (), ("xh%d" % (co % 2),), "xh%d" % (co % 2))
            for cc in range(8):
                mm(PS[pbk][:, :], Wov[:, cc, co * 128:(co + 1) * 128], MGv[:, cc, :], cc == 0, cc == 7,
                   ("Wob", "MG"), ("ps%d" % pbk,))
            tt("dve", ACCv[:, co, t0:t0 + 512], PS[pbk][:, :], xb[:], ALU.add, ("ps%d" % pbk, "xh%d" % (co % 2)), ("ACC%d" % tcx,))
        for kc in range(8):
            act(sq2v[:, kc, :], ACCv[:, kc, t0:t0 + 512], AF.Square, ("ACC%d" % tcx,), ("YH",))
        for kc in range(8):
            mm(PS[6][:, :], onesb[:], sq2v[:, kc, :], kc == 0, kc == 7, ("onesb", "YH"), ("ps6",))
        act(rstd2[tcx % 2][:], PS[6][:, :], AF.Sqrt, ("ps6",), ("rstd2_%d" % (tcx % 2),), bias=EPS, scale=1.0 / D)
        recip(rstd2[tcx % 2][:], rstd2[tcx % 2][:], ("rstd2_%d" % (tcx % 2),), ("rstd2_%d" % (tcx % 2),))
        for kc in range(8):
            stt("dve", HXv[:, kc, t0:t0 + 512], ACCv[:, kc, t0:t0 + 512], gv[:, 8 + kc:9 + kc], rstd2[tcx % 2][:],
                ALU.mult, ALU.mult, ("ACC%d" % tcx, "rstd2_%d" % (tcx % 2), "gv"), ("HX",))
        end()
        if tcx > 0:
            begin("R")
            emit_routing(tcx - 1, (tcx - 1) * 512)
            end()
            merge(["M", "R"])
        else:
            merge(["M"])
    emit_routing(3, 3 * 512)
    if debug:
        dma("sp", dbg_x1.rearrange("(kc p) t -> p kc t", p=128), ACCv, tuple("ACC%d" % i for i in range(4)), ("dbgx",), "dbg")

    if stop <= 5:
        P.stopped = True
    P.barrier(["pe", "act", "dve", "pool", "sp"])
    dma("sp", gfs[:], gfin, (), ("gfs",), "c7")
    NE = OPTS["nexp"]

    def moe_load(e_):
        i = e_ % 4
        load_w2(v3(WGb[i][:], 8), wg_d[e_].rearrange("(kc p) n -> p kc n", p=128), 8, 256, "WGb%d" % i, q="sp")
        load_w2(v3(WUb[i][:], 8), wu_d[e_].rearrange("(kc p) n -> p kc n", p=128), 8, 256, "WUb%d" % i, q="sp")
        load_w2(v3(WDb[i][:], 2), wd_d[e_].rearrange("(kc p) n -> p kc n", p=128), 2, D, "WDb%d" % i, q="sp")

    def moe_prep(st):
        p, tcx = divmod(st, 4)
        t0 = tcx * 512
        for x in range(2):
            e_ = 2 * p + x
            j = (st % 2) * 2 + x
            ts("dve", cmask[j][:], COMBT[:, t0:t0 + 512], identf[0:32, e_:e_ + 1], None, ALU.mult, None,
               ("COMBT%d" % tcx, "identf"), ("cmask%d" % j,))

    def moe_gu(st):
        p, tcx = divmod(st, 4)
        t0 = tcx * 512
        for x in range(2):
            j = (st % 2) * 2 + x
            mm(PS[6 + x][:, :], onesb[0:32, :], cmask[j][:], True, True, ("onesb", "cmask%d" % j), ("ps%d" % (6 + x),))
            cp("act", CB[j][:], PS[6 + x][:, :], ("ps%d" % (6 + x),), ("CB%d" % j,))
        for x in range(2):
            e_ = 2 * p + x
            i = e_ % 4
            j = (st % 2) * 2 + x
            WGv = v3(WGb[i][:], 8)
            WUv = v3(WUb[i][:], 8)
            hdv = v3(hid[j][:], 2)
            for fc in range(2):
                for kc in range(8):
                    mm(PS[fc][:, :], WGv[:, kc, fc * 128:(fc + 1) * 128], HXv[:, kc, t0:t0 + 512], kc == 0, kc == 7,
                       ("WGb%d" % i, "HX"), ("ps%d" % fc,))
                for kc in range(8):
                    mm(PS[2 + fc][:, :], WUv[:, kc, fc * 128:(fc + 1) * 128], HXv[:, kc, t0:t0 + 512], kc == 0, kc == 7,
                       ("WUb%d" % i, "HX"), ("ps%d" % (2 + fc),))
                act(sil[fc][:], PS[fc][:, :], AF.Silu, ("ps%d" % fc,), ("sil%d" % fc,))
                tt("dve", t1b[fc][:], sil[fc][:], PS[2 + fc][:, :], ALU.mult, ("sil%d" % fc, "ps%d" % (2 + fc)), ("t1b%d" % fc,))
                tt("dve", hdv[:, fc, :], t1b[fc][:], CB[j][:], ALU.mult, ("t1b%d" % fc, "CB%d" % j), ("hid%d" % j,))

    def moe_down(st):
        p, tcx = divmod(st, 4)
        t0 = tcx * 512
        for co in range(8):
            pbk = 4 + co % 2
            k = 0
            for x in range(2):
                i = (2 * p + x) % 4
                j = (st % 2) * 2 + x
                WDv = v3(WDb[i][:], 2)
                hdv = v3(hid[j][:], 2)
                for fc in range(2):
                    mm(PS[pbk][:, :], WDv[:, fc, co * 128:(co + 1) * 128], hdv[:, fc, :], k == 0, k == 3,
                       ("WDb%d" % i, "hid%d" % j), ("ps%d" % pbk,))
                    k += 1
            tt("dve", ACCv[:, co, t0:t0 + 512], PS[pbk][:, :], ACCv[:, co, t0:t0 + 512], ALU.add,
               ("ps%d" % pbk, "ACC%d" % tcx), ("ACC%d" % tcx,))

    NP = NE // 2
    for e_ in range(min(4, NE)):
        moe_load(e_)
    moe_prep(0)
    for st in range(NP * 4):
        moe_gu(st)
        if st + 1 < NP * 4:
            moe_prep(st + 1)
        if st > 0:
            moe_down(st - 1)
        p, tcx = divmod(st, 4)
        if tcx == 0 and p >= 1 and p + 1 < NP:
            moe_load(2 * (p + 1))
            moe_load(2 * (p + 1) + 1)
    moe_down(NP * 4 - 1)

    if stop <= 6:
        P.stopped = True
    for tl in range(16):
        tcx = tl // 4
        tt0 = tl * 128
        for co in range(8):
            tr(PS[co // 4][:, (co % 4) * 128:(co % 4 + 1) * 128], ACCv[:, co, tt0:tt0 + 128], identf[:],
               ("ACC%d" % tcx, "identf"), ("ps%d" % (co // 4),))
        cp("act", xtm[:, 0:512], PS[0][:, :], ("ps0",), ("xtm",))
        cp("dve", xtm[:, 512:1024], PS[1][:, :], ("ps1",), ("xtm",))
        ms("dve", fss[:, 0:1], 0.0, ("fss",))
        act(junk[:], xtm[:], AF.Square, ("xtm", "fss"), ("hid0", "fss"), accum=fss[:, 0:1])
        act(fss[:, 1:2], fss[:, 0:1], AF.Sqrt, ("fss",), ("fss1",), bias=EPS, scale=1.0 / D)
        recip(fss[:, 2:3], fss[:, 1:2], ("fss1",), ("fss2",))
        stt("dve", otm[:], xtm[:], fss[:, 2:3], gfs[:], ALU.mult, ALU.mult, ("xtm", "fss2", "gfs"), ("otm",))
        dma("sp", out_d[tt0:tt0 + 128, :], otm[:], ("otm",), ("outd",), "out")

    keys = P.finalize()
    P.check()
    semh = {}
    for k in ("pe", "act", "dve", "pool", "sp"):
        semh[k] = es.enter_context(nc.semaphore("s_" + k))
    for k in keys:
        semh["dma:" + k] = es.enter_context(nc.semaphore("d_" + k))
    with nc.Block() as block:
        block.tensor(lambda e: P.emit_engine("pe", e, semh))
        block.scalar(lambda e: P.emit_engine("act", e, semh))
        block.vector(lambda e: P.emit_engine("dve", e, semh))
        block.gpsimd(lambda e: P.emit_engine("pool", e, semh))
        block.sync(lambda e: P.emit_engine("sp", e, semh))
    es.close()
    return nc


_C = None


def make_in_maps(inp):
    global _C
    if _C is None:
        _C = _consts()
    C = _C
    f = lambda a: np.ascontiguousarray(a, dtype=np.float32)
    x = inp["x"]
    w_in = inp["w_in"][0]
    rb = inp["rel_bias"]
    gvec = np.concatenate([inp["g_mix"][0].reshape(8, 128).T, inp["g_ffn"][0].reshape(8, 128).T], axis=1)
    gfin = np.broadcast_to(inp["g_final"][None, :], (128, D))
    wr = np.concatenate([inp["w_router_group"][0], inp["w_router_expert"][0]], axis=1)
    br = np.broadcast_to(np.concatenate([inp["b_router_group"][0], inp["b_router_expert"][0]])[None, :], (128, 36))

    def cw1_layout(w):
        return w.reshape(2, 16, 64, 128).transpose(0, 2, 1, 3).reshape(128, 16 * 128)

    def pe_layout(pe):
        return pe.reshape(2, 16, 64).transpose(0, 2, 1).reshape(128, 16)

    cw1 = np.stack([cw1_layout(inp["nsa_cmp_k_w1"][0]), cw1_layout(inp["nsa_cmp_v_w1"][0])])
    cw2 = np.stack([inp["nsa_cmp_k_w2"][0], inp["nsa_cmp_v_w2"][0]])
    cpe = np.stack([pe_layout(inp["nsa_pe_k"][0]), pe_layout(inp["nsa_pe_v"][0])])
    shared = dict(
        w3=f(w_in[:, 2856:4904]), gvec=f(gvec), gfin=f(gfin), cw1=f(cw1), cw2=f(cw2), cpe=f(cpe),
        ovl=C["ovl"], M0=C["M0"], M4=C["M4"], MW=C["MW"], KEEP=C["KEEP"], ADD=C["ADD"], EXPM=C["EXPM"],
        identb=C["identb"], identf=C["identf"], MT=C["MT"], scanmask=C["scanmask"],
        wa=f(inp["w_branch_a"][0]), wb=f(inp["w_branch_b"][0]), wo=f(inp["w_out"][0]), wr=f(wr), br=f(br),
        SELM=C["SELM"], wg=f(inp["w_exp_gate"][0]), wu=f(inp["w_exp_up"][0]), wd=f(inp["w_exp_down"][0]),
    )
    maps = []
    for core in range(8):
        b, g = core // 2, core % 2
        hs = np.arange(4 * g, 4 * g + 4)
        cols1 = np.concatenate([
            np.arange(256 * g, 256 * g + 256),
            np.tile(np.arange(512 + 64 * g, 512 + 64 * g + 64), 2),
            np.tile(np.arange(640 + 64 * g, 640 + 64 * g + 64), 2),
            np.arange(768 + 64 * g, 768 + 64 * g + 64),
            np.arange(1024 + 64 * g, 1024 + 64 * g + 64),
            np.arange(1304 + 128 * g, 1304 + 128 * g + 128),
            np.arange(1560 + 128 * g, 1560 + 128 * g + 128),
            np.arange(2328, 2344),
            np.arange(2344 + 256 * g, 2344 + 256 * g + 256),
        ])
        gate_cols = np.concatenate([1280 + brn * 8 + hs for brn in range(3)])
        cols2 = np.concatenate([
            np.arange(896 + 64 * g, 896 + 64 * g + 64),
            np.arange(1152 + 64 * g, 1152 + 64 * g + 64),
            gate_cols,
            np.arange(1816 + 256 * g, 1816 + 256 * g + 256),
        ])
        rbh = rb[:, hs]
        t0raw = np.where(C["vis0"][:, None, :], rbh[C["idx0"]].transpose(0, 2, 1), 0.0)
        t1raw = rbh[C["idx1"]].transpose(0, 2, 1)
        wraw = np.where(C["visw"][:, None, :], rbh[C["idxw"]].transpose(0, 2, 1), 0.0)
        sel = np.zeros((128, 2), np.float32)
        sel[:, g] = 1.0
        m = dict(shared)
        m.update(
            xT=f(x[b].T), xTh=f(x[b, g * HALF:(g + 1) * HALF].T), sel01=sel,
            w1=f(w_in[:, cols1]), w2=f(w_in[:, cols2]),
            t0raw=f(t0raw.reshape(128, 512)), t1raw=f(t1raw.reshape(128, 512)), wraw=f(wraw.reshape(128, 4 * 504)),
            b31=f(np.broadcast_to(rbh[31][None, :], (128, 4))),
            wal=f(inp["gla_w_alpha"][0][:, 128 * g:128 * g + 128]),
            nbal=f(-inp["gla_b_alpha"][0][128 * g:128 * g + 128].reshape(2, 64).T),
            ng=f(inp["gla_norm_g"][0][256 * g:256 * g + 256].reshape(2, 128).T),
        )
        maps.append(m)
    return maps


_NC = {}


def kernel(**inputs):
    inp = {k: np.asarray(v) for k, v in inputs.items()}
    if "nc" not in _NC:
        _NC["nc"] = build_nc(False)
    nc = _NC["nc"]
    maps = make_in_maps(inp)
    res = run_bass_kernel_spmd(nc, maps, core_ids=list(range(8)))
    out = np.zeros((4, S, D), np.float32)
    for core in range(8):
        b, g = core // 2, core % 2
        out[b, g * HALF:(g + 1) * HALF] = res.results[core]["out"]
    return out
```

```python
import math
from contextlib import ExitStack
import numpy as np
import ml_dtypes
import concourse.bass as bass
import concourse.mybir as mybir
from concourse.bass_utils import run_bass_kernel_spmd

F32 = mybir.dt.float32
BF16 = mybir.dt.bfloat16
AF = mybir.ActivationFunctionType
ALU = mybir.AluOpType
AX = mybir.AxisListType
BF = ml_dtypes.bfloat16

S = 4096
D = 1024
NB = 32
HALF = 2048
NEG = -30000.0
EPS = 1e-6
N_EXP = 32
W1C = 1168
W2C = 396
GC1 = 0.7978845608028654
GC2 = 0.044715


class Prog:
    def __init__(self):
        self.ops = []
        self.lw = {}
        self.rd = {}

    stopped = False

    def add(self, eng, fn, reads=(), writes=(), dma=None, inc=16):
        if self.stopped:
            return -1
        i = len(self.ops)
        reads = tuple(reads)
        writes = tuple(writes) + tuple(k for k in reads if k.startswith("ps") and k not in writes)
        deps = {}
        for k in reads:
            w = self.lw.get(k)
            if w is not None:
                deps[w] = True
        for k in writes:
            w = self.lw.get(k)
            if w is not None:
                deps.setdefault(w, False)
            for r in self.rd.get(k, ()):
                deps.setdefault(r, False)
        for k in writes:
            self.lw[k] = i
            self.rd[k] = []
        for k in reads:
            if k not in writes:
                self.rd.setdefault(k, []).append(i)
        self.ops.append(dict(eng=eng, fn=fn, deps=deps, dma=dma, need=False, waits=[], inc=inc))
        return i

    def barrier(self, engines):
        if self.stopped:
            return
        last = {}
        lastd = {}
        for i, o in enumerate(self.ops):
            last[o["eng"]] = i
            if o["dma"] is not None:
                lastd[o["dma"]] = i
        deps = {i: True for i in list(last.values()) + list(lastd.values())}
        for e in engines:
            self.ops.append(dict(eng=e, fn=(lambda en: en.nop()), deps=dict(deps), dma=None,
                                 need=False, waits=[], bar=True, inc=16))

    def finalize(self):
        ops = self.ops
        for i, o in enumerate(ops):
            for j, raw in o["deps"].items():
                pj = ops[j]
                if pj["dma"] is None and o["dma"] is None and pj["eng"] == o["eng"] and not o.get("bar"):
                    if o["eng"] == "pe":
                        continue
                pj["need"] = True
                o["waits"].append(j)
        cnt = {}
        dcnt = {}
        for o in ops:
            ws = []
            for j in sorted(o["waits"]):
                pj = ops[j]
                if pj["dma"] is not None:
                    ws.append(("dma:" + pj["dma"], dcnt[pj["dma"]]))
                else:
                    ws.append(pj["sig"])
            o["ws"] = ws
            if o["dma"] is not None:
                dcnt[o["dma"]] = dcnt.get(o["dma"], 0) + o["inc"]
                o["sig"] = ("dma:" + o["dma"], dcnt[o["dma"]])
            elif o["need"]:
                cnt[o["eng"]] = cnt.get(o["eng"], 0) + 1
                o["sig"] = (o["eng"], cnt[o["eng"]])
        self.dcnt = dcnt
        print('SEM counts', cnt, 'n_dma_keys', len(dcnt))
        return sorted(dcnt.keys())

    def check(self):
        ops = self.ops
        q = {}
        for i, o in enumerate(ops):
            q.setdefault(o["eng"], []).append(i)
        pos = {e: 0 for e in q}
        sem = {}
        prog = True
        n_done = 0
        while prog:
            prog = False
            for e, lst in q.items():
                while pos[e] < len(lst):
                    o = ops[lst[pos[e]]]
                    if all(sem.get(s_, 0) >= v_ for s_, v_ in o["ws"]):
                        if o["dma"] is not None:
                            k_ = "dma:" + o["dma"]
                            sem[k_] = sem.get(k_, 0) + o["inc"]
                        elif o["need"]:
                            sem[e] = sem.get(e, 0) + 1
                        pos[e] += 1
                        n_done += 1
                        prog = True
                    else:
                        break
        stuck = {e: (pos[e], len(lst)) for e, lst in q.items() if pos[e] < len(lst)}
        if stuck:
            for e, (p_, n_) in stuck.items():
                o = ops[q[e][p_]]
                print("STUCK", e, p_, n_, [(s_, v_, sem.get(s_, 0)) for s_, v_ in o["ws"] if sem.get(s_, 0) < v_])
        else:
            print("protocol check OK", n_done)
        return not stuck

    def emit_engine(self, ename, e, semh):
        ops = self.ops
        seen = {}
        for o in ops:
            if o["eng"] != ename:
                continue
            for s, v in o["ws"]:
                if seen.get(s, 0) >= v:
                    continue
                seen[s] = v
                e.wait_ge(semh[s], v)
            ins = o["fn"](e)
            if o["dma"] is not None:
                ins.then_inc(semh["dma:" + o["dma"]], o["inc"])
            elif o["need"]:
                ins.then_inc(semh[ename], 1)
        if ename == "sp":
            for k, v in self.dcnt.items():
                if seen.get("dma:" + k, 0) < v:
                    e.wait_ge(semh["dma:" + k], v)


def _bucket(rel):
    n = np.maximum(rel, 0)
    nf = np.maximum(n, 1).astype(np.float32)
    large = 16 + (np.log(nf / np.float32(16)) / np.float32(math.log(8.0)) * np.float32(16)).astype(np.int32)
    return np.where(n < 16, n, np.minimum(large, 31))


def _consts():
    c = {}
    k = np.arange(128)[:, None]
    q = np.arange(128)[None, :]
    c["idx0"] = _bucket(q - k)
    c["vis0"] = (q >= k)
    c["idx1"] = _bucket(128 + q - k)
    c["M0"] = np.where(q >= k, 0.0, NEG).astype(np.float32)
    c["M4"] = np.where(k > q, 0.0, NEG).astype(np.float32)
    qq = np.arange(128)[:, None]
    dd = np.arange(504)[None, :] - 248
    relw = qq - 16 * dd - 31
    c["idxw"] = _bucket(relw)
    c["visw"] = relw >= 0
    c["MW"] = np.where(relw >= 0, 0.0, NEG).astype(np.float32)
    jj = np.arange(128)[None, :] - 64
    tbr = (np.arange(128)[:, None] >= 64).astype(np.int64)
    forced = (jj == tbr) | (jj == tbr - 1)
    future = jj > tbr
    c["KEEP"] = np.where(forced | future, 0.0, 1.0).astype(np.float32)
    c["ADD"] = np.where(forced, 1e30, np.where(future, -1e30, 0.0)).astype(np.float32)
    c["EXPM"] = (np.arange(S)[None, :] // 64 == np.arange(64)[:, None]).astype(BF)
    c["identb"] = np.eye(128).astype(BF)
    c["identf"] = np.eye(128).astype(np.float32)
    c["MT"] = ((k // 64 == q // 64) & (k <= q)).astype(np.float32)
    sm = np.ones((64, 512), np.float32)
    sm[:, ::64] = 0.0
    c["scanmask"] = sm
    ci = np.arange(256)[:, None] * 16
    sj = np.arange(64)[None, :] * 64
    ov = ((ci < sj + 64) & (ci + 32 > sj)).astype(np.float32)
    ov[255] = 0.0
    c["ovl"] = np.ascontiguousarray(ov.reshape(2, 128, 64).transpose(1, 0, 2)).astype(BF)
    selm = np.zeros((32, 32, 128), np.float32)
    for e in range(32):
        selm[e, e, :] = 1.0
    c["SELM"] = selm.reshape(32, 32 * 128)
    return c


OPTS = dict(nch=8, sub=99, nexp=32)


def build_nc(debug=False, stop=99):
    nc = bass.Bass("TRN2", target_bir_lowering=False)
    P = Prog()
    din = {}

    def dram_in(name, shape, dt=F32):
        din[name] = nc.dram_tensor(name, list(shape), dt, kind="ExternalInput").ap()
        return din[name]

    xT = dram_in("xT", [D, S])
    xTh = dram_in("xTh", [D, HALF])
    sel01 = dram_in("sel01", [128, 2])
    w1 = dram_in("w1", [D, W1C])
    w2 = dram_in("w2", [D, W2C])
    w3 = dram_in("w3", [D, 2048])
    gvec = dram_in("gvec", [128, 16])
    gfin = dram_in("gfin", [128, D])
    cw1 = dram_in("cw1", [2, 128, 16 * 128])
    cw2 = dram_in("cw2", [2, 128, 64])
    cpe = dram_in("cpe", [2, 128, 16])
    ovl_d = dram_in("ovl", [128, 2, 64], BF16)
    t0raw = dram_in("t0raw", [128, 512])
    t1raw = dram_in("t1raw", [128, 512])
    wraw = dram_in("wraw", [128, 4 * 504])
    m0_d = dram_in("M0", [128, 128])
    m4_d = dram_in("M4", [128, 128])
    mw_d = dram_in("MW", [128, 504])
    b31_d = dram_in("b31", [128, 4])
    keep_d = dram_in("KEEP", [128, 128])
    add_d = dram_in("ADD", [128, 128])
    expm_d = dram_in("EXPM", [64, S], BF16)
    identb_d = dram_in("identb", [128, 128], BF16)
    identf_d = dram_in("identf", [128, 128])
    mt_d = dram_in("MT", [128, 128])
    scanm_d = dram_in("scanmask", [64, 512])
    wal_d = dram_in("wal", [16, 128])
    nbal_d = dram_in("nbal", [64, 2])
    ng_d = dram_in("ng", [128, 2])
    wa_d = dram_in("wa", [512, D])
    wb_d = dram_in("wb", [512, D])
    wo_d = dram_in("wo", [D, D])
    wr_d = dram_in("wr", [D, 36])
    br_d = dram_in("br", [128, 36])
    selm_d = dram_in("SELM", [32, 32 * 128])
    wg_d = dram_in("wg", [OPTS["nexp"], D, 256])
    wu_d = dram_in("wu", [OPTS["nexp"], D, 256])
    wd_d = dram_in("wd", [OPTS["nexp"], 256, D])
    out_d = nc.dram_tensor("out", [HALF, D], F32, kind="ExternalOutput").ap()
    ysrc = [nc.dram_tensor("ysrc%d" % i, [512, 512], BF16) for i in range(8)]
    ydst = [nc.dram_tensor("ydst%d" % i, [1024, 512], BF16) for i in range(8)]
    w3b = [nc.dram_tensor("w3b%d" % i, [128, 8 * 128], BF16, **(dict(kind="ExternalOutput") if debug else {})) for i in range(16)]
    _k = dict(kind="ExternalOutput") if debug else {}
    wab_s = nc.dram_tensor("wab_s", [128, 4 * D], BF16, **_k)
    wbb_s = nc.dram_tensor("wbb_s", [128, 4 * D], BF16, **_k)
    wob_s = nc.dram_tensor("wob_s", [128, 8 * D], BF16, **_k)
    if debug:
        dbg_y = nc.dram_tensor("dbg_y", [1024, S], BF16, kind="ExternalOutput").ap()
        dbg_x1 = nc.dram_tensor("dbg_x1", [D, HALF], F32, kind="ExternalOutput").ap()
        dbg_br = nc.dram_tensor("dbg_br", [3, S, 256], F32, kind="ExternalOutput").ap()

    SB0 = 16512
    sboff = {}
    off = [SB0]

    def sb(name, shape, dt, at=None):
        nbytes = int(np.prod(shape[1:])) * (4 if dt == F32 else 2)
        nbytes = (nbytes + 63) // 64 * 64
        if at is None:
            o = off[0]
            off[0] += nbytes
        else:
            o = at
        assert o + nbytes <= 229344, (name, o, nbytes)
        sboff[name] = o
        return nc.alloc_sbuf_tensor_at(name, list(shape), dt, offset=o)

    BIG = sb("BIG", [128, 16384], F32)
    hT = nc.alloc_sbuf_tensor_at("hT", [128, 8 * S], BF16, offset=SB0)
    identb = sb("identb", [128, 128], BF16)
    identf = sb("identf", [128, 128], F32)
    onesb = sb("onesb", [128, 128], BF16)
    gv = sb("gv", [128, 16], F32)
    s01 = sb("s01", [128, 2], F32)
    common_end = off[0]

    W1b = sb("W1b", [128, 8 * W1C], BF16)
    W2b = sb("W2b", [128, 8 * W2C], BF16)
    KE = sb("KE", [128, S], BF16)
    KwT = sb("KwT", [64, S], BF16)
    VS = sb("VS", [128, NB * 65], BF16)
    VW = sb("VW", [128, NB * 65], BF16)
    cw2b = [sb("cw2b%d" % i, [128, 64], BF16) for i in range(2)]
    cconst = sb("cconst", [128, 2], F32)
    kcbT = sb("kcbT", [64, 256], BF16)
    VAUG = sb("VAUG", [128, 2 * 128], BF16)
    T0 = sb("T0", [128, 512], BF16)
    T1 = sb("T1", [128, 512], BF16)
    T4 = sb("T4", [128, 512], BF16)
    Wt = sb("Wt", [128, 4 * 504], F32)
    b31 = sb("b31", [128, 4], F32)
    KEEP = sb("KEEP", [128, 128], F32)
    ADDT = sb("ADDT", [128, 128], F32)
    MTs = sb("MTs", [128, 128], F32)
    scanm = sb("scanm", [64, 512], F32)
    walb = sb("walb", [16, 128], BF16)
    nbal = sb("nbal", [64, 2], F32)
    ngs = sb("ngs", [128, 2], F32)
    GS = sb("GS", [128, NB * 12], F32)
    r_start = off[0]
    wst = [sb("wst%d" % i, [128, 8 * 256], F32) for i in range(2)]
    xs = wst
    sqb = sb("sqb", [128, 8 * 256], BF16)
    rstd = sb("rstd", [128, 256], F32)
    kcT2 = sb("kcT2", [128, S + 16], BF16)
    vcT2 = sb("vcT2", [128, S + 16], BF16)
    cw1b = [sb("cw1b%d" % i, [128, 16 * 128], BF16) for i in range(2)]
    cpeb = [sb("cpeb%d" % i, [128, 16], BF16) for i in range(2)]
    cx = sb("cx", [128, 256], F32)
    ct1 = sb("ct1", [128, 256], F32)
    ct2 = sb("ct2", [128, 256], F32)
    chid = [sb("chid%d" % i, [128, 256], BF16) for i in range(2)]
    tmpT = sb("tmpT", [128, 4 * 504], F32)
    MWs = sb("MWs", [128, 504], F32)
    M0s = sb("M0s", [128, 128], F32)
    M4s = sb("M4s", [128, 128], F32)
    walf = sb("walf", [16, 128], F32)
    r1_end = off[0]
    off[0] = r_start
    QN = [sb("QN%d" % i, [128, 4 * 512], BF16) for i in range(2)]
    VB = sb("VB", [128, 4 * 256], BF16)
    scc = sb("scc", [128, 256], F32)
    ecb = sb("ecb", [128, 256], BF16)
    eT = sb("eT", [128, 256], BF16)
    ssum = sb("ssum", [128, 4], F32)
    rr = sb("rr", [128, 4], F32)
    coef = sb("coef", [128, 4], F32)
    ya = sb("ya", [128, 256], F32)
    yab = sb("yab", [128, 256], BF16)
    imp = sb("imp", [128, 64], F32)
    impm = sb("impm", [128, 64], F32)
    imp2 = sb("imp2", [128, 64], F32)
    m8 = sb("m8", [128, 16], F32)
    negp = sb("negp", [128, 128], BF16)
    PT = [sb("PT%d" % i, [128, 512], BF16) for i in range(4)]
    yw = sb("yw", [128, 256], F32)
    yc = [sb("yc%d" % i, [128, 256], F32) for i in range(2)]
    rrC = sb("rrC", [128, 4], F32)
    coefC = sb("coefC", [128, 4], F32)
    rrB = sb("rrB", [128, 4], F32)
    coefB = sb("coefB", [128, 4], F32)
    ngs2 = sb("ngs2", [128, 2], F32)
    gl1 = sb("gl1", [128, 128], F32)
    YT = [sb("YT%d" % i, [128, 4 * 512], BF16) for i in range(2)]
    gq = sb("gq", [64, 2 * 512], F32)
    gk = sb("gk", [64, 2 * 512], F32)
    gab = sb("gab", [16, 512], BF16)
    ge1 = sb("ge1", [64, 2 * 512], F32)
    gbc = sb("gbc", [64, 2 * 512], F32)
    gAq = sb("gAq", [64, 2 * 512], F32)
    gAk = sb("gAk", [64, 2 * 512], F32)
    qin = sb("qin", [64, 2 * 512], BF16)
    kin = sb("kin", [64, 2 * 512], BF16)
    kdT = sb("kdT", [64, 2 * 512], BF16)
    gdec = sb("gdec", [64, 2 * 8], F32)
    rTs = sb("rTs", [128, 2 * 512], BF16)
    ATb = [sb("ATb%d" % i, [128, 128], BF16) for i in range(2)]
    kdtok = [sb("kdtok%d" % i, [128, 64], BF16) for i in range(2)]
    St = [sb("St%d" % i, [64, 128], F32) for i in range(2)]
    Sbf = [[sb("Sbf%d_%d" % (i, j), [64, 128], BF16) for j in range(2)] for i in range(2)]
    gsq = sb("gsq", [128, 128], BF16)
    grs = sb("grs", [128, 128], F32)
    gyb = sb("gyb", [128, 128], F32)
    ybr = [sb("ybr%d" % i, [128, 256], F32) for i in range(2)] if debug else None
    off[0] = max(off[0], r1_end)
    cvf = [sb("cvf%d" % i, [128, 512], F32) for i in range(2)]
    cvb = [sb("cvb%d" % i, [128, 512], BF16) for i in range(2)]
    p1_end = off[0]

    off[0] = common_end
    HX = sb("HX", [128, 8 * HALF], BF16)
    COMBT = sb("COMBT", [32, HALF], F32)
    WRf = sb("WRf", [128, 8 * 36], F32)
    brs = sb("brs", [128, 36], F32)
    onesf = sb("onesf", [32, 128], F32)
    rt = sb("rt", [128, 64], F32)
    Lg = sb("Lg", [128, 36], F32)
    comb = sb("comb", [128, 32], F32)
    esel = sb("esel", [128, 8], F32)
    wst2 = [sb("wst2_%d" % i, [128, 8 * 256], F32) for i in range(2)]
    pm_start = off[0]
    Wab = sb("Wab", [128, 4 * D], BF16)
    Wbb = sb("Wbb", [128, 4 * D], BF16)
    Wob = sb("Wob", [128, 8 * D], BF16)
    W3p = [sb("W3p%d" % i, [128, 8 * 128], BF16) for i in range(4)]
    w3cv = [sb("w3cv%d" % i, [128, 8 * 128], BF16) for i in range(2)]
    YM = sb("YM", [128, 8 * 512], BF16)
    YH = sb("YH", [128, 8 * 512], BF16)
    SGp = [sb("SGp%d" % i, [128, 512], BF16) for i in range(2)]
    MG = sb("MG", [128, 8 * 512], BF16)
    m1 = sb("m1", [128, 512], F32)
    m2 = sb("m2", [128, 512], F32)
    xh = [sb("xh%d" % i, [128, 512], F32) for i in range(2)]
    sq2 = YH
    rstd2 = [sb("rstd2_%d" % i, [128, 512], F32) for i in range(2)]
    assert sboff["m2"] == sboff["m1"] + 2048 and sboff["xh1"] == sboff["xh0"] + 2048
    w3st = [nc.alloc_sbuf_tensor_at("w3st0", [128, 8 * 128], F32, offset=sboff["m1"]),
            nc.alloc_sbuf_tensor_at("w3st1", [128, 8 * 128], F32, offset=sboff["xh0"])]
    w3k = [("m1", "m2"), ("xh0", "xh1")]
    pm_end = off[0]
    off[0] = pm_start
    WGb = [sb("WGb%d" % i, [128, 8 * 256], BF16) for i in range(4)]
    WUb = [sb("WUb%d" % i, [128, 8 * 256], BF16) for i in range(4)]
    WDb = [sb("WDb%d" % i, [128, 2 * D], BF16) for i in range(4)]
    CB = [sb("CB%d" % i, [128, 512], BF16) for i in range(4)]
    cmask = [sb("cmask%d" % i, [32, 512], BF16) for i in range(4)]
    sil = [sb("sil%d" % i, [128, 512], BF16) for i in range(2)]
    t1b = [sb("t1b%d" % i, [128, 512], BF16) for i in range(2)]
    hid = [sb("hid%d" % i, [128, 2 * 512], BF16) for i in range(4)]
    xtm = sb("xtm", [128, D], F32)
    otm = sb("otm", [128, D], F32)
    gfs = sb("gfs", [128, D], F32)
    fss = sb("fss", [128, 4], F32)
    junk = hid[0]
    off[0] = max(off[0], pm_end)

    print('SBUF usage', r1_end, pm_end, off[0])
    es = ExitStack()
    PS = [es.enter_context(nc.psum_tensor("ps%d" % i, [128, 512], F32)) for i in range(8)]

    streams = {}
    cur = [None]

    def A(eng, fn, r=(), w=(), dma=None):
        if cur[0] is None:
            return P.add(eng, fn, r, w, dma)
        streams[cur[0]].append((eng, fn, r, w, dma))

    def begin(name):
        cur[0] = name
        streams.setdefault(name, [])

    def end():
        cur[0] = None

    def merge(names):
        lists = [streams.pop(n_) for n_ in names]
        idx = [0] * len(lists)
        tot = [max(1, len(l_)) for l_ in lists]
        while True:
            best = None
            for i_, l_ in enumerate(lists):
                if idx[i_] < len(l_):
                    f_ = (idx[i_] + 1) / tot[i_]
                    if best is None or f_ < best[0]:
                        best = (f_, i_)
            if best is None:
                break
            i_ = best[1]
            P.add(*lists[i_][idx[i_]])
            idx[i_] += 1

    ckn = [0]

    def dma(q, out, in_, r, w, key):
        if len(key) == 2 and key[0] == "c" and key[1].isdigit():
            ckn[0] += 1
            key = "k%d" % ckn[0]
        A(q, lambda e, out=out, in_=in_: e.dma_start(out=out, in_=in_), r, w, dma=key)

    def mm(out, lhsT, rhs, start, stop, r, w, sgc=False):
        A("pe", lambda e, out=out, lhsT=lhsT, rhs=rhs, start=start, stop=stop, sgc=sgc:
          e.matmul(out, lhsT=lhsT, rhs=rhs, start=start, stop=stop, skip_group_check=sgc), r, w)

    def tr(out, in_, ident, r, w):
        A("pe", lambda e, out=out, in_=in_, ident=ident: e.transpose(out=out, in_=in_, identity=ident), r, w)

    def act(out, in_, func, r, w, bias=None, scale=None, accum=None):
        def f(e, out=out, in_=in_, func=func, bias=bias, scale=scale, accum=accum):
            kw = {}
            if bias is not None:
                kw["bias"] = bias
            if scale is not None:
                kw["scale"] = scale
            if accum is not None:
                kw["accum_out"] = accum
            return e.activation(out=out, in_=in_, func=func, **kw)
        A("act", f, r, w)

    def tt(eng, out, in0, in1, op, r, w):
        A(eng, lambda e, out=out, in0=in0, in1=in1, op=op: e.tensor_tensor(out=out, in0=in0, in1=in1, op=op), r, w)

    def ts(eng, out, in0, s1, s2, op0, op1, r, w):
        def f(e, out=out, in0=in0, s1=s1, s2=s2, op0=op0, op1=op1):
            if op1 is None:
                return e.tensor_scalar(out=out, in0=in0, scalar1=s1, scalar2=None, op0=op0)
            return e.tensor_scalar(out=out, in0=in0, scalar1=s1, scalar2=s2, op0=op0, op1=op1)
        A(eng, f, r, w)

    def stt(eng, out, in0, scalar, in1, op0, op1, r, w):
        A(eng, lambda e, out=out, in0=in0, scalar=scalar, in1=in1, op0=op0, op1=op1:
          e.scalar_tensor_tensor(out=out, in0=in0, scalar=scalar, in1=in1, op0=op0, op1=op1), r, w)

    def cp(eng, out, in_, r, w):
        if eng == "act":
            A("act", lambda e, out=out, in_=in_: e.copy(out=out, in_=in_), r, w)
        else:
            A(eng, lambda e, out=out, in_=in_: e.tensor_copy(out=out, in_=in_), r, w)

    def ms(eng, ap, val, w):
        A(eng, lambda e, ap=ap, val=val: e.memset(ap, val), (), w)

    def recip(out, in_, r, w):
        A("dve", lambda e, out=out, in_=in_: e.reciprocal(out=out, in_=in_), r, w)

    def v3(ap, a):
        return ap.rearrange("p (a b) -> p a b", a=a)

    dma("sp", identb[:], identb_d, (), ("identb",), "c0")
    dma("sp", identf[:], identf_d, (), ("identf",), "c0")
    dma("sp", gv[:], gvec, (), ("gv",), "c0")
    dma("sp", s01[:], sel01, (), ("s01",), "c0")
    ms("dve", onesb[:], 1.0, ("onesb",))
    dma("sp", tmpT[:, 0:512], t0raw, (), ("tmpT",), "c1")
    dma("sp", M0s[:], m0_d, (), ("M0s",), "c1")
    dma("sp", M4s[:], m4_d, (), ("M4s",), "c1")
    dma("sp", b31[:], b31_d, (), ("b31",), "c1")
    for h in range(4):
        ts("dve", tmpT[:, h * 128:(h + 1) * 128], tmpT[:, h * 128:(h + 1) * 128], b31[:, h:h + 1], None,
           ALU.subtract, None, ("tmpT", "b31"), ("tmpT",))
        tt("dve", T0[:, h * 128:(h + 1) * 128], tmpT[:, h * 128:(h + 1) * 128], M0s[:], ALU.add,
           ("tmpT", "M0s"), ("T0",))
        cp("dve", T4[:, h * 128:(h + 1) * 128], M4s[:], ("M4s",), ("T4",))
    dma("sp", tmpT[:, 0:512], t1raw, ("T0",), ("tmpT",), "c2")
    for h in range(4):
        ts("dve", T1[:, h * 128:(h + 1) * 128], tmpT[:, h * 128:(h + 1) * 128], b31[:, h:h + 1], None,
           ALU.subtract, None, ("tmpT", "b31"), ("T1",))
    dma("sp", tmpT[:], wraw, ("T1",), ("tmpT",), "c3")
    dma("sp", MWs[:], mw_d, (), ("MWs",), "c3")
    for h in range(4):
        ts("dve", tmpT[:, h * 504:(h + 1) * 504], tmpT[:, h * 504:(h + 1) * 504], b31[:, h:h + 1], None,
           ALU.subtract, None, ("tmpT", "b31"), ("tmpT",))
        tt("dve", Wt[:, h * 504:(h + 1) * 504], tmpT[:, h * 504:(h + 1) * 504], MWs[:], ALU.add,
           ("tmpT", "MWs"), ("Wt",))
    dma("sp", KEEP[:], keep_d, (), ("KEEP",), "c4")
    dma("sp", ADDT[:], add_d, (), ("ADDT",), "c4")
    dma("sp", MTs[:], mt_d, (), ("MTs",), "c4")
    dma("sp", scanm[:], scanm_d, (), ("scanm",), "c4")
    dma("sp", walf[:], wal_d, (), ("walf",), "c4")
    cp("dve", walb[:], walf[:], ("walf",), ("walb",))
    dma("sp", nbal[:], nbal_d, (), ("nbal",), "c4")
    dma("sp", ngs[:], ng_d, (), ("ngs",), "c4")
    dma("sp", KE[64:128, :], expm_d, (), ("KEexp",), "c4")
    dma("sp", v3(VAUG[:], 2)[:, :, 64:128], ovl_d, (), ("VAUGo",), "c4")
    ms("dve", v3(VS[:], NB)[:, :, 64:65], 1.0, ("VSone",))
    ms("dve", v3(VW[:], NB)[:, :, 64:65], 1.0, ("VWone",))
    ms("dve", v3(VAUG[:], 2)[:, :, 0:64], 0.0, ("VAUGv",))
    ms("dve", kcbT[:], 0.0, ("kcbT",))

    wcnt = [0]

    def load_w(dst, src, ncols, gofs, key, dstkey):
        dv = v3(dst[:], 8)
        sv = src.rearrange("(kc p) n -> p kc n", p=128)
        c0 = 0
        while c0 < ncols:
            cw = min(256, ncols - c0)
            i = wcnt[0] % 2
            wcnt[0] += 1
            st = v3(wst[i][:], 8)
            dma("sp", st[:, :, 0:cw], sv[:, :, c0:c0 + cw], (), ("wst%d" % i,), "w%d" % i)
            cp("act" if (wcnt[0] % 2) else "pool", dv[:, :, c0:c0 + cw], st[:, :, 0:cw], ("wst%d" % i,), (dstkey,))
            c0 += cw

    load_w(W1b, w1, W1C, 0, "w", "W1b")
    load_w(W2b, w2, W2C, 0, "w", "W2b")
    for i in range(2):
        dma("sp", wst[i][:, 0:2048], cw1[i], ("W1b", "W2b"), ("wst%d" % i,), "w%d" % i)
        cp("dve", cw1b[i][:], wst[i][:, 0:2048], ("wst%d" % i,), ("cw1b%d" % i,))
        dma("sp", ct1[:, 0:64], cw2[i], ("cw2b%d" % (i - 1),) if i else (), ("ct1",), "c5")
        cp("dve", cw2b[i][:], ct1[:, 0:64], ("ct1",), ("cw2b%d" % i,))
        dma("sp", ct2[:, 0:16], cpe[i], ("cpeb%d" % (i - 1),) if i else (), ("ct2",), "c5")
        cp("dve", cpeb[i][:], ct2[:, 0:16], ("ct2",), ("cpeb%d" % i,))

    W1v = v3(W1b[:], 8)
    W2v = v3(W2b[:], 8)
    hTv = v3(hT[:], 8)
    if stop <= 0:
        P.stopped = True
    for c in range(16):
        t0 = c * 256
        i = c % 2
        xv = v3(xs[i][:], 8)
        dma("sp", xv, xT.rearrange("(kc p) t -> p kc t", p=128)[:, :, t0:t0 + 256],
            (), ("wst%d" % i,), "w%d" % i)
        act(sqb[:], xs[i][:], AF.Square, ("wst%d" % i,), ("sqb",))
        sqv = v3(sqb[:], 8)
        for kc in range(8):
            mm(PS[0][:, 0:256], onesb[:], sqv[:, kc, :], kc == 0, kc == 7, ("onesb", "sqb"), ("ps0",))
        act(rstd[:], PS[0][:, 0:256], AF.Sqrt, ("ps0",), ("rstd",), bias=EPS, scale=1.0 / D)
        recip(rstd[:], rstd[:], ("rstd",), ("rstd",))
        for kc in range(8):
            stt("dve", hTv[:, kc, t0:t0 + 256], xv[:, kc, :], gv[:, kc:kc + 1], rstd[:],
                ALU.mult, ALU.mult, ("wst%d" % i, "rstd", "gv"), ("hT%d" % (c // 2),))
        if c % 2 == 1:
            ch = c // 2
            tc0 = ch * 512
            for which, (dstT, col0) in enumerate(((kcT2, 256), (vcT2, 384))):
                pb = PS[1 + which]
                for kc in range(8):
                    mm(pb[:, :], W1v[:, kc, col0:col0 + 128], hTv[:, kc, tc0:tc0 + 512], kc == 0, kc == 7,
                       ("W1b", "hT%d" % ch), ("ps%d" % (1 + which),))
                nm = "kcT2" if which == 0 else "vcT2"
                cp("act", dstT[0:64, 16 + tc0:16 + tc0 + 512], pb[0:64, :], ("ps%d" % (1 + which),), (nm,))
                cp("dve", dstT[64:128, tc0:tc0 + 512], pb[64:128, :], ("ps%d" % (1 + which),), (nm,))

    if stop <= 1:
        P.stopped = True
    for i in range(2):
        srcT = kcT2 if i == 0 else vcT2
        nm = "kcT2" if i == 0 else "vcT2"
        w1v_ = v3(cw1b[i][:], 16)
        for l in range(16):
            mm(PS[0][:, 0:1], w1v_[:, l, :], cpeb[i][:, l:l + 1], l == 0, l == 15,
               ("cw1b%d" % i, "cpeb%d" % i), ("ps0",))
        cp("dve", cconst[:, i:i + 1], PS[0][:, 0:1], ("ps0",), ("cconst",))
        for l in range(16):
            mm(PS[1][:, 0:255], w1v_[:, l, :], srcT[:, 16 + l:16 + l + 16 * 255:16], l == 0, l == 15,
               ("cw1b%d" % i, nm), ("ps1",))
        act(cx[:, 0:255], PS[1][:, 0:255], AF.Identity, ("ps1", "cconst"), ("cx",), bias=cconst[:, i:i + 1])
        tt("dve", ct1[:, 0:255], cx[:, 0:255], cx[:, 0:255], ALU.mult, ("cx",), ("ct1",))
        ts("dve", ct1[:, 0:255], ct1[:, 0:255], GC2, 1.0, ALU.mult, ALU.add, ("ct1",), ("ct1",))
        tt("dve", ct1[:, 0:255], ct1[:, 0:255], cx[:, 0:255], ALU.mult, ("ct1", "cx"), ("ct1",))
        act(ct2[:, 0:255], ct1[:, 0:255], AF.Sigmoid, ("ct1",), ("ct2",), scale=2.0 * GC1)
        tt("dve", chid[i][:, 0:255], cx[:, 0:255], ct2[:, 0:255], ALU.mult, ("cx", "ct2"), ("chid%d" % i,))
    mm(PS[2][0:64, 0:255], cw2b[0][:], chid[0][:, 0:255], True, True, ("cw2b0", "chid0"), ("ps2",))
    cp("act", kcbT[:, 0:255], PS[2][0:64, 0:255], ("ps2", "kcbT"), ("kcbT",))
    VAv = v3(VAUG[:], 2)
    mm(PS[3][:, 0:64], chid[1][:, 0:128], cw2b[1][:], True, True, ("cw2b1", "chid1"), ("ps3",))
    cp("act", VAv[:, 0, 0:64], PS[3][:, 0:64], ("ps3", "VAUGv"), ("VAUGv",))
    mm(PS[3][0:127, 64:128], chid[1][:, 128:255], cw2b[1][:], True, True, ("cw2b1", "chid1"), ("ps3",))
    cp("act", VAv[0:127, 1, 0:64], PS[3][0:127, 64:128], ("ps3", "VAUGv"), ("VAUGv",))

    if stop <= 2:
        P.stopped = True
    P.barrier(["pe", "act", "dve", "pool", "sp"])
    ms("dve", negp[:, 0:64], 0.0, ("negp0",))
    for i in range(2):
        ms("dve", St[i][:], 0.0, ("St%d" % i,))
        ms("dve", Sbf[i][0][:], 0.0, ("Sbf%d_0" % i,))
    VSv = v3(VS[:], NB)
    VWv = v3(VW[:], NB)
    GSv = v3(GS[:], NB)
    VBv = v3(VB[:], 4)
    Wtv = v3(Wt[:], 4)
    psb5 = PS[4][:, 320:512].bitcast(BF16)
    psb6 = PS[6][:, 384:512].bitcast(BF16)
    psb7 = PS[7][:, 128:160].bitcast(BF16)

    def fm_proj(col0, m, pbank, tc0, rkeys):
        for kc in range(8):
            mm(PS[pbank][0:m, :], W1v[:, kc, col0:col0 + m], hTv[:, kc, tc0:tc0 + 512], kc == 0, kc == 7,
               ("W1b",) + rkeys, ("ps%d" % pbank,))

    sidx = [0, 0]
    w3v = w3.rearrange("(kc p) n -> p kc n", p=128)
    cvjobs = []
    for c16 in range(16):
        for hf in range(2):
            cvjobs.append((w3v[:, hf * 4:hf * 4 + 4, c16 * 128:(c16 + 1) * 128], 4,
                           w3b[c16].ap()[:, hf * 512:(hf + 1) * 512], "w3b%d" % c16))
    for wsrc, wdst, wkey, nkc in ((wa_d, wab_s, "wab_s", 4), (wb_d, wbb_s, "wbb_s", 4), (wo_d, wob_s, "wob_s", 8)):
        for kc in range(nkc):
            for hf in range(2):
                cvjobs.append((wsrc[kc * 128:(kc + 1) * 128, hf * 512:(hf + 1) * 512], 1,
                               wdst.ap()[:, kc * D + hf * 512:kc * D + (hf + 1) * 512], wkey))
    cvi = [0]

    def emit_cv(njobs):
        for _ in range(njobs):
            if cvi[0] >= len(cvjobs):
                return
            src, nk, dst, dkey = cvjobs[cvi[0]]
            i = cvi[0] % 2
            cvi[0] += 1
            stv = v3(cvf[i][:], nk) if nk > 1 else cvf[i][:]
            dma("sp", stv, src, (), ("cvf%d" % i,), "cvi%d" % i)
            cp("pool", cvb[i][:], cvf[i][:], ("cvf%d" % i,), ("cvb%d" % i,))
            dma("sp", dst, cvb[i][:], ("cvb%d" % i,), (dkey,), "cvo%d" % i)

    for ch in range(OPTS["nch"]):
        tc0 = ch * 512
        emit_cv(8)
        hk = ("hT%d" % ch,)
        QNc = QN[ch % 2]
        qnk = "QN%d" % (ch % 2)
        QNv = QNc[:].rearrange("p (b h q) -> p b h q", b=4, h=4)
        YTc = YT[ch % 2]
        ytk = "YT%d" % (ch % 2)
        YTv = v3(YTc[:], 4)
        for h in range(4):
            pbk = h % 2
            fm_proj(h * 64, 64, pbk, tc0, hk)
            act(QNv[0:64, :, h, :], PS[pbk][0:64, :].rearrange("p (b q) -> p b q", b=4), AF.Identity,
                ("ps%d" % pbk,), (qnk + "q",), scale=0.125)
        for tl in range(4):
            n = ch * 4 + tl
            pbk = tl % 2
            for kc in range(8):
                mm(PS[pbk][:, 0:W2C], hTv[:, kc, n * 128:(n + 1) * 128], W2v[:, kc, :], kc == 0, kc == 7,
                   ("W2b",) + hk, ("ps%d" % pbk,))
            cp("dve", VSv[:, n, 0:64], PS[pbk][:, 0:64], ("ps%d" % pbk,), ("VS",))
            cp("dve", VWv[:, n, 0:64], PS[pbk][:, 64:128], ("ps%d" % pbk,), ("VW",))
            act(GSv[:, n, :], PS[pbk][:, 128:140], AF.Sigmoid, ("ps%d" % pbk,), ("GS",))
            cp("act", VBv[:, tl, :], PS[pbk][:, 140:396], ("ps%d" % pbk,), ("VB",))

        begin("P")
        fm_proj(512, 64, 0, tc0, hk)
        cp("act", KE[0:64, tc0:tc0 + 512], PS[0][0:64, :], ("ps0",), ("KEk",))
        fm_proj(576, 64, 1, tc0, hk)
        cp("dve", KwT[:, tc0:tc0 + 512], PS[1][0:64, :], ("ps1",), ("KwT",))
        gqv = v3(gq[:], 2)
        gkv = v3(gk[:], 2)
        for h in range(2):
            fm_proj(640 + h * 64, 64, 0, tc0, hk)
            act(gqv[:, h, :], PS[0][0:64, :], AF.Identity, ("ps0",), ("gq",), scale=0.125)
            fm_proj(768 + h * 64, 64, 1, tc0, hk)
            cp("dve", gkv[:, h, :], PS[1][0:64, :], ("ps1",), ("gk",))
        fm_proj(896, 16, 0, tc0, hk)
        cp("act", gab[:], PS[0][0:16, :], ("ps0",), ("gab",))
        rTv = v3(rTs[:], 2)
        for h in range(2):
            fm_proj(912 + h * 128, 128, 1, tc0, hk)
            act(rTv[:, h, :], PS[1][:, :], AF.Silu, ("ps1",), ("rTs",))
        ge1v = v3(ge1[:], 2)
        gbcv = v3(gbc[:], 2)
        gAqv = v3(gAq[:], 2)
        gAkv = v3(gAk[:], 2)
        qinv = v3(qin[:], 2)
        kinv = v3(kin[:], 2)
        kdTv = v3(kdT[:], 2)
        gdecv = v3(gdec[:], 2)
        for h in range(2):
            mm(PS[0][0:64, :], walb[:, h * 64:(h + 1) * 64], gab[:], True, True, ("walb", "gab"), ("ps0",))
            act(ge1v[:, h, :], PS[0][0:64, :], AF.Exp, ("ps0", "nbal"), ("ge1",), bias=nbal[:, h:h + 1], scale=-1.0)
            act(ge1v[:, h, :], ge1v[:, h, :], AF.Ln, ("ge1",), ("ge1",), bias=1.0)
            A("dve", lambda e, o=gbcv[:, h, :], d1=ge1v[:, h, :]: e.tensor_tensor_scan(
                out=o, data0=scanm[:], data1=d1, initial=0.0, op0=ALU.mult, op1=ALU.add),
              ("ge1", "scanm"), ("gbc",))
        act(gAq[:], gbc[:], AF.Exp, ("gbc",), ("gAq",), scale=-1.0 / 16.0)
        act(gAk[:], gbc[:], AF.Exp, ("gbc",), ("gAk",), scale=1.0 / 16.0)
        tt("dve", qin[:], gq[:], gAq[:], ALU.mult, ("gq", "gAq"), ("qin",))
        tt("dve", kin[:], gk[:], gAk[:], ALU.mult, ("gk", "gAk"), ("kin",))
        for h in range(2):
            cp("dve", gdecv[:, h, :], gAqv[:, h, 63:512:64], ("gAq",), ("gdec",))
            tt("dve", kdTv[:, h, :].rearrange("p (c t) -> p c t", c=8),
               kinv[:, h, :].rearrange("p (c t) -> p c t", c=8),
               gdecv[:, h, :].unsqueeze(2).to_broadcast([64, 8, 64]), ALU.mult, ("kin", "gdec"), ("kdT",))

        end()
        def emit_A1(n, tl):
            for h in range(4):
                mm(PS[6][:, 0:256], QNv[0:64, tl, h, :], kcbT[:], True, True, (qnk + "q", "kcbT"), ("ps6",))
                o0 = 248 - 8 * n
                tt("dve", scc[:], PS[6][:, 0:256], Wtv[:, h, o0:o0 + 256], ALU.add, ("ps6", "Wt"), ("scc",))
                ms("dve", ssum[:, h:h + 1], 0.0, ("ssum",))
                act(ecb[:], scc[:], AF.Exp, ("scc", "ssum"), ("ecb", "ssum"), accum=ssum[:, h:h + 1])
                for cc in range(2):
                    tr(psb6[:, cc * 128:(cc + 1) * 128], ecb[:, cc * 128:(cc + 1) * 128], identb[:],
                       ("ecb", "identb"), ("ps6",))
                cp("act", eT[:], psb6[:, :], ("ps6",), ("eT",))
                for cc in range(2):
                    mm(PS[6][:, 256:384], eT[:, cc * 128:(cc + 1) * 128], VAv[:, cc, :], cc == 0, cc == 1,
                       ("eT", "VAUGv", "VAUGo"), ("ps6",))
                ts("dve", rrC[:, h:h + 1], ssum[:, h:h + 1], 1e-30, None, ALU.max, None, ("ssum",), ("rrC",))
                recip(rrC[:, h:h + 1], rrC[:, h:h + 1], ("rrC",), ("rrC",))
                tt("dve", coefC[:, h:h + 1], rrC[:, h:h + 1], GSv[:, n, h:h + 1], ALU.mult, ("rrC", "GS"), ("coefC",))
                ts("dve", yc[n % 2][:, h * 64:(h + 1) * 64], PS[6][:, 256:320], coefC[:, h:h + 1], None, ALU.mult, None,
                   ("ps6", "coefC"), ("yc%d" % (n % 2),))
                if h == 0:
                    ts("dve", imp[:], PS[6][:, 320:384], rrC[:, h:h + 1], None, ALU.mult, None, ("ps6", "rrC"), ("imp",))
                else:
                    stt("dve", imp[:], PS[6][:, 320:384], rrC[:, h:h + 1], imp[:], ALU.mult, ALU.add,
                        ("ps6", "rrC", "imp"), ("imp",))
            k0 = 64 - 2 * n
            tt("dve", impm[:], imp[:], KEEP[:, k0:k0 + 64], ALU.mult, ("imp", "KEEP"), ("impm",))
            tt("dve", impm[:], impm[:], ADDT[:, k0:k0 + 64], ALU.add, ("impm", "ADDT"), ("impm",))
            ms("dve", impm[:, 0:1], 1e30, ("impm",))
            A("dve", lambda e: e.max(out=m8[:, 0:8], in_=impm[:]), ("impm",), ("m8",))
            A("dve", lambda e: e.match_replace(out=imp2[:], in_to_replace=m8[:, 0:8], in_values=impm[:],
                                              imm_value=-3e38), ("impm", "m8"), ("imp2",))
            A("dve", lambda e: e.max(out=m8[:, 8:16], in_=imp2[:]), ("imp2",), ("m8",))
            ts("dve", negp[:, 64:128], impm[:], m8[:, 15:16], NEG, ALU.is_lt, ALU.mult, ("impm", "m8"), ("negp",))
            tr(psb6[:, 0:128], negp[:], identb[:], ("negp", "negp0", "identb"), ("ps6",))
            for h in range(4):
                cp("act" if h % 2 else "dve", QNv[64:128, tl, h, :], psb6[64:128, 0:128], ("ps6",), (qnk + "n%d" % tl,))
        for tl in range(4):
            n = ch * 4 + tl
            if tl == 0:
                begin("A1")
                emit_A1(n, tl)
                end()
                merge(["P", "A1"])
            begin("A")
            for br in range(2):
                if br == 1:
                    end()
                    begin("B")
                kbs = list(range(0, n + 1)) if br == 0 else list(range(max(0, n - 4), n + 1))
                accb = PS[4] if br == 0 else PS[5]
                acck = "ps4" if br == 0 else "ps5"
                accv = accb[:, 0:260].rearrange("p (h c) -> p h c", h=4)
                Vv = VSv if br == 0 else VWv
                vk = "VS" if br == 0 else "VW"
                rrX = rr if br == 0 else rrB
                coefX = coef if br == 0 else coefB
                rrk = "rr" if br == 0 else "rrB"
                cfk = "coef" if br == 0 else "coefB"
                for ki, kb in enumerate(kbs):
                    sb_ = (2 + (sidx[br] % 2)) if br == 0 else 1
                    pi_ = (0 if br == 0 else 2) + (sidx[br] % 2)
                    pt = PT[pi_]
                    ptk = "PT%d" % pi_
                    sidx[br] += 1
                    extra = []
                    if kb == n:
                        extra.append((T0, "T0"))
                    if kb == n - 1:
                        extra.append((T1, "T1"))
                    if br == 1 and kb == n - 4:
                        extra.append((T4, "T4"))
                    if br == 0:
                        mm(PS[sb_][:, :], KE[:, kb * 128:(kb + 1) * 128], QNc[:, tl * 512:(tl + 1) * 512],
                           True, not extra, ("KEk", "KEexp", qnk + "q", qnk + "n%d" % tl), ("ps%d" % sb_,))
                    else:
                        mm(PS[sb_][:, :], KwT[:, kb * 128:(kb + 1) * 128], QNc[0:64, tl * 512:(tl + 1) * 512],
                           True, not extra, ("KwT", qnk + "q"), ("ps%d" % sb_,))
                    for xi, (tb, tk) in enumerate(extra):
                        mm(PS[sb_][:, :], identb[:], tb[:], False, xi == len(extra) - 1, ("identb", tk), ("ps%d" % sb_,))
                    act(pt[:], PS[sb_][:, :], AF.Exp, ("ps%d" % sb_,), (ptk,))
                    for h in range(4):
                        mm(accv[:, h, :], pt[:, h * 128:(h + 1) * 128], Vv[:, kb, :], ki == 0 and h == 0, ki == len(kbs) - 1,
                           (ptk, vk, vk + "one"), (acck,), sgc=True)
                gofs = 4 if br == 0 else 8
                cp("dve", rrX[:, 0:4].unsqueeze(2), accv[:, :, 64:65], (acck,), (rrk,))
                recip(rrX[:, 0:4], rrX[:, 0:4], (rrk,), (rrk,))
                tt("dve", coefX[:, 0:4], rrX[:, 0:4], GSv[:, n, gofs:gofs + 4], ALU.mult, (rrk, "GS"), (cfk,))
                if debug and br == 0:
                    dma("sp", dbg_br[0, n * 128:(n + 1) * 128, :], yc[n % 2][:], ("yc%d" % (n % 2),), ("dbgbr",), "dbgb")
                for h in range(4):
                    if debug:
                        ts("dve", ybr[br][:, h * 64:(h + 1) * 64], accv[:, h, 0:64], coefX[:, h:h + 1], None, ALU.mult, None,
                           (acck, cfk), ("ybr%d" % br,))
                    if br == 0:
                        stt("dve", ya[:, h * 64:(h + 1) * 64], accv[:, h, 0:64], coefX[:, h:h + 1],
                            yc[n % 2][:, h * 64:(h + 1) * 64], ALU.mult, ALU.add, (acck, cfk, "yc%d" % (n % 2)), ("ya",))
                    else:
                        ts("dve", yw[:, h * 64:(h + 1) * 64], accv[:, h, 0:64], coefX[:, h:h + 1], None, ALU.mult, None,
                           (acck, cfk), ("yw",))
                if debug:
                    dma("sp", dbg_br[1 + br, n * 128:(n + 1) * 128, :], ybr[br][:], ("ybr%d" % br,), ("dbgbr",), "dbgb")
            end()
            begin("C")
            for h in range(2):
                ts0 = tl * 128
                mm(PS[7][:, 0:128], kinv[:, h, ts0:ts0 + 128], qinv[:, h, ts0:ts0 + 128], True, True,
                   ("kin", "qin"), ("ps7",))
                atb = ATb[h]
                tt("dve", atb[:], PS[7][:, 0:128], MTs[:], ALU.mult, ("ps7", "MTs"), ("ATb%d" % h,))
                tr(psb7[:, 0:64], kdTv[:, h, ts0:ts0 + 128], identb[0:64, 0:64], ("kdT", "identb"), ("ps7",))
                cp("act", kdtok[h][:], psb7[:, 0:64], ("ps7",), ("kdtok%d" % h,))
                mm(PS[7][0:64, 256:384], kdtok[h][0:64, :], VBv[0:64, tl, h * 128:(h + 1) * 128], True, True,
                   ("kdtok%d" % h, "VB"), ("ps7",))
                mm(PS[0][0:64, 0:128], kdtok[h][64:128, :], VBv[64:128, tl, h * 128:(h + 1) * 128], True, True,
                   ("kdtok%d" % h, "VB"), ("ps0",))
                stt("dve", St[h][:], St[h][:], gdecv[:, h, tl * 2:tl * 2 + 1], PS[7][0:64, 256:384], ALU.mult, ALU.add,
                    ("St%d" % h, "gdec", "ps7"), ("St%d" % h,))
                cp("act", Sbf[h][1][:], St[h][:], ("St%d" % h,), ("Sbf%d_1" % h,))
                mm(PS[7][:, 128:192], Sbf[h][0][:], qinv[:, h, ts0:ts0 + 64], True, False, ("Sbf%d_0" % h, "qin"), ("ps7",), sgc=True)
                mm(PS[7][:, 192:256], Sbf[h][1][:], qinv[:, h, ts0 + 64:ts0 + 128], False, False, ("Sbf%d_1" % h, "qin"), ("ps7",), sgc=True)
                mm(PS[7][:, 128:256], VBv[:, tl, h * 128:(h + 1) * 128], atb[:], False, True, ("VB", "ATb%d" % h), ("ps7",), sgc=True)
                stt("dve", St[h][:], St[h][:], gdecv[:, h, tl * 2 + 1:tl * 2 + 2], PS[0][0:64, 0:128], ALU.mult, ALU.add,
                    ("St%d" % h, "gdec", "ps0"), ("St%d" % h,))
                cp("act", Sbf[h][0][:], St[h][:], ("St%d" % h,), ("Sbf%d_0" % h,))
                act(gsq[:], PS[7][:, 128:256], AF.Square, ("ps7",), ("gsq",))
                mm(PS[7][:, 0:128], onesb[:], gsq[:], True, True, ("onesb", "gsq"), ("ps7",))
                act(gl1[:], PS[7][:, 0:128], AF.Ln, ("ps7",), ("gl1",), bias=EPS, scale=1.0 / 128.0)
                act(grs[:], gl1[:], AF.Exp, ("gl1",), ("grs",), scale=-0.5)
                tt("dve", gyb[:], PS[7][:, 128:256], grs[:], ALU.mult, ("ps7", "grs"), ("gyb",))
                stt("dve", YTv[:, 2 + h, ts0:ts0 + 128], gyb[:], ngs[:, h:h + 1], rTv[:, h, ts0:ts0 + 128],
                    ALU.mult, ALU.mult, ("gyb", "ngs", "rTs"), (ytk,))
            end()
            if tl < 3:
                begin("A1")
                emit_A1(n + 1, tl + 1)
                end()
                merge(["A", "B", "C", "A1"])
            else:
                merge(["A", "B", "C"])
            tt("dve", ya[:], ya[:], yw[:], ALU.add, ("ya", "yw"), ("ya",))
            cp("act", yab[:], ya[:], ("ya",), ("yab",))
            for cc in range(2):
                tr(psb5[:, cc * 128:(cc + 1) * 128], yab[:, cc * 128:(cc + 1) * 128], identb[:], ("yab", "identb"), ("ps4",))
            cp("act", YTv[:, 0:2, tl * 128:(tl + 1) * 128], psb5[:, 0:256].rearrange("p (c q) -> p c q", c=2),
               ("ps4",), (ytk,))
        dma("act", ysrc[ch].ap().rearrange("(c p) t -> p c t", p=128), YTv, (ytk,), ("ysrc%d" % ch,), "ys%d" % ch)
        P.add("pool", lambda e, ch=ch: e.collective_compute("AllGather", ALU.bypass,
                                                          replica_groups=[[0, 1], [2, 3], [4, 5], [6, 7]],
                                                          ins=[ysrc[ch].ap()], outs=[ydst[ch].ap()]),
              ("ysrc%d" % ch,), ("ydst%d" % ch,), dma="cc%d" % ch, inc=1)

    if stop <= 3:
        P.stopped = True
    if debug:
        for i in range(8):
            dma("sp", dbg_y[:, i * 512:(i + 1) * 512], ydst[i].ap(), ("ydst%d" % i,), ("dbgy",), "dbg")

    if stop <= 4:
        P.stopped = True
    HXv = v3(HX[:], 8)
    P.barrier(["pe", "act", "dve", "pool", "sp"])
    for kc in range(8):
        eng = "dve"
        ts(eng, HXv[:, kc, :], hTv[:, kc, 0:HALF], s01[:, 0:1], None, ALU.mult, None,
           tuple("hT%d" % i for i in range(8)) + ("s01",), ("HX",))
        stt(eng, HXv[:, kc, :], hTv[:, kc, HALF:S], s01[:, 1:2], HXv[:, kc, :], ALU.mult, ALU.add,
            tuple("hT%d" % i for i in range(8)) + ("s01", "HX"), ("HX",))
    P.barrier(["pe", "act", "dve", "pool", "sp"])

    ACC = BIG
    ACCv = v3(ACC[:], 8)
    wcnt2 = [0]

    def load_w2(dstv, src_view, nk, ncols, dstkey, q="sp"):
        c0 = 0
        while c0 < ncols:
            cw = min(2048 // nk, ncols - c0)
            i = wcnt2[0] % 2
            wcnt2[0] += 1
            st = v3(wst2[i][:, 0:nk * cw], nk)
            dma(q, st, src_view[:, :, c0:c0 + cw], (), ("wst2_%d" % i,), "v%d" % i)
            cp("act" if (wcnt2[0] % 2) else "pool", dstv[:, :, c0:c0 + cw], st, ("wst2_%d" % i,), (dstkey,))
            c0 += cw

    Wav = v3(Wab[:], 4)
    Wbv = v3(Wbb[:], 4)
    Wov = v3(Wob[:], 8)
    emit_cv(len(cvjobs))
    dma("sp", Wab[:], wab_s.ap(), ("wab_s",), ("Wab",), "wl")
    dma("sp", Wbb[:], wbb_s.ap(), ("wbb_s",), ("Wbb",), "wl")
    dma("sp", Wob[:], wob_s.ap(), ("wob_s",), ("Wob",), "wl")
    dma("sp", v3(WRf[:], 8), wr_d.rearrange("(kc p) n -> p kc n", p=128), (), ("WRf0",), "c6")
    WRv = v3(WRf[:], 8)
    for kc in range(8):
        ts("dve", WRv[:, kc, :], WRv[:, kc, :], gv[:, 8 + kc:9 + kc], None, ALU.mult, None, ("WRf0", "gv"), ("WRf",))
    dma("sp", brs[:], br_d, (), ("brs",), "c6")
    ms("dve", onesf[:], 1.0, ("onesf",))

    ydv = [y_.ap().rearrange("(c p) t -> p c t", p=128) for y_ in ydst]
    w3v = w3.rearrange("(kc p) n -> p kc n", p=128)
    xhd = xTh.rearrange("(kc p) t -> p kc t", p=128)
    YMv = v3(YM[:], 8)
    MGv = v3(MG[:], 8)
    sq2v = v3(sq2[:], 8)
    FA = (0, 1, 4, 5)
    FB = (2, 3, 6, 7)
    w3c = [0]
    def emit_routing(tcx, t0):
        for tl in range(4):
            tt0 = t0 + tl * 128
            for kc in range(8):
                mm(PS[7][:, 0:36], ACCv[:, kc, tt0:tt0 + 128], WRv[:, kc, :], kc == 0, kc == 7,
                   ("ACC%d" % tcx, "WRf"), ("ps7",))
            mm(PS[7][:, 64:65], rstd2[tcx % 2][0:1, tl * 128:(tl + 1) * 128], identf[0:1, 0:1], True, True,
               ("rstd2_%d" % (tcx % 2), "identf"), ("ps7",))
            cp("dve", rt[:, 0:1], PS[7][:, 64:65], ("ps7",), ("rt0",))
            stt("dve", Lg[:], PS[7][:, 0:36], rt[:, 0:1], brs[:], ALU.mult, ALU.add, ("ps7", "rt0", "brs"), ("Lg",))
            A("dve", lambda e: e.reduce_max(out=rt[:, 1:2], in_=Lg[:, 0:4], axis=AX.X), ("Lg",), ("rt1",))
            ts("dve", rt[:, 2:3], rt[:, 1:2], -1.0, None, ALU.mult, None, ("rt1",), ("rt2",))
            ms("dve", rt[:, 3:4], 0.0, ("rt3",))
            act(rt[:, 8:12], Lg[:, 0:4], AF.Exp, ("Lg", "rt2", "rt3"), ("rt8", "rt3"), bias=rt[:, 2:3], accum=rt[:, 3:4])
            recip(rt[:, 4:5], rt[:, 3:4], ("rt3",), ("rt4",))
            ts("dve", rt[:, 12:16], Lg[:, 0:4], rt[:, 1:2], None, ALU.is_ge, None, ("Lg", "rt1"), ("rt12",))
            for gi in range(4):
                if gi == 0:
                    ts("dve", esel[:], Lg[:, 4:12], rt[:, 12:13], None, ALU.mult, None, ("Lg", "rt12"), ("esel",))
                else:
                    stt("dve", esel[:], Lg[:, 4 + 8 * gi:12 + 8 * gi], rt[:, 12 + gi:13 + gi], esel[:], ALU.mult, ALU.add,
                        ("Lg", "rt12", "esel"), ("esel",))
            A("dve", lambda e: e.max(out=rt[:, 16:24], in_=esel[:]), ("esel",), ("rt16",))
            tt("dve", rt[:, 24:25], rt[:, 17:18], rt[:, 16:17], ALU.subtract, ("rt16",), ("rt24",))
            act(rt[:, 25:26], rt[:, 24:25], AF.Exp, ("rt24",), ("rt25",))
            ts("dve", rt[:, 25:26], rt[:, 25:26], 1.0, None, ALU.add, None, ("rt25",), ("rt25",))
            recip(rt[:, 26:27], rt[:, 25:26], ("rt25",), ("rt26",))
            ts("dve", rt[:, 27:28], rt[:, 26:27], -1.0, 1.0, ALU.mult, ALU.add, ("rt26",), ("rt27",))
            tt("dve", rt[:, 26:27], rt[:, 26:27], rt[:, 4:5], ALU.mult, ("rt26", "rt4"), ("rt26",))
            tt("dve", rt[:, 27:28], rt[:, 27:28], rt[:, 4:5], ALU.mult, ("rt27", "rt4"), ("rt27",))
            ts("dve", rt[:, 32:40], esel[:], rt[:, 16:17], rt[:, 26:27], ALU.is_equal, ALU.mult, ("esel", "rt16", "rt26"), ("rt32",))
            ts("dve", rt[:, 40:48], esel[:], rt[:, 17:18], rt[:, 27:28], ALU.is_equal, ALU.mult, ("esel", "rt16", "rt27"), ("rt40",))
            tt("dve", rt[:, 32:40], rt[:, 32:40], rt[:, 40:48], ALU.add, ("rt32", "rt40"), ("rt32",))
            for gi in range(4):
                ts("dve", comb[:, gi * 8:(gi + 1) * 8], rt[:, 32:40], rt[:, 12 + gi:13 + gi], None, ALU.mult, None,
                   ("rt32", "rt12"), ("comb",))
            tr(PS[7][0:32, 128:256], comb[:], identf[:], ("comb", "identf"), ("ps7",))
            cp("act", COMBT[:, tt0:tt0 + 128], PS[7][0:32, 128:256], ("ps7",), ("COMBT%d" % tcx,))

    for tcx in range(4):
        t0 = tcx * 512
        begin("M")
        dma("sp", v3(YH[:], 8), ydv[tcx], ("ydst%d" % tcx,), ("YH",), "yh")
        ts("dve", YM[:], YH[:], s01[:, 0:1], None, ALU.mult, None, ("YH", "s01"), ("YM",))
        dma("sp", v3(YH[:], 8), ydv[4 + tcx], ("ydst%d" % (4 + tcx),), ("YH",), "yh")
        stt("dve", YM[:], YH[:], s01[:, 1:2], YM[:], ALU.mult, ALU.add, ("YH", "s01", "YM"), ("YM",))
        for cc in range(8):
            sgs = []
            for ab in range(2):
                i = w3c[0] % 4
                w3c[0] += 1
                dma("sp", W3p[i][:], w3b[ab * 8 + cc].ap(), ("w3b%d" % (ab * 8 + cc),), ("W3p%d" % i,), "w3p%d" % i)
                W3pv = v3(W3p[i][:], 8)
                for kc in range(8):
                    mm(PS[ab][:, :], W3pv[:, kc, :], HXv[:, kc, t0:t0 + 512], kc == 0, kc == 7,
                       ("W3p%d" % i, "HX"), ("ps%d" % ab,))
                act(SGp[ab][:], PS[ab][:, :], AF.Sigmoid, ("ps%d" % ab,), ("SGp%d" % ab,))
            for j, fc in enumerate(FA):
                mm(PS[2][:, :], Wav[:, j, cc * 128:(cc + 1) * 128], YMv[:, fc, :], j == 0, j == 3, ("Wab", "YM"), ("ps2",))
            for j, fc in enumerate(FB):
                mm(PS[3][:, :], Wbv[:, j, cc * 128:(cc + 1) * 128], YMv[:, fc, :], j == 0, j == 3, ("Wbb", "YM"), ("ps3",))
            tt("dve", m1[:], PS[2][:, :], SGp[0][:], ALU.mult, ("ps2", "SGp0"), ("m1",))
            tt("dve", m2[:], PS[3][:, :], SGp[1][:], ALU.mult, ("ps3", "SGp1"), ("m2",))
            tt("dve", MGv[:, cc, :], m1[:], m2[:], ALU.add, ("m1", "m2"), ("MG",))
        for co in range(8):
            pbk = 4 + co % 2
            xb = xh[co % 2]
            dma("sp", xb[:], xhd[:, co, t0:t0 + 512], (), ("xh%d" % (co % 2),), "xh%d" % (co % 2))
            for cc in range(8):
                mm(PS[pbk][:, :], Wov[:, cc, co * 128:(co + 1) * 128], MGv[:, cc, :], cc == 0, cc == 7,
                   ("Wob", "MG"), ("ps%d" % pbk,))
            tt("dve", ACCv[:, co, t0:t0 + 512], PS[pbk][:, :], xb[:], ALU.add, ("ps%d" % pbk, "xh%d" % (co % 2)), ("ACC%d" % tcx,))
        for kc in range(8):
            act(sq2v[:, kc, :], ACCv[:, kc, t0:t0 + 512], AF.Square, ("ACC%d" % tcx,), ("YH",))
        for kc in range(8):
            mm(PS[6][:, :], onesb[:], sq2v[:, kc, :], kc == 0, kc == 7, ("onesb", "YH"), ("ps6",))
        act(rstd2[tcx % 2][:], PS[6][:, :], AF.Sqrt, ("ps6",), ("rstd2_%d" % (tcx % 2),), bias=EPS, scale=1.0 / D)
        recip(rstd2[tcx % 2][:], rstd2[tcx % 2][:], ("rstd2_%d" % (tcx % 2),), ("rstd2_%d" % (tcx % 2),))
        for kc in range(8):
            stt("dve", HXv[:, kc, t0:t0 + 512], ACCv[:, kc, t0:t0 + 512], gv[:, 8 + kc:9 + kc], rstd2[tcx % 2][:],
                ALU.mult, ALU.mult, ("ACC%d" % tcx, "rstd2_%d" % (tcx % 2), "gv"), ("HX",))
        end()
        if tcx > 0:
            begin("R")
            emit_routing(tcx - 1, (tcx - 1) * 512)
            end()
            merge(["M", "R"])
        else:
            merge(["M"])
    emit_routing(3, 3 * 512)
    if debug:
        dma("sp", dbg_x1.rearrange("(kc p) t -> p kc t", p=128), ACCv, tuple("ACC%d" % i for i in range(4)), ("dbgx",), "dbg")

    if stop <= 5:
        P.stopped = True
    P.barrier(["pe", "act", "dve", "pool", "sp"])
    dma("sp", gfs[:], gfin, (), ("gfs",), "c7")
    NE = OPTS["nexp"]

    def moe_load(e_):
        i = e_ % 4
        load_w2(v3(WGb[i][:], 8), wg_d[e_].rearrange("(kc p) n -> p kc n", p=128), 8, 256, "WGb%d" % i, q="sp")
        load_w2(v3(WUb[i][:], 8), wu_d[e_].rearrange("(kc p) n -> p kc n", p=128), 8, 256, "WUb%d" % i, q="sp")
        load_w2(v3(WDb[i][:], 2), wd_d[e_].rearrange("(kc p) n -> p kc n", p=128), 2, D, "WDb%d" % i, q="sp")

    def moe_prep(st):
        p, tcx = divmod(st, 4)
        t0 = tcx * 512
        for x in range(2):
            e_ = 2 * p + x
            j = (st % 2) * 2 + x
            ts("dve", cmask[j][:], COMBT[:, t0:t0 + 512], identf[0:32, e_:e_ + 1], None, ALU.mult, None,
               ("COMBT%d" % tcx, "identf"), ("cmask%d" % j,))

    def moe_gu(st):
        p, tcx = divmod(st, 4)
        t0 = tcx * 512
        for x in range(2):
            j = (st % 2) * 2 + x
            mm(PS[6 + x][:, :], onesb[0:32, :], cmask[j][:], True, True, ("onesb", "cmask%d" % j), ("ps%d" % (6 + x),))
            cp("act", CB[j][:], PS[6 + x][:, :], ("ps%d" % (6 + x),), ("CB%d" % j,))
        for x in range(2):
            e_ = 2 * p + x
            i = e_ % 4
            j = (st % 2) * 2 + x
            WGv = v3(WGb[i][:], 8)
            WUv = v3(WUb[i][:], 8)
            hdv = v3(hid[j][:], 2)
            for fc in range(2):
                for kc in range(8):
                    mm(PS[fc][:, :], WGv[:, kc, fc * 128:(fc + 1) * 128], HXv[:, kc, t0:t0 + 512], kc == 0, kc == 7,
                       ("WGb%d" % i, "HX"), ("ps%d" % fc,))
                for kc in range(8):
                    mm(PS[2 + fc][:, :], WUv[:, kc, fc * 128:(fc + 1) * 128], HXv[:, kc, t0:t0 + 512], kc == 0, kc == 7,
                       ("WUb%d" % i, "HX"), ("ps%d" % (2 + fc),))
                act(sil[fc][:], PS[fc][:, :], AF.Silu, ("ps%d" % fc,), ("sil%d" % fc,))
                tt("dve", t1b[fc][:], sil[fc][:], PS[2 + fc][:, :], ALU.mult, ("sil%d" % fc, "ps%d" % (2 + fc)), ("t1b%d" % fc,))
                tt("dve", hdv[:, fc, :], t1b[fc][:], CB[j][:], ALU.mult, ("t1b%d" % fc, "CB%d" % j), ("hid%d" % j,))

    def moe_down(st):
        p, tcx = divmod(st, 4)
        t0 = tcx * 512
        for co in range(8):
            pbk = 4 + co % 2
            k = 0
            for x in range(2):
                i = (2 * p + x) % 4
                j = (st % 2) * 2 + x
                WDv = v3(WDb[i][:], 2)
                hdv = v3(hid[j][:], 2)
                for fc in range(2):
                    mm(PS[pbk][:, :], WDv[:, fc, co * 128:(co + 1) * 128], hdv[:, fc, :], k == 0, k == 3,
                       ("WDb%d" % i, "hid%d" % j), ("ps%d" % pbk,))
                    k += 1
            tt("dve", ACCv[:, co, t0:t0 + 512], PS[pbk][:, :], ACCv[:, co, t0:t0 + 512], ALU.add,
               ("ps%d" % pbk, "ACC%d" % tcx), ("ACC%d" % tcx,))

    NP = NE // 2
    for e_ in range(min(4, NE)):
        moe_load(e_)
    moe_prep(0)
    for st in range(NP * 4):
        moe_gu(st)
        if st + 1 < NP * 4:
            moe_prep(st + 1)
        if st > 0:
            moe_down(st - 1)
        p, tcx = divmod(st, 4)
        if tcx == 0 and p >= 1 and p + 1 < NP:
            moe_load(2 * (p + 1))
            moe_load(2 * (p + 1) + 1)
    moe_down(NP * 4 - 1)

    if stop <= 6:
        P.stopped = True
    for tl in range(16):
        tcx = tl // 4
        tt0 = tl * 128
        for co in range(8):
            tr(PS[co // 4][:, (co % 4) * 128:(co % 4 + 1) * 128], ACCv[:, co, tt0:tt0 + 128], identf[:],
               ("ACC%d" % tcx, "identf"), ("ps%d" % (co // 4),))
        cp("act", xtm[:, 0:512], PS[0][:, :], ("ps0",), ("xtm",))
        cp("dve", xtm[:, 512:1024], PS[1][:, :], ("ps1",), ("xtm",))
        ms("dve", fss[:, 0:1], 0.0, ("fss",))
        act(junk[:], xtm[:], AF.Square, ("xtm", "fss"), ("hid0", "fss"), accum=fss[:, 0:1])
        act(fss[:, 1:2], fss[:, 0:1], AF.Sqrt, ("fss",), ("fss1",), bias=EPS, scale=1.0 / D)
        recip(fss[:, 2:3], fss[:, 1:2], ("fss1",), ("fss2",))
        stt("dve", otm[:], xtm[:], fss[:, 2:3], gfs[:], ALU.mult, ALU.mult, ("xtm", "fss2", "gfs"), ("otm",))
        dma("sp", out_d[tt0:tt0 + 128, :], otm[:], ("otm",), ("outd",), "out")

    keys = P.finalize()
    P.check()
    semh = {}
    for k in ("pe", "act", "dve", "pool", "sp"):
        semh[k] = es.enter_context(nc.semaphore("s_" + k))
    for k in keys:
        semh["dma:" + k] = es.enter_context(nc.semaphore("d_" + k))
    with nc.Block() as block:
        block.tensor(lambda e: P.emit_engine("pe", e, semh))
        block.scalar(lambda e: P.emit_engine("act", e, semh))
        block.vector(lambda e: P.emit_engine("dve", e, semh))
        block.gpsimd(lambda e: P.emit_engine("pool", e, semh))
        block.sync(lambda e: P.emit_engine("sp", e, semh))
    es.close()
    return nc


_C = None


def make_in_maps(inp):
    global _C
    if _C is None:
        _C = _consts()
    C = _C
    f = lambda a: np.ascontiguousarray(a, dtype=np.float32)
    x = inp["x"]
    w_in = inp["w_in"][0]
    rb = inp["rel_bias"]
    gvec = np.concatenate([inp["g_mix"][0].reshape(8, 128).T, inp["g_ffn"][0].reshape(8, 128).T], axis=1)
    gfin = np.broadcast_to(inp["g_final"][None, :], (128, D))
    wr = np.concatenate([inp["w_router_group"][0], inp["w_router_expert"][0]], axis=1)
    br = np.broadcast_to(np.concatenate([inp["b_router_group"][0], inp["b_router_expert"][0]])[None, :], (128, 36))

    def cw1_layout(w):
        return w.reshape(2, 16, 64, 128).transpose(0, 2, 1, 3).reshape(128, 16 * 128)

    def pe_layout(pe):
        return pe.reshape(2, 16, 64).transpose(0, 2, 1).reshape(128, 16)

    cw1 = np.stack([cw1_layout(inp["nsa_cmp_k_w1"][0]), cw1_layout(inp["nsa_cmp_v_w1"][0])])
    cw2 = np.stack([inp["nsa_cmp_k_w2"][0], inp["nsa_cmp_v_w2"][0]])
    cpe = np.stack([pe_layout(inp["nsa_pe_k"][0]), pe_layout(inp["nsa_pe_v"][0])])
    shared = dict(
        w3=f(w_in[:, 2856:4904]), gvec=f(gvec), gfin=f(gfin), cw1=f(cw1), cw2=f(cw2), cpe=f(cpe),
        ovl=C["ovl"], M0=C["M0"], M4=C["M4"], MW=C["MW"], KEEP=C["KEEP"], ADD=C["ADD"], EXPM=C["EXPM"],
        identb=C["identb"], identf=C["identf"], MT=C["MT"], scanmask=C["scanmask"],
        wa=f(inp["w_branch_a"][0]), wb=f(inp["w_branch_b"][0]), wo=f(inp["w_out"][0]), wr=f(wr), br=f(br),
        SELM=C["SELM"], wg=f(inp["w_exp_gate"][0]), wu=f(inp["w_exp_up"][0]), wd=f(inp["w_exp_down"][0]),
    )
    maps = []
    for core in range(8):
        b, g = core // 2, core % 2
        hs = np.arange(4 * g, 4 * g + 4)
        cols1 = np.concatenate([
            np.arange(256 * g, 256 * g + 256),
            np.tile(np.arange(512 + 64 * g, 512 + 64 * g + 64), 2),
            np.tile(np.arange(640 + 64 * g, 640 + 64 * g + 64), 2),
            np.arange(768 + 64 * g, 768 + 64 * g + 64),
            np.arange(1024 + 64 * g, 1024 + 64 * g + 64),
            np.arange(1304 + 128 * g, 1304 + 128 * g + 128),
            np.arange(1560 + 128 * g, 1560 + 128 * g + 128),
            np.arange(2328, 2344),
            np.arange(2344 + 256 * g, 2344 + 256 * g + 256),
        ])
        gate_cols = np.concatenate([1280 + brn * 8 + hs for brn in range(3)])
        cols2 = np.concatenate([
            np.arange(896 + 64 * g, 896 + 64 * g + 64),
            np.arange(1152 + 64 * g, 1152 + 64 * g + 64),
            gate_cols,
            np.arange(1816 + 256 * g, 1816 + 256 * g + 256),
        ])
        rbh = rb[:, hs]
        t0raw = np.where(C["vis0"][:, None, :], rbh[C["idx0"]].transpose(0, 2, 1), 0.0)
        t1raw = rbh[C["idx1"]].transpose(0, 2, 1)
        wraw = np.where(C["visw"][:, None, :], rbh[C["idxw"]].transpose(0, 2, 1), 0.0)
        sel = np.zeros((128, 2), np.float32)
        sel[:, g] = 1.0
        m = dict(shared)
        m.update(
            xT=f(x[b].T), xTh=f(x[b, g * HALF:(g + 1) * HALF].T), sel01=sel,
            w1=f(w_in[:, cols1]), w2=f(w_in[:, cols2]),
            t0raw=f(t0raw.reshape(128, 512)), t1raw=f(t1raw.reshape(128, 512)), wraw=f(wraw.reshape(128, 4 * 504)),
            b31=f(np.broadcast_to(rbh[31][None, :], (128, 4))),
            wal=f(inp["gla_w_alpha"][0][:, 128 * g:128 * g + 128]),
            nbal=f(-inp["gla_b_alpha"][0][128 * g:128 * g + 128].reshape(2, 64).T),
            ng=f(inp["gla_norm_g"][0][256 * g:256 * g + 256].reshape(2, 128).T),
        )
        maps.append(m)
    return maps


_NC = {}


def kernel(**inputs):
    inp = {k: np.asarray(v) for k, v in inputs.items()}
    if "nc" not in _NC:
        _NC["nc"] = build_nc(False)
    nc = _NC["nc"]
    maps = make_in_maps(inp)
    res = run_bass_kernel_spmd(nc, maps, core_ids=list(range(8)))
    out = np.zeros((4, S, D), np.float32)
    for core in range(8):
        b, g = core // 2, core % 2
        out[b, g * HALF:(g + 1) * HALF] = res.results[core]["out"]
    return out
```
